# Optimizing a Trainium2 kernel written in Bass

```python
import math, functools
import jax, jax.numpy as jnp
from jax import lax
import numpy as np

D_MODEL = 1024
BATCH = 16
SEQ = 2048
DEPTH = 1
DEC_BATCH = 128
DEC_SEQ = 1
PAST_LEN = 8192
PAGE_SIZE = 128

D_MIX = D_MODEL
GLA_WIDTH = D_MIX // 2
MLA_WIDTH = D_MIX - GLA_WIDTH
GLA_HEADS = 4
GLA_DV = GLA_WIDTH // GLA_HEADS
GLA_DK = GLA_DV // 2
GLA_GATE_RANK = 16
GLA_TAU = 16.0
GLA_CHUNK = 64
MLA_HEADS = 8
MLA_DV = MLA_WIDTH // MLA_HEADS
MLA_D_NOPE = 64
MLA_D_ROPE = 32
MLA_Q_RANK = 384
MLA_KV_RANK = 256
MLA_SCALE = (MLA_D_NOPE + MLA_D_ROPE) ** -0.5
ROPE_THETA = 10000.0
Q_BLOCK = 128
MEM_TOKENS = 256
X_HEADS = 4
X_DH = D_MODEL // X_HEADS
N_GROUPS = 4
EXPERTS_PER_GROUP = 8
N_EXPERTS = N_GROUPS * EXPERTS_PER_GROUP
TOP_K_IN_GROUP = 2
D_EXPERT = 256
ALPHA = (2 * DEPTH) ** 0.25
BETA = (8 * DEPTH) ** -0.25
LN_EPS = 1e-5
RMS_EPS = 1e-6
IN_SIZES = (GLA_HEADS * GLA_DK, GLA_HEADS * GLA_DK, GLA_WIDTH, GLA_GATE_RANK, GLA_WIDTH,
            MLA_Q_RANK, MLA_KV_RANK, MLA_D_ROPE)
D_IN_PROJ = sum(IN_SIZES)

kernel_name = 'hymba_gla_mla_hmoe_deepnorm_step'


def layer_norm(x, g, b):
    xf = x.astype(jnp.float32)
    mu = jnp.mean(xf, axis=-1, keepdims=True)
    var = jnp.mean(jnp.square(xf - mu), axis=-1, keepdims=True)
    return ((xf - mu) * lax.rsqrt(var + LN_EPS) * g + b).astype(x.dtype)


def rms_norm(x, g):
    xf = x.astype(jnp.float32)
    return (xf * lax.rsqrt(jnp.mean(xf * xf, axis=-1, keepdims=True) + RMS_EPS) * g).astype(x.dtype)


def rope_angles(positions):
    inv = ROPE_THETA ** (-jnp.arange(0, MLA_D_ROPE, 2, dtype=jnp.float32) / MLA_D_ROPE)
    ang = positions.astype(jnp.float32)[:, None] * inv[None, :]
    return jnp.cos(ang), jnp.sin(ang)


def apply_rope(x, cos, sin):
    x1, x2 = jnp.split(x.astype(jnp.float32), 2, axis=-1)
    c, s = cos[None, :, None, :], sin[None, :, None, :]
    return jnp.concatenate([x1 * c - x2 * s, x1 * s + x2 * c], axis=-1).astype(x.dtype)


def gla_recurrence(q, k, v, log_a, s0, chunk):
    B, L, H, dk = q.shape
    dv = v.shape[-1]
    n = L // chunk
    def to_chunks(t):
        return t.astype(jnp.float32).reshape(B, n, chunk, H, t.shape[-1]).transpose(1, 0, 3, 2, 4)
    causal = jnp.tril(jnp.ones((chunk, chunk), bool))
    def step(s, inp):
        qi, ki, vi, gi = inp
        b = jnp.cumsum(gi, axis=2)
        b_last = b[:, :, -1:, :]
        rel = jnp.where(causal[None, None, :, :, None], b[:, :, :, None, :] - b[:, :, None, :, :], -jnp.inf)
        attn = jnp.einsum('bhid,bhjd,bhijd->bhij', qi, ki, jnp.exp(rel))
        o = jnp.einsum('bhij,bhjv->bhiv', attn, vi) + jnp.einsum('bhid,bhdv->bhiv', qi * jnp.exp(b), s)
        s_new = jnp.exp(b_last)[:, :, 0, :, None] * s + jnp.einsum('bhjd,bhjv->bhdv', ki * jnp.exp(b_last - b), vi)
        return s_new, o
    s_fin, o = lax.scan(step, s0.astype(jnp.float32), (to_chunks(q), to_chunks(k), to_chunks(v), to_chunks(log_a)))
    o = o.transpose(1, 0, 3, 2, 4).reshape(B, L, H, dv)
    return o.astype(v.dtype), s_fin.astype(s0.dtype)


def gla_group(q_in, k_in, v_in, a_in, r_in, s0, w_gla_gate, b_gla_gate, gla_norm_g):
    B, L, _ = q_in.shape
    def heads(t, d):
        return t.reshape(B, L, GLA_HEADS, d)
    q = heads(q_in, GLA_DK) * (GLA_DK ** -0.5)
    k = heads(k_in, GLA_DK)
    v = heads(v_in, GLA_DV)
    log_a = jax.nn.log_sigmoid((a_in @ w_gla_gate + b_gla_gate).astype(jnp.float32)) / GLA_TAU
    o, s = gla_recurrence(q, k, v, heads(log_a, GLA_DK), s0, math.gcd(L, GLA_CHUNK))
    o = rms_norm(o, gla_norm_g) * jax.nn.silu(heads(r_in, GLA_DV))
    return o.reshape(B, L, GLA_WIDTH), s


def mla_project(cq_in, ckv_in, kr_in, positions, mla_q_norm_g, w_uq, mla_kv_norm_g):
    q = jnp.einsum('blr,rhd->blhd', rms_norm(cq_in, mla_q_norm_g), w_uq)
    cos, sin = rope_angles(positions)
    q_nope = q[..., :MLA_D_NOPE]
    q_pe = apply_rope(q[..., MLA_D_NOPE:], cos, sin)
    c_kv = rms_norm(ckv_in, mla_kv_norm_g)
    k_pe = apply_rope(kr_in[:, :, None, :], cos, sin)[:, :, 0, :]
    return q_nope, q_pe, c_kv, k_pe


def mla_prompt_attention(q_nope, q_pe, c_kv, k_pe, w_uk, w_uv):
    B, L, H, _ = q_nope.shape
    k_nope = jnp.einsum('blr,rhd->blhd', c_kv, w_uk)
    v = jnp.einsum('blr,rhd->blhd', c_kv, w_uv)
    n_blk = L // Q_BLOCK
    def blocks(t):
        return t.reshape(B, n_blk, Q_BLOCK, *t.shape[2:]).swapaxes(0, 1)
    key_pos = jnp.arange(L)
    def one_block(args):
        qn, qp, blk = args
        s = (jnp.einsum('bqhd,bkhd->bhqk', qn, k_nope, preferred_element_type=jnp.float32)
             + jnp.einsum('bqhd,bkd->bhqk', qp, k_pe, preferred_element_type=jnp.float32)) * MLA_SCALE
        q_pos = blk * Q_BLOCK + jnp.arange(Q_BLOCK)
        s = jnp.where(q_pos[:, None] >= key_pos[None, :], s, -jnp.inf)
        p = jax.nn.softmax(s, axis=-1).astype(v.dtype)
        return jnp.einsum('bhqk,bkhd->bqhd', p, v)
    o = lax.map(one_block, (blocks(q_nope), blocks(q_pe), jnp.arange(n_blk)))
    return o.swapaxes(0, 1).reshape(B, L, H * MLA_DV)


def mla_sample_attention(q_nope, q_pe, c_new, kpe_new, w_uk, w_uv, past_c, past_kpe):
    B, T, H, _ = q_nope.shape
    f32 = jnp.float32
    q_lat = jnp.einsum('bthd,rhd->bthr', q_nope, w_uk)
    s_past = (jnp.einsum('bthr,bpr->bhtp', q_lat, past_c, preferred_element_type=f32)
              + jnp.einsum('bthd,bpd->bhtp', q_pe, past_kpe, preferred_element_type=f32))
    s_new = (jnp.einsum('bthr,bur->bhtu', q_lat, c_new, preferred_element_type=f32)
             + jnp.einsum('bthd,bud->bhtu', q_pe, kpe_new, preferred_element_type=f32))
    s_new = jnp.where(jnp.tril(jnp.ones((T, T), bool)), s_new, -jnp.inf)
    p = jax.nn.softmax(jnp.concatenate([s_past, s_new], axis=-1) * MLA_SCALE, axis=-1).astype(past_c.dtype)
    n_past = past_c.shape[1]
    o_lat = (jnp.einsum('bhtp,bpr->bthr', p[..., :n_past], past_c)
             + jnp.einsum('bhtu,bur->bthr', p[..., n_past:], c_new))
    o = jnp.einsum('bthr,rhd->bthd', o_lat, w_uv)
    return o.reshape(B, T, H * MLA_DV)


def mixer(h, positions, gla_s0, mla_attend, w_in, w_gla_gate, b_gla_gate, gla_norm_g,
          mla_q_norm_g, w_uq, mla_kv_norm_g, w_uk, w_uv, w_out):
    z = h @ w_in
    split_pts = np.cumsum(IN_SIZES)[:-1].tolist()
    q_g, k_g, v_g, a_g, r_g, cq, ckv, kr = jnp.split(z, split_pts, axis=-1)
    gla_o, gla_s = gla_group(q_g, k_g, v_g, a_g, r_g, gla_s0, w_gla_gate, b_gla_gate, gla_norm_g)
    q_nope, q_pe, c_kv, k_pe = mla_project(cq, ckv, kr, positions, mla_q_norm_g, w_uq, mla_kv_norm_g)
    mla_o = mla_attend(q_nope, q_pe, c_kv, k_pe, w_uk, w_uv)
    out = jnp.concatenate([gla_o, mla_o], axis=-1) @ w_out
    return out, gla_s, c_kv, k_pe


def memory_kv(mem, w_mk, w_mv):
    B, M, _ = mem.shape
    return (mem @ w_mk).reshape(B, M, X_HEADS, X_DH), (mem @ w_mv).reshape(B, M, X_HEADS, X_DH)


def cross_attention(h, mem_k, mem_v, w_xq, w_xo):
    B, L, _ = h.shape
    q = (h @ w_xq).reshape(B, L, X_HEADS, X_DH)
    s = jnp.einsum('blhd,bmhd->bhlm', q, mem_k, preferred_element_type=jnp.float32) * (X_DH ** -0.5)
    p = jax.nn.softmax(s, axis=-1).astype(mem_v.dtype)
    o = jnp.einsum('bhlm,bmhd->blhd', p, mem_v).reshape(B, L, X_HEADS * X_DH)
    return o @ w_xo


def hier_moe(h, w_grp, b_grp, w_rtr, b_rtr, w_e_gate, w_e_up, w_e_down):
    B, L, D = h.shape
    t = h.reshape(B * L, D)
    T = t.shape[0]
    grp_logits = jnp.einsum('td,dg->tg', t, w_grp, preferred_element_type=jnp.float32)
    grp_prob = jax.nn.softmax(grp_logits, axis=-1)
    g_idx = jnp.argmax(grp_logits + b_grp, axis=-1)
    p_g = jnp.take_along_axis(grp_prob, g_idx[:, None], axis=1)
    e_logits = jnp.einsum('td,de->te', t, w_rtr, preferred_element_type=jnp.float32)
    e_logits = e_logits.reshape(T, N_GROUPS, EXPERTS_PER_GROUP)
    in_grp = jnp.take_along_axis(e_logits, g_idx[:, None, None], axis=1)[:, 0]
    in_bias = b_rtr.reshape(N_GROUPS, EXPERTS_PER_GROUP)[g_idx]
    _, top_i = lax.top_k(in_grp + in_bias, TOP_K_IN_GROUP)
    w_sel = jax.nn.softmax(jnp.take_along_axis(in_grp, top_i, axis=1), axis=-1)
    expert_ids = g_idx[:, None] * EXPERTS_PER_GROUP + top_i
    gates = jnp.sum(jax.nn.one_hot(expert_ids, N_EXPERTS, dtype=jnp.float32)
                    * (p_g * w_sel)[..., None], axis=1)
    y = jnp.zeros((T, D), jnp.float32)
    for e in range(N_EXPERTS):
        he = jax.nn.silu(t @ w_e_gate[e]) * (t @ w_e_up[e])
        y = y + gates[:, e:e + 1] * (he @ w_e_down[e])
    return y.astype(h.dtype).reshape(B, L, D)


def setup_inputs(seed: int = 0) -> dict:
    key = jax.random.key(seed)
    keys = jax.random.split(key, 48)
    idx = iter(range(48))
    def nrm(shape, scale):
        return jax.random.normal(keys[next(idx)], shape, jnp.float32) * scale
    def gain(shape):
        return 1.0 + nrm(shape, 0.02)
    n_pages = PAST_LEN // PAGE_SIZE
    used = DEC_BATCH * n_pages
    n_phys = used + used // 4
    Ld = DEPTH
    return {
        'x_prompt': nrm((BATCH, SEQ, D_MODEL), 1.0),
        'x_sample': nrm((DEC_BATCH, DEC_SEQ, D_MODEL), 1.0),
        'mem_prompt': nrm((BATCH, MEM_TOKENS, D_MODEL), 1.0),
        'state_gla': nrm((Ld, DEC_BATCH, GLA_HEADS, GLA_DK, GLA_DV), 1.0),
        'cache_latent': nrm((Ld, n_phys, PAGE_SIZE, MLA_KV_RANK), 1.0),
        'cache_krope': nrm((Ld, n_phys, PAGE_SIZE, MLA_D_ROPE), 1.0),
        'cache_mem_k': nrm((Ld, DEC_BATCH, MEM_TOKENS, X_HEADS, X_DH), 1.0),
        'cache_mem_v': nrm((Ld, DEC_BATCH, MEM_TOKENS, X_HEADS, X_DH), 1.0),
        'page_table': jax.random.permutation(keys[next(idx)], n_phys)[:used].reshape(DEC_BATCH, n_pages).astype(jnp.int32),
        'w_in': nrm((Ld, D_MODEL, D_IN_PROJ), D_MODEL ** -0.5),
        'w_gla_gate': nrm((Ld, GLA_GATE_RANK, GLA_HEADS * GLA_DK), GLA_GATE_RANK ** -0.5),
        'b_gla_gate': nrm((Ld, GLA_HEADS * GLA_DK), 0.1),
        'gla_norm_g': gain((Ld, GLA_DV)),
        'mla_q_norm_g': gain((Ld, MLA_Q_RANK)),
        'w_uq': nrm((Ld, MLA_Q_RANK, MLA_HEADS, MLA_D_NOPE + MLA_D_ROPE), MLA_Q_RANK ** -0.5),
        'mla_kv_norm_g': gain((Ld, MLA_KV_RANK)),
        'w_uk': nrm((Ld, MLA_KV_RANK, MLA_HEADS, MLA_D_NOPE), MLA_KV_RANK ** -0.5),
        'w_uv': nrm((Ld, MLA_KV_RANK, MLA_HEADS, MLA_DV), MLA_KV_RANK ** -0.5),
        'w_out': nrm((Ld, D_MIX, D_MODEL), BETA * D_MIX ** -0.5),
        'ln1_g': gain((Ld, D_MODEL)),
        'ln1_b': nrm((Ld, D_MODEL), 0.02),
        'w_xq': nrm((Ld, D_MODEL, X_HEADS * X_DH), D_MODEL ** -0.5),
        'w_mk': nrm((Ld, D_MODEL, X_HEADS * X_DH), D_MODEL ** -0.5),
        'w_mv': nrm((Ld, D_MODEL, X_HEADS * X_DH), D_MODEL ** -0.5),
        'w_xo': nrm((Ld, X_HEADS * X_DH, D_MODEL), BETA * (X_HEADS * X_DH) ** -0.5),
        'ln2_g': gain((Ld, D_MODEL)),
        'ln2_b': nrm((Ld, D_MODEL), 0.02),
        'w_grp': nrm((Ld, D_MODEL, N_GROUPS), D_MODEL ** -0.5),
        'b_grp': nrm((Ld, N_GROUPS), 0.01),
        'w_rtr': nrm((Ld, D_MODEL, N_EXPERTS), D_MODEL ** -0.5),
        'b_rtr': nrm((Ld, N_EXPERTS), 0.01),
        'w_e_gate': nrm((Ld, N_EXPERTS, D_MODEL, D_EXPERT), D_MODEL ** -0.5),
        'w_e_up': nrm((Ld, N_EXPERTS, D_MODEL, D_EXPERT), D_MODEL ** -0.5),
        'w_e_down': nrm((Ld, N_EXPERTS, D_EXPERT, D_MODEL), BETA * D_EXPERT ** -0.5),
        'ln3_g': gain((Ld, D_MODEL)),
        'ln3_b': nrm((Ld, D_MODEL), 0.02),
    }


def reference(x_prompt, x_sample, mem_prompt, state_gla, cache_latent, cache_krope, cache_mem_k,
              cache_mem_v, page_table, w_in, w_gla_gate, b_gla_gate, gla_norm_g, mla_q_norm_g, w_uq,
              mla_kv_norm_g, w_uk, w_uv, w_out, ln1_g, ln1_b, w_xq, w_mk, w_mv, w_xo, ln2_g, ln2_b,
              w_grp, b_grp, w_rtr, b_rtr, w_e_gate, w_e_up, w_e_down, ln3_g, ln3_b):
    Bp, Lp, _ = x_prompt.shape
    Bd, Ld, _ = x_sample.shape
    past_len = page_table.shape[1] * cache_latent.shape[2]
    pos_p = jnp.arange(Lp)
    pos_d = past_len + jnp.arange(Ld)
    hp, hd = x_prompt, x_sample
    sp_list, cp_list, kp_list, mk_list, mv_list = [], [], [], [], []
    sd_list, cd_list, kd_list = [], [], []
    for l in range(DEPTH):
        mix_w = (w_in[l], w_gla_gate[l], b_gla_gate[l], gla_norm_g[l], mla_q_norm_g[l], w_uq[l],
                 mla_kv_norm_g[l], w_uk[l], w_uv[l], w_out[l])
        moe_w = (w_grp[l], b_grp[l], w_rtr[l], b_rtr[l], w_e_gate[l], w_e_up[l], w_e_down[l])
        s0 = jnp.zeros((Bp, GLA_HEADS, GLA_DK, GLA_DV), x_prompt.dtype)
        mix_p, s_p, c_p, kpe_p = mixer(hp, pos_p, s0, mla_prompt_attention, *mix_w)
        hp = layer_norm(ALPHA * hp + mix_p, ln1_g[l], ln1_b[l])
        mk_p, mv_p = memory_kv(mem_prompt, w_mk[l], w_mv[l])
        hp = layer_norm(ALPHA * hp + cross_attention(hp, mk_p, mv_p, w_xq[l], w_xo[l]), ln2_g[l], ln2_b[l])
        hp = layer_norm(ALPHA * hp + hier_moe(hp, *moe_w), ln3_g[l], ln3_b[l])
        past_c = cache_latent[l][page_table].reshape(Bd, past_len, MLA_KV_RANK)
        past_kpe = cache_krope[l][page_table].reshape(Bd, past_len, MLA_D_ROPE)
        attend_d = functools.partial(mla_sample_attention, past_c=past_c, past_kpe=past_kpe)
        mix_d, s_d, c_d, kpe_d = mixer(hd, pos_d, state_gla[l], attend_d, *mix_w)
        hd = layer_norm(ALPHA * hd + mix_d, ln1_g[l], ln1_b[l])
        hd = layer_norm(ALPHA * hd + cross_attention(hd, cache_mem_k[l], cache_mem_v[l], w_xq[l], w_xo[l]),
                        ln2_g[l], ln2_b[l])
        hd = layer_norm(ALPHA * hd + hier_moe(hd, *moe_w), ln3_g[l], ln3_b[l])
        sp_list.append(s_p); cp_list.append(c_p); kp_list.append(kpe_p)
        mk_list.append(mk_p); mv_list.append(mv_p)
        sd_list.append(s_d); cd_list.append(c_d); kd_list.append(kpe_d)
    return (hp, hd, jnp.stack(sp_list), jnp.stack(cp_list), jnp.stack(kp_list), jnp.stack(mk_list),
            jnp.stack(mv_list), jnp.stack(sd_list), jnp.stack(cd_list), jnp.stack(kd_list))
```

```python
import numpy as np
from contextlib import ExitStack
import concourse.bass as bass
import concourse.mybir as mybir
from concourse.bass_utils import run_bass_kernel_spmd

F32 = mybir.dt.float32
BF16 = mybir.dt.bfloat16
I32 = mybir.dt.int32
AF = mybir.ActivationFunctionType
ALU = mybir.AluOpType
AX = mybir.AxisListType

NCORES = 8
D = 1024
SEQ = 2048
NSEQ = 2
NS = 16
NPAGE = 64
ALPHA = 2.0 ** 0.25
LN_EPS = 1e-5
RMS_EPS = 1e-6
MLA_SCALE = 96.0 ** -0.5


class _Op:
    __slots__ = ("idx", "eng", "fn", "deps", "dma", "signal", "sem", "val", "prev")

    def __init__(self, idx, eng, fn, dma):
        self.idx, self.eng, self.fn, self.dma = idx, eng, fn, dma
        self.deps = set()
        self.signal = False
        self.sem = None
        self.val = 0
        self.prev = 0


class Prog:
    ENGS = ("pe", "act", "dve", "pool", "sp")
    NDS = 48

    def __init__(self):
        self.ops = []
        self.lastw = {}
        self.readers = {}

    def add(self, eng, fn, r=(), w=(), dma=False):
        op = _Op(len(self.ops), eng, fn, dma)
        xs = [k for k in r if isinstance(k, tuple) and k and k[0] == "ps"]
        if xs:
            r = [k for k in r if k not in xs]
            w = list(w) + [k for k in xs if k not in w]
        for k in r:
            lw = self.lastw.get(k)
            if lw is not None:
                op.deps.add(lw)
        for k in w:
            lw = self.lastw.get(k)
            if lw is not None:
                op.deps.add(lw)
            rs = self.readers.get(k)
            if rs:
                op.deps.update(rs)
        for k in r:
            self.readers.setdefault(k, []).append(op.idx)
        for k in w:
            self.lastw[k] = op.idx
            self.readers[k] = []
        op.deps.discard(op.idx)
        self.ops.append(op)
        return op

    def emit(self, nc, es):
        ops = self.ops
        esem = {e: es.enter_context(nc.semaphore("se_" + e)) for e in self.ENGS}
        dsem = [es.enter_context(nc.semaphore("sd_%d" % i)) for i in range(self.NDS)]
        qrange = {"sp": (0, self.NDS - 12), "act": (0, self.NDS - 12), "pool": (self.NDS - 12, self.NDS)}
        qnext = {q: lo for q, (lo, hi) in qrange.items()}

        def pruned(op, dop):
            return (not op.dma) and (not dop.dma) and op.eng == "pe" and dop.eng == "pe"

        for op in ops:
            for d in op.deps:
                if not pruned(op, ops[d]):
                    ops[d].signal = True
        cnt = {e: 0 for e in self.ENGS}
        dcnt = [0] * self.NDS
        dn = 0
        for op in ops:
            if op.dma:
                qk = "pool" if op.eng == "pool" else "sp"
                dn = qnext[qk]
                lo, hi = qrange[qk]
                qnext[qk] = lo + (dn + 1 - lo) % (hi - lo)
                op.sem = dsem[dn]
                op.prev = dcnt[dn]
                dcnt[dn] += 16
                op.val = dcnt[dn]
            elif op.signal:
                cnt[op.eng] += 1
                op.sem = esem[op.eng]
                op.val = cnt[op.eng]
        by = {e: [op for op in ops if op.eng == e] for e in self.ENGS}

        def body_for(e):
            def body(engh):
                waited = {}
                for op in by[e]:
                    needs = {}
                    for d in op.deps:
                        dop = ops[d]
                        if pruned(op, dop):
                            continue
                        key = id(dop.sem)
                        if key not in needs or needs[key][1] < dop.val:
                            needs[key] = (dop.sem, dop.val)
                    if op.dma and op.prev > 0:
                        key = id(op.sem)
                        if key not in needs or needs[key][1] < op.prev:
                            needs[key] = (op.sem, op.prev)
                    for key, (sem, val) in needs.items():
                        if waited.get(key, 0) < val:
                            engh.wait_ge(sem, val)
                            waited[key] = val
                    ins = op.fn(engh)
                    if op.dma:
                        ins.then_inc(op.sem, 16)
                    elif op.signal:
                        ins.then_inc(op.sem, 1)
                if e == "sp":
                    for i in range(self.NDS):
                        if dcnt[i] > 0:
                            engh.wait_ge(dsem[i], dcnt[i])
            return body

        with nc.Block() as block:
            block.tensor(body_for("pe"))
            block.scalar(body_for("act"))
            block.vector(body_for("dve"))
            block.gpsimd(body_for("pool"))
            block.sync(body_for("sp"))


class Builder:
    def __init__(self, stages):
        self.stages = stages
        self.nc = bass.Bass("TRN2", target_bir_lowering=False)
        self.es = ExitStack()
        self.P = Prog()
        self.psn = 0
        self.bank_lim = 8
        self.ptn = 0
        self.ev = 0

    def din(self, name, shape, dt=F32):
        return self.nc.dram_tensor(name, list(shape), dt, kind="ExternalInput").ap()

    def dout(self, name, shape, dt=F32):
        return self.nc.dram_tensor(name, list(shape), dt, kind="ExternalOutput").ap()

    def sb(self, name, shape, dt):
        return self.es.enter_context(self.nc.sbuf_tensor("sb_" + name, list(shape), dt))

    def next_bank(self):
        b = self.psn % self.bank_lim
        self.psn = (b + 1) % self.bank_lim
        return b

    def next_tbank(self):
        b = self.ptn
        self.ptn = (self.ptn + 1) % 2
        return b

    def dma(self, out, in_, r=(), w=(), q="sp"):
        self.P.add(q, lambda e: e.dma_start(out=out, in_=in_), r=r, w=w, dma=True)

    def mm(self, out, lhsT, rhs, start, stop, r=(), w=()):
        self.P.add("pe", lambda e: e.matmul(out, lhsT, rhs, start=start, stop=stop), r=r, w=w)

    def tr(self, out, in_, ident, r=(), w=()):
        self.P.add("pe", lambda e: e.transpose(out, in_, ident), r=r, w=w)

    def act(self, out, in_, func, r=(), w=(), **kw):
        self.P.add("act", lambda e: e.activation(out, in_, func, **kw), r=r, w=w)

    def copy(self, out, in_, r=(), w=(), eng=None):
        if eng is None:
            eng = ("dve", "act")[self.ev % 2]
            self.ev += 1
        if eng == "act":
            self.P.add("act", lambda e: e.copy(out, in_), r=r, w=w)
        else:
            self.P.add(eng, lambda e: e.tensor_copy(out, in_), r=r, w=w)

    def tt(self, out, in0, in1, op, r=(), w=(), eng="dve"):
        self.P.add(eng, lambda e: e.tensor_tensor(out, in0, in1, op), r=r, w=w)

    def ts(self, out, in0, s1, s2, op0, op1=ALU.bypass, r=(), w=(), eng="dve"):
        if s2 is None:
            self.P.add(eng, lambda e: e.tensor_scalar(out, in0, s1, None, op0), r=r, w=w)
        else:
            self.P.add(eng, lambda e: e.tensor_scalar(out, in0, s1, s2, op0, op1), r=r, w=w)

    def stt(self, out, in0, scalar, in1, op0, op1, r=(), w=()):
        self.P.add("dve", lambda e: e.scalar_tensor_tensor(out, in0, scalar, in1, op0, op1), r=r, w=w)

    def rsqrt(self, out, in_, c, tmp, r=(), w=(), tk="rsq_tmp"):
        self.P.add("act", lambda e: e.activation(tmp, in_, AF.Sqrt, bias=float(c)), r=r, w=[tk])
        self.P.add("dve", lambda e: e.reciprocal(out, tmp), r=[tk], w=w)

    def red(self, out, in_, op, r=(), w=()):
        self.P.add("dve", lambda e: e.tensor_reduce(out, in_, AX.X, op), r=r, w=w)


def _bc(ap, n):
    return bass.AP(ap.tensor, ap.offset, [[0, 128], [1, n]])


class AV:
    CH = 2048

    def __init__(self, arena, off, shape):
        n = int(np.prod(shape))
        a = arena[:, off:off + n]
        if len(shape) == 2:
            a = a.rearrange("p (a b) -> p a b", b=shape[1])
        self.ap = a
        self.keys = [("W", c) for c in range(off // self.CH, (off + n - 1) // self.CH + 1)]


C_Q, C_K, C_V, C_A, C_R, C_CQ, C_CKV, C_KR = 0, 256, 512, 1024, 1040, 1552, 1936, 2192


def build(stages=("memkv", "A")):
    B = Builder(stages)
    nc, P = B.nc, B.P
    NT = NSEQ * SEQ

    xp = B.din("xp", [NT, D])
    memp = B.din("memp", [NSEQ * 256, D])
    ident_d = B.din("ident", [128, 128])
    us_d = B.din("c_us", [128, 128])
    lw_d = B.din("c_lw", [128, 128])
    mask_d = B.din("c_mask", [128, 128])
    cos_d = B.din("c_cos", [128, 16, 16])
    sin_d = B.din("c_sin", [128, 16, 16])
    w_mk = B.din("w_mk", [D, D])
    w_mv = B.din("w_mv", [D, D])
    w_in = B.din("w_in", [D, 2224])
    wgate_d = B.din("w_gate_aug", [17, 256])
    ggla_d = B.din("gla_norm_g", [1, 128])
    gq_d = B.din("mla_q_norm_g", [1, 384])
    gkv_d = B.din("mla_kv_norm_g", [1, 256])
    w_uq = B.din("w_uq", [384, 768])
    w_uk = B.din("w_uk", [256, 512])
    w_uv = B.din("w_uv", [256, 512])
    w_out = B.din("w_out", [D, D])
    w_xq = B.din("w_xq", [D, D])
    w_xo = B.din("w_xo", [D, D])
    ln_d = [B.din("ln%d" % i, [1, D]) for i in range(6)]
    sc_h2 = nc.dram_tensor("sc_h2", [NT + NS, D], F32).ap()
    w_rt = B.din("w_rt", [D, 36])
    b_rt = B.din("b_rt", [1, 36])
    w_eg = B.din("w_e_gate", [32, D, 256])
    w_eu = B.din("w_e_up", [32, D, 256])
    w_ed = B.din("w_e_down", [32, 256, D])
    xs = B.din("xs", [NS, D])
    sgla_s = B.din("sgla_s", [NS, 4, 64, 128])
    NPHYS = 10240
    c_lat = B.din("cache_lat", [NPHYS * 128, 256])
    c_kr = B.din("cache_kr", [NPHYS * 128, 32])
    cmk = B.din("cmk", [NS, 256, D])
    cmv = B.din("cmv", [NS, 256, D])
    pt_d = B.din("pt", [1, NS * NPAGE], I32)
    pcol_d = B.din("c_pcol", [128, 1])
    i16_d = B.din("c_i16", [64, 256])
    coss_d = B.din("c_cos_s", [NS, 16])
    sins_d = B.din("c_sin_s", [NS, 16])
    bm8_d = B.din("c_bm8", [8, 512])
    bm4_d = B.din("c_bm4", [4, 1024])
    o_sgla_s = B.dout("o_sgla_s", [NS, 4, 64, 128])
    o_lat_s = B.dout("o_lat_s", [NS, 256])
    o_kr_s = B.dout("o_kr_s", [NS, 32])
    o_y = B.dout("o_y", [NT, D])
    o_ys = B.dout("o_ys", [NS, D])
    o_memk = B.dout("o_memk", [NSEQ * 256, D])
    o_memv = B.dout("o_memv", [NSEQ * 256, D])
    o_lat = B.dout("o_lat", [NT, 256])
    o_kr = B.dout("o_kr", [NT, 32])
    o_sgla = B.dout("o_sgla", [NSEQ, 4, 64, 128])
    sc_q = nc.dram_tensor("sc_q", [NT, 768], BF16).ap()
    sc_go = nc.dram_tensor("sc_go", [NT, 512], BF16).ap()

    psum = [B.es.enter_context(nc.psum_tensor("ps%d" % i, [128, 512], F32)) for i in range(8)]
    ident = B.sb("ident", [128, 128], F32)
    identb = B.sb("identb", [128, 128], BF16)
    c_us = B.sb("c_us", [128, 128], F32)
    c_lw = B.sb("c_lw", [128, 128], F32)
    c_mask = B.sb("c_mask", [128, 128], F32)
    cosT = B.sb("cosT", [128, 16, 16], F32)
    sinT = B.sb("sinT", [128, 16, 16], F32)
    arena = B.sb("arena", [128, 24576], BF16)
    WmkA = AV(arena, 0, [8, 1024])
    WmvA = AV(arena, 8192, [8, 1024])
    wA, wB = WmkA.ap, WmvA.ap
    xin = [B.sb("xin%d" % i, [128, D], F32) for i in range(2)]
    xT = [B.sb("xT%d" % i, [128, 8, 128], BF16) for i in range(2)]
    stage = [B.sb("stage%d" % i, [128, D], F32) for i in range(2)]
    memT = B.sb("memT", [128, 8, 256], BF16)
    memkT = B.sb("memkT", [128, NSEQ, 8, 256], BF16)
    memv = B.sb("memv", [128, NSEQ, 2, 4, 260], BF16)
    arenaS = B.sb("arenaS", [128, 12800], F32)

    def sview(off, n, dt=F32, parts=128):
        a = arenaS[0:parts, off:off + n]
        if dt != F32:
            a = a.bitcast(dt)
        return a, [("S", c) for c in range(off // 1024, (off + n - 1) // 1024 + 1)]
    kT_flat, kT_keys = sview(0, 8192, BF16, 96)
    kT_seq = kT_flat.rearrange("p (h t) -> p h t", t=SEQ)
    V_flat, V_keys = sview(8192, 4224, BF16)
    V_seq = V_flat.rearrange("p (a h d) -> p a h d", h=8, d=66)
    Sst = B.sb("Sst", [64, 4, 128], F32)
    Sbf = B.sb("Sbf", [64, 4, 128], BF16)
    wgate = B.sb("wgate", [17, 256], F32)
    ggla = B.sb("ggla", [128, 128], F32)
    gq = B.sb("gq", [128, 384], F32)
    gkv = B.sb("gkv", [128, 256], F32)
    aT = B.sb("aT", [32, 128], F32)
    zqk = B.sb("zqk", [128, 512], F32)
    vbf = B.sb("vbf", [128, 512], BF16)
    sr = B.sb("sr", [128, 512], F32)
    cq = B.sb("cq", [128, 384], F32)
    ze = B.sb("ze", [128, 288], F32)
    gtmp = B.sb("gtmp", [128, 5, 256], F32)
    e1, l1, eb, enb, ekb = [gtmp[:, k_, :] for k_ in range(5)]
    eblT = B.sb("eblT", [64, 4], F32)
    qt = B.sb("qt", [128, 256], BF16)
    kt = B.sb("kt", [128, 256], BF16)
    kp = B.sb("kp", [128, 256], BF16)
    qkT = B.sb("qkT", [64, 8, 128], BF16)
    ATm = B.sb("ATm", [128, 4, 128], BF16)
    osb = B.sb("osb", [128, 4, 128], F32)
    junk = B.sb("junk", [128, 512], F32)
    ss4 = B.sb("ss4", [128, 4], F32)
    rs4 = B.sb("rs4", [128, 4], F32)
    big1 = B.sb("big1", [128, 1024], F32)
    t1 = big1[:, 0:512].rearrange("p (a b) -> p a b", b=128)
    SG = big1[:, 512:1024].rearrange("p (a b) -> p a b", b=128)
    gobf = B.sb("gobf", [128, 512], BF16)
    rsq = B.sb("rsq", [128, 8], F32)
    sskv = B.sb("sskv", [128, 2], F32)
    rskv = B.sb("rskv", [128, 2], F32)
    ckvn = B.sb("ckvn", [128, 256], F32)
    ckvT = B.sb("ckvT", [128, 2, 128], BF16)
    cqn = B.sb("cqn", [128, 384], F32)
    cqnT = B.sb("cqnT", [128, 3, 128], BF16)
    qtm = B.sb("qtm", [128, 8, 96], F32)
    qbf = B.sb("qbf", [128, 768], BF16)
    kpe = B.sb("kpe", [128, 96], F32)
    rp = [B.sb("rp%d" % i, [128, 8, 16], F32) for i in range(4)]

    lnp = [B.sb("lnp%d" % i, [128, D], F32) for i in range(4)]
    maskb = B.sb("maskb", [128, 128], BF16)
    rec8 = B.sb("rec8", [128, 8], F32)
    lnst = B.sb("lnst", [128, 2, 6], F32)
    lnmv = B.sb("lnmv", [128, 2], F32)
    lnrs = B.sb("lnrs", [128, 2], F32)
    pcol = B.sb("pcol", [128, 1], F32)
    i16 = B.sb("i16", [64, 16, 16], F32)
    cos_s = B.sb("cos_s", [NS, 16], F32)
    sin_s = B.sb("sin_s", [NS, 16], F32)
    bm8 = B.sb("bm8", [8, 512], F32)
    bm4 = memkT[:].rearrange("p a b c -> p (a b c)")[0:4, 0:2048].bitcast(F32)
    ones16 = B.sb("ones16", [16, 128], F32)
    rt = gtmp[:, 0:2, :].rearrange("p a (b c) -> p (a b) c", c=32)
    wrt = gtmp[:, 2:4, :].rearrange("p a b -> p (a b)")[:, 0:288].rearrange("p (k n) -> p k n", n=36)
    gates = gtmp[:, 4, 0:128].rearrange("p (t e) -> p t e", e=32)
    brt = gtmp[:, 4, 128:164]
    B.dma(ident[:], ident_d, w=["ident"])
    B.dma(c_us[:], us_d, w=["c_us"])
    B.dma(c_lw[:], lw_d, w=["c_lw"])
    B.dma(c_mask[:], mask_d, w=["c_mask"])
    B.dma(cosT[:], cos_d, w=["cosT"])
    B.dma(sinT[:], sin_d, w=["sinT"])
    B.dma(wgate[:], wgate_d, w=["wgate"])
    B.dma(ggla[:], _bc(ggla_d, 128), w=["ggla"])
    B.dma(gq[:], _bc(gq_d, 384), w=["gq"])
    B.dma(gkv[:], _bc(gkv_d, 256), w=["gkv"])
    B.dma(pcol[:], pcol_d, w=["pcol"])
    B.dma(i16[:].rearrange("p a b -> p (a b)"), i16_d, w=["i16"])
    B.dma(cos_s[:], coss_d, w=["cos_s"])
    B.dma(sin_s[:], sins_d, w=["sin_s"])
    B.dma(bm8[:], bm8_d, w=["bm8"])
    P.add("dve", lambda e: e.memset(ones16[:], 1.0), w=["ones16"])
    B.copy(identb[:], ident[:], r=["ident"], w=["identb"], eng="dve")
    B.copy(maskb[:], c_mask[:], r=["c_mask"], w=["maskb"], eng="dve")
    for i in range(4):
        B.dma(lnp[i][:], _bc(ln_d[i], D), w=[("lnp", i)])
    P.add("act", lambda e: e.mul(ggla[:], ggla[:], float(128.0 ** 0.5)), r=["ggla"], w=["ggla"])
    P.add("act", lambda e: e.mul(gq[:], gq[:], float(384.0 ** 0.5)), r=["gq"], w=["gq"])
    P.add("act", lambda e: e.mul(gkv[:], gkv[:], float(256.0 ** 0.5)), r=["gkv"], w=["gkv"])
    P.add("dve", lambda e: e.memset(aT[:], 1.0), w=["aT"])
    P.add("dve", lambda e: e.memset(kpe[:], 0.0), w=["kpe"])

    def load_w(dst, src, keys, q="pool", kcn=8):
        v = src.rearrange("(kc p) n -> p kc n", p=128)
        for kc0 in range(0, kcn, 4):
            kc1 = min(kcn, kc0 + 4)
            B.dma(dst[:, kc0:kc1, :], v[:, kc0:kc1, :], w=keys, q=q)

    cnt = {"x": 0, "st": 0}

    def transpose_tile(src_tile, src_key, dstT, dst_key, ncols, tok0=0, ntok=128):
        nchunk = ncols // 128
        for g0 in range(0, nchunk, 4):
            g1 = min(nchunk, g0 + 4)
            b = B.next_bank()
            for c in range(g0, g1):
                B.tr(psum[b][:, (c - g0) * 128:(c - g0) * 128 + ntok],
                     src_tile[:ntok, c * 128:(c + 1) * 128], ident[:ntok, :ntok],
                     r=[src_key, "ident"], w=[("ps", b)])
            pv = psum[b][:, 0:(g1 - g0) * 128].rearrange("p (c t) -> p c t", t=128)
            B.copy(dstT[:, g0:g1, tok0:tok0 + ntok], pv[:, :, :ntok], r=[("ps", b)], w=[dst_key])

    if "memkv" in stages:
        P.add("dve", lambda e: e.memset(memv[:].rearrange("p a b c d -> p (a b c d)"), 1.0), w=["memv"])
        load_w(wA, w_mk, WmkA.keys)
        load_w(wB, w_mv, WmvA.keys)
        for s in range(NSEQ):
            for mt in range(2):
                i = cnt["x"] % 2
                cnt["x"] += 1
                B.dma(xin[i][:], memp[s * 256 + mt * 128: s * 256 + (mt + 1) * 128, :], w=[("xin", i)])
                transpose_tile(xin[i], ("xin", i), memT, "memT", D, tok0=mt * 128)
            for wi, (wt, wkey, odram) in enumerate(((wA, WmkA.keys, o_memk), (wB, WmvA.keys, o_memv))):
                for mt in range(2):
                    si = cnt["st"] % 2
                    cnt["st"] += 1
                    for half in range(2):
                        b = B.next_bank()
                        for kc in range(8):
                            B.mm(psum[b][:, :], memT[:, kc, mt * 128:(mt + 1) * 128],
                                 wt[:, kc, half * 512:(half + 1) * 512], kc == 0, kc == 7,
                                 r=["memT"] + wkey, w=[("ps", b)])
                        B.copy(stage[si][:, half * 512:(half + 1) * 512], psum[b][:, :],
                               r=[("ps", b)], w=[("stage", si)])
                        if wi == 1:
                            B.copy(memv[:, s, mt, half * 2:half * 2 + 2, 0:256],
                                   psum[b][:, :].rearrange("p (h d) -> p h d", d=256),
                                   r=[("ps", b)], w=["memv"])
                    B.dma(odram[s * 256 + mt * 128: s * 256 + (mt + 1) * 128, :], stage[si][:],
                          r=[("stage", si)], w=[("o", wi, s, mt)])
            for j in range(8):
                b = B.next_bank()
                for kc in range(8):
                    B.mm(psum[b][:, 0:256], wA[:, kc, j * 128:(j + 1) * 128], memT[:, kc, :],
                         kc == 0, kc == 7, r=["memT"] + WmkA.keys, w=[("ps", b)])
                B.copy(memkT[:, s, j, :], psum[b][:, 0:256], r=[("ps", b)], w=["memkT"])

    Wqk = AV(arena, 0, [8, 512])
    Wv = AV(arena, 4096, [8, 512])
    Wr = AV(arena, 8192, [8, 512])
    Wcq = AV(arena, 12288, [8, 384])
    We = AV(arena, 15360, [8, 288])
    Wa = AV(arena, 17664, [8, 16])
    Wuq = AV(arena, 17792, [3, 768])
    Wuk = AV(arena, 20096, [2, 512])
    Wuv = AV(arena, 21120, [2, 512])

    def bc_h(ap2d, nh):
        return ap2d.unsqueeze(1).to_broadcast([ap2d.shape[0], nh, ap2d.shape[1]])

    def rope(x1, x2, o1, o2, cs, sn, nh, rk, wk, ntok=128, ck="cosT", sk_="sinT"):
        c3, s3 = bc_h(cs, nh), bc_h(sn, nh)
        t = [rp[i][:ntok, 0:nh, :] for i in range(4)]
        B.tt(t[0], x1, c3, ALU.mult, r=rk + [ck], w=["rp0"])
        B.tt(t[1], x2, s3, ALU.mult, r=rk + [sk_], w=["rp1"])
        B.tt(t[2], x1, s3, ALU.mult, r=rk + [sk_], w=["rp2"])
        B.tt(t[3], x2, c3, ALU.mult, r=rk + [ck], w=["rp3"])
        B.tt(o1, t[0], t[1], ALU.subtract, r=["rp0", "rp1"], w=wk)
        B.tt(o2, t[2], t[3], ALU.add, r=["rp2", "rp3"], w=wk)

    def load_A_weights():
        wi = w_in
        for (av, c0, n) in ((Wqk, 0, 512), (Wv, C_V, 512), (Wr, C_R, 512), (Wcq, C_CQ, 384),
                            (We, C_CKV, 288), (Wa, C_A, 16)):
            load_w(av.ap, wi[:, c0:c0 + n], av.keys)
        load_w(Wuq.ap, w_uq, Wuq.keys, kcn=3)
        load_w(Wuk.ap, w_uk, Wuk.keys, kcn=2)
        load_w(Wuv.ap, w_uv, Wuv.keys, kcn=2)

    def phaseA(s):
        load_A_weights()
        P.add("dve", lambda e: e.memset(Sst[:].rearrange("p a b -> p (a b)"), 0.0), w=["Sst"])
        P.add("dve", lambda e: e.memset(Sbf[:].rearrange("p a b -> p (a b)"), 0.0), w=["Sbf"])
        P.add("dve", lambda e: e.memset(V_flat, 1.0), w=V_keys)
        for t in range(16):
            tok0 = s * SEQ + t * 128
            tsl = slice(t * 128, (t + 1) * 128)
            i = cnt["x"] % 2
            cnt["x"] += 1
            xk, xtk = ("xin", i), ("xT", i)
            B.dma(xin[i][:], xp[tok0:tok0 + 128, :], w=[xk])
            transpose_tile(xin[i], xk, xT[i], xtk, D)

            def zgroup(av, n):
                b = B.next_bank()
                for kc in range(8):
                    B.mm(psum[b][:, 0:n], xT[i][:, kc, :], av.ap[:, kc, :], kc == 0, kc == 7,
                         r=[xtk] + av.keys, w=[("ps", b)])
                return b
            b = zgroup(Wqk, 512)
            B.copy(zqk[:], psum[b][:, :], r=[("ps", b)], w=["zqk"])
            b = zgroup(Wv, 512)
            B.copy(vbf[:], psum[b][:, :], r=[("ps", b)], w=["vbf"])
            b = zgroup(Wr, 512)
            B.act(sr[:], psum[b][:, :], AF.Silu, r=[("ps", b)], w=["sr"])
            b = zgroup(Wcq, 384)
            B.copy(cq[:], psum[b][:, 0:384], r=[("ps", b)], w=["cq"])
            b = zgroup(We, 288)
            B.copy(ze[:], psum[b][:, 0:288], r=[("ps", b)], w=["ze"])
            b = B.next_bank()
            for kc in range(8):
                B.mm(psum[b][0:16, 0:128], Wa.ap[:, kc, :], xT[i][:, kc, :], kc == 0, kc == 7,
                     r=[xtk] + Wa.keys, w=[("ps", b)])
            B.copy(aT[0:16, :], psum[b][0:16, 0:128], r=[("ps", b)], w=["aT"])
            b = B.next_bank()
            B.mm(psum[b][:, 0:256], aT[0:17, :], wgate[0:17, :], True, True, r=["aT", "wgate"], w=[("ps", b)])
            B.act(e1[:], psum[b][:, 0:256], AF.Exp, r=[("ps", b)], w=["e1"], scale=-1.0)
            B.act(l1[:], e1[:], AF.Ln, r=["e1"], w=["l1"], bias=1.0)
            b = B.next_bank()
            B.mm(psum[b][:, 0:256], c_us[:], l1[:], True, True, r=["c_us", "l1"], w=[("ps", b)])
            B.act(eb[:], psum[b][:, 0:256], AF.Exp, r=[("ps", b)], w=["eb"])
            B.act(enb[:], psum[b][:, 0:256], AF.Exp, r=[("ps", b)], w=["enb"], scale=-1.0)
            b = B.next_bank()
            B.mm(psum[b][:, 0:256], c_lw[:], l1[:], True, True, r=["c_lw", "l1"], w=[("ps", b)])
            B.act(ekb[:], psum[b][:, 0:256], AF.Exp, r=[("ps", b)], w=["ekb"])
            b = B.next_bank()
            for h in range(4):
                B.mm(psum[b][0:64, h:h + 1], l1[:, h * 64:(h + 1) * 64], c_us[:, 127:128], True, True,
                     r=["c_us", "l1"], w=[("ps", b)])
            B.act(eblT[:], psum[b][0:64, 0:4], AF.Exp, r=[("ps", b)], w=["eblT"])
            B.stt(qt[:], zqk[:, 0:256], 0.125, eb[:], ALU.mult, ALU.mult, r=["zqk", "eb"], w=["qt"])
            B.tt(kt[:], zqk[:, 256:512], enb[:], ALU.mult, r=["zqk", "enb"], w=["kt"])
            B.tt(kp[:], zqk[:, 256:512], ekb[:], ALU.mult, r=["zqk", "ekb"], w=["kp"])
            b = B.next_bank()
            pb = psum[b][:, :].bitcast(BF16)
            for h in range(4):
                B.tr(pb[0:64, h * 128:(h + 1) * 128], qt[:, h * 64:(h + 1) * 64], identb[:], r=["qt", "identb"], w=[("ps", b)])
                B.tr(pb[0:64, (4 + h) * 128:(5 + h) * 128], kt[:, h * 64:(h + 1) * 64], identb[:], r=["kt", "identb"], w=[("ps", b)])
            B.copy(qkT[:].rearrange("p a b -> p (a b)"), pb[0:64, :], r=[("ps", b)], w=["qkT"])
            b = B.next_bank()
            for h in range(4):
                B.mm(psum[b][:, h * 128:(h + 1) * 128], qkT[:, 4 + h, :], qkT[:, h, :], True, True, r=["qkT"], w=[("ps", b)])
            B.tt(ATm[:], psum[b][:, :].rearrange("p (h i) -> p h i", i=128), bc_h(c_mask[:, :], 4), ALU.mult,
                 r=[("ps", b), "c_mask"], w=["ATm"])
            bo = B.next_bank()
            for h in range(4):
                B.mm(psum[bo][:, h * 128:(h + 1) * 128], ATm[:, h, :], vbf[:, h * 128:(h + 1) * 128], True, False,
                     r=["ATm", "vbf"], w=[("ps", bo)])
                B.mm(psum[bo][:, h * 128:(h + 1) * 128], qkT[:, h, :], Sbf[:, h, :], False, True,
                     r=["qkT", "Sbf"], w=[("ps", bo)])
            B.copy(osb[:].rearrange("p a b -> p (a b)"), psum[bo][:, :], r=[("ps", bo)], w=["osb"], eng="act")
            b = B.next_bank()
            for h in range(4):
                B.mm(psum[b][0:64, h * 128:(h + 1) * 128], kp[:, h * 64:(h + 1) * 64], vbf[:, h * 128:(h + 1) * 128], True, True,
                     r=["kp", "vbf"], w=[("ps", b)])
            for h in range(4):
                B.stt(Sst[:, h, :], Sst[:, h, :], eblT[:, h:h + 1], psum[b][0:64, h * 128:(h + 1) * 128], ALU.mult, ALU.add,
                      r=["eblT", ("ps", b), "Sst"], w=["Sst"])
            B.copy(Sbf[:].rearrange("p a b -> p (a b)"), Sst[:].rearrange("p a b -> p (a b)"), r=["Sst"], w=["Sbf"], eng="dve")
            of = osb[:].rearrange("p a b -> p (a b)")
            B.tt(junk[:], of, of, ALU.mult, r=["osb"], w=["junk"])
            B.red(ss4[:], junk[:].rearrange("p (a b) -> p a b", b=128), ALU.add, r=["junk"], w=["ss4"])
            B.rsqrt(rs4[:], ss4[:], 128.0 * RMS_EPS, rsq[:, 0:4], r=["ss4"], w=["rs4"])
            B.tt(t1, osb[:], rs4[:, :].unsqueeze(2).to_broadcast([128, 4, 128]), ALU.mult, r=["osb", "rs4"], w=["t1"])
            B.tt(SG, sr[:].rearrange("p (a b) -> p a b", b=128), bc_h(ggla[:, :], 4), ALU.mult, r=["sr", "ggla"], w=["SG"])
            B.tt(gobf[:].rearrange("p (a b) -> p a b", b=128), t1, SG, ALU.mult, r=["t1", "SG"], w=["gobf"])
            B.dma(sc_go[tok0:tok0 + 128, :], gobf[:], r=["gobf"], w=[("sc_go", s, t)])
            B.act(junk[:, 0:256], ze[:, 0:256], AF.Square, r=["ze"], w=["junk", "sskv"], accum_out=sskv[:, 0:1])
            B.rsqrt(rskv[:, 0:1], sskv[:, 0:1], 256.0 * RMS_EPS, rsq[:, 0:1], r=["sskv"], w=["rskv"])
            B.stt(ckvn[:], ze[:, 0:256], rskv[:, 0:1], gkv[:], ALU.mult, ALU.mult, r=["ze", "rskv", "gkv"], w=["ckvn"])
            B.dma(o_lat[tok0:tok0 + 128, :], ckvn[:], r=["ckvn"], w=[("o_lat", s, t)])
            transpose_tile(ckvn, "ckvn", ckvT, "ckvT", 256)
            for hg in range(2):
                b = B.next_bank()
                for hh in range(4):
                    h = hg * 4 + hh
                    for kc in range(2):
                        B.mm(psum[b][0:64, hh * 128:(hh + 1) * 128], Wuk.ap[:, kc, h * 64:(h + 1) * 64], ckvT[:, kc, :],
                             kc == 0, kc == 1, r=["ckvT"] + Wuk.keys, w=[("ps", b)])
                B.copy(kT_seq[0:64, hg * 4:hg * 4 + 4, tsl], psum[b][0:64, :].rearrange("p (h t) -> p h t", t=128),
                       r=[("ps", b)], w=kT_keys)
            b = B.next_bank()
            for kc in range(2):
                B.mm(psum[b][:, :], ckvT[:, kc, :], Wuv.ap[:, kc, :], kc == 0, kc == 1, r=["ckvT"] + Wuv.keys, w=[("ps", b)])
            B.copy(V_seq[:, t, :, 0:64], psum[b][:, :].rearrange("p (h d) -> p h d", d=64), r=[("ps", b)], w=V_keys)
            rope(ze[:, 256:272].unsqueeze(1), ze[:, 272:288].unsqueeze(1), kpe[:, 64:80].unsqueeze(1), kpe[:, 80:96].unsqueeze(1),
                 cosT[:, t, :], sinT[:, t, :], 1, ["ze"], ["kpe"])
            B.dma(o_kr[tok0:tok0 + 128, :], kpe[:, 64:96], r=["kpe"], w=[("o_kr", s, t)])
            b = B.next_bank()
            B.tr(psum[b][0:96, 0:128], kpe[:, :], ident[:], r=["kpe", "ident"], w=[("ps", b)])
            B.copy(kT_seq[64:96, :, tsl], psum[b][64:96, 0:128].unsqueeze(1).to_broadcast([32, 8, 128]),
                   r=[("ps", b)], w=kT_keys, eng="dve")
            B.act(junk[:, 0:384], cq[:], AF.Square, r=["cq"], w=["junk", "sskv"], accum_out=sskv[:, 1:2])
            B.rsqrt(rskv[:, 1:2], sskv[:, 1:2], 384.0 * RMS_EPS, rsq[:, 0:1], r=["sskv"], w=["rskv"])
            B.stt(cqn[:], cq[:], rskv[:, 1:2], gq[:], ALU.mult, ALU.mult, r=["cq", "rskv", "gq"], w=["cqn"])
            transpose_tile(cqn, "cqn", cqnT, "cqnT", 384)
            for (c0, n, h0, nh) in ((0, 480, 0, 5), (480, 288, 5, 3)):
                b = B.next_bank()
                for kc in range(3):
                    B.mm(psum[b][:, 0:n], cqnT[:, kc, :], Wuq.ap[:, kc, c0:c0 + n], kc == 0, kc == 2,
                         r=["cqnT"] + Wuq.keys, w=[("ps", b)])
                P.add("act", lambda e, b=b, n=n, h0=h0, nh=nh: e.mul(
                    qtm[:, h0:h0 + nh, :], psum[b][:, 0:n].rearrange("p (h d) -> p h d", d=96), float(MLA_SCALE)),
                    r=[("ps", b)], w=["qtm"])
            rope(qtm[:, :, 64:80], qtm[:, :, 80:96], qtm[:, :, 64:80], qtm[:, :, 80:96],
                 cosT[:, t, :], sinT[:, t, :], 8, ["qtm"], ["qtm"])
            B.copy(qbf[:].rearrange("p (h d) -> p h d", d=96), qtm[:], r=["qtm"], w=["qbf"], eng="dve")
            B.dma(sc_q[tok0:tok0 + 128, :], qbf[:], r=["qbf"], w=[("sc_q", s, t)])
        B.dma(o_sgla[s].rearrange("h d v -> d h v"), Sst[:], r=["Sst"], w=[("o_sgla", s)])


    oxb = zqk[:, :].bitcast(BF16).rearrange("p (h d) -> p h d", d=256)
    oxT = sr[:, :].bitcast(BF16).rearrange("p (c t) -> p c t", t=128)
    qT = osb[:].rearrange("p a b -> p (a b)").bitcast(BF16)[0:96, :].rearrange("p (h t) -> p h t", t=128)
    _jb = junk[:, :].bitcast(BF16)
    _qb = qtm[:].rearrange("p a b -> p (a b)").bitcast(BF16)
    PT = [_jb[:, 0:512].rearrange("p (a b) -> p a b", b=128), _jb[:, 512:1024].rearrange("p (a b) -> p a b", b=128),
          _qb[:, 0:512].rearrange("p (a b) -> p a b", b=128), _qb[:, 512:1024].rearrange("p (a b) -> p a b", b=128)]
    PTk = ["junk", "junk", "qtm", "qtm"]
    Wout = AV(arena, 0, [8, 1024])
    Wxq = AV(arena, 8192, [8, 1024])
    Wxo = AV(arena, 16384, [8, 1024])
    catT, h1T = xT[0], xT[1]
    catk, h1Tk = ("xT", 0), ("xT", 1)
    qxT = memT[:, :, 0:128]
    PX = memT[:, :, 128:256]
    mlao = vbf

    class _Tile:
        def __init__(self, v, ti, ntok):
            self.v, self.ti, self.n = v, ti, ntok

        def __getitem__(self, key):
            if isinstance(key, tuple):
                return self.v[:self.n, self.ti, key[1]]
            return self.v[:self.n, self.ti, :]

    class _Tile2:
        def __init__(self, v, ntok):
            self.v, self.n = v, ntok

        def __getitem__(self, key):
            if isinstance(key, tuple):
                return self.v[:self.n, key[1]]
            return self.v[:self.n, :]

    def layer_norm(pre, prek, gi, out, outk, ntok=128):
        n = ntok
        for hf in range(2):
            P.add("dve", lambda e, hf=hf: e.bn_stats(lnst[:n, hf, :], pre[:, hf * 512:(hf + 1) * 512]), r=[prek], w=["lnst"])
        P.add("dve", lambda e: e.bn_aggr(lnmv[:n, :], lnst[:n, :, :]), r=["lnst"], w=["lnmv"])
        B.rsqrt(lnrs[:n, 0:1], lnmv[:n, 1:2], LN_EPS, rsq[:n, 4:5], r=["lnmv"], w=["lnrs"], tk="rsq_ln")
        B.stt(lnrs[:n, 1:2], lnmv[:n, 0:1], -1.0, lnrs[:n, 0:1], ALU.mult, ALU.mult, r=["lnmv", "lnrs"], w=["lnrs"])
        B.act(big1[:n, :], pre[:], AF.Identity, r=[prek, "lnrs"], w=["t1", "SG"], scale=lnrs[:n, 0:1], bias=lnrs[:n, 1:2])
        B.tt(big1[:n, :], big1[:n, :], lnp[gi][:n, :], ALU.mult, r=["t1", "SG", ("lnp", gi)], w=["t1", "SG"])
        B.tt(out[:], big1[:n, :], lnp[gi + 1][:n, :], ALU.add, r=["t1", "SG", ("lnp", gi + 1)], w=[outk])

    def load_B_weights():
        load_w(Wout.ap, w_out, Wout.keys)
        load_w(Wxq.ap, w_xq, Wxq.keys)
        load_w(Wxo.ap, w_xo, Wxo.keys)

    def phaseB(s):
        load_B_weights()
        B.bank_lim = 6
        B.psn = 0
        for t in range(16):
            tok0 = s * SEQ + t * 128
            i = cnt["x"] % 2
            cnt["x"] += 1
            xk = ("xin", i)
            B.dma(xin[i][:], xp[tok0:tok0 + 128, :], w=[xk])
            B.dma(qbf[:], sc_q[tok0:tok0 + 128, :], r=[("sc_q", s, t)], w=["qbf"])
            B.dma(gobf[:], sc_go[tok0:tok0 + 128, :], r=[("sc_go", s, t)], w=["gobf"])
            b = B.next_bank()
            pb = psum[b][:, :].bitcast(BF16)
            for h in range(8):
                B.tr(pb[0:96, h * 128:(h + 1) * 128], qbf[:, h * 96:(h + 1) * 96], identb[:], r=["qbf", "identb"], w=[("ps", b)])
            B.copy(qT, pb[0:96, :].rearrange("p (h t) -> p h t", t=128), r=[("ps", b)], w=["osb"])
            b = B.next_bank()
            pb = psum[b][:, :].bitcast(BF16)
            for c in range(4):
                B.tr(pb[:, c * 128:(c + 1) * 128], gobf[:, c * 128:(c + 1) * 128], identb[:], r=["gobf", "identb"], w=[("ps", b)])
            B.copy(catT[:, 0:4, :], pb[:, 0:512].rearrange("p (c t) -> p c t", t=128), r=[("ps", b)], w=[catk])
            nk = t + 1
            for h in range(8):
                ob = 6 + h // 4
                oc = (h % 4) * 65
                for g0 in range(0, nk, 4):
                    g1 = min(nk, g0 + 4)
                    b = B.next_bank()
                    for kt_ in range(g0, g1):
                        j = kt_ - g0
                        B.mm(psum[b][:, j * 128:(j + 1) * 128], kT_seq[0:96, h, kt_ * 128:(kt_ + 1) * 128], qT[:, h, :],
                             True, True, r=["osb"] + kT_keys, w=[("ps", b)])
                    pi = cnt["st"] % 4
                    cnt["st"] += 1
                    n = g1 - g0
                    B.act(PT[pi][:, 0:n, :], psum[b][:, 0:n * 128].rearrange("p (a b) -> p a b", b=128), AF.Exp,
                          r=[("ps", b)], w=[PTk[pi]])
                    if g1 == nk:
                        B.tt(PT[pi][:, n - 1, :], PT[pi][:, n - 1, :], maskb[:], ALU.mult, r=[PTk[pi], "maskb"], w=[PTk[pi]])
                    for kt_ in range(g0, g1):
                        j = kt_ - g0
                        B.mm(psum[ob][:, oc:oc + 65], PT[pi][:, j, :], V_seq[:, kt_, h, 0:65], kt_ == 0, kt_ == nk - 1,
                             r=[PTk[pi]] + V_keys, w=[("ps", ob)])
            for hg in range(2):
                ov = psum[6 + hg][:, 0:260].rearrange("p (h d) -> p h d", d=65)
                P.add("dve", lambda e, ov=ov, hg=hg: e.reciprocal(rec8[:, hg * 4:hg * 4 + 4], ov[:, :, 64]),
                      r=[("ps", 6 + hg)], w=[("rec8", hg)])
                B.tt(mlao[:, hg * 256:(hg + 1) * 256].rearrange("p (h d) -> p h d", d=64), ov[:, :, 0:64],
                     rec8[:, hg * 4:hg * 4 + 4].unsqueeze(2).to_broadcast([128, 4, 64]), ALU.mult,
                     r=[("ps", 6 + hg), ("rec8", hg)], w=["vbf"])
            b = B.next_bank()
            pb = psum[b][:, :].bitcast(BF16)
            for c in range(4):
                B.tr(pb[:, c * 128:(c + 1) * 128], mlao[:, c * 128:(c + 1) * 128], identb[:], r=["vbf", "identb"], w=[("ps", b)])
            B.copy(catT[:, 4:8, :], pb[:, 0:512].rearrange("p (c t) -> p c t", t=128), r=[("ps", b)], w=[catk])
            pre, prek = stage[0], ("stage", 0)
            h1, h1k = stage[1], ("stage", 1)
            for hf in range(2):
                b = B.next_bank()
                for kc in range(8):
                    B.mm(psum[b][:, :], catT[:, kc, :], Wout.ap[:, kc, hf * 512:(hf + 1) * 512], kc == 0, kc == 7,
                         r=[catk] + Wout.keys, w=[("ps", b)])
                B.stt(pre[:, hf * 512:(hf + 1) * 512], xin[i][:, hf * 512:(hf + 1) * 512], float(ALPHA), psum[b][:, :],
                      ALU.mult, ALU.add, r=[xk, ("ps", b)], w=[prek])
            layer_norm(pre, prek, 0, h1, h1k)
            transpose_tile(h1, h1k, h1T, h1Tk, D)
            for jg in range(2):
                b = B.next_bank()
                for jj in range(4):
                    j = jg * 4 + jj
                    for kc in range(8):
                        B.mm(psum[b][:, jj * 128:(jj + 1) * 128], Wxq.ap[:, kc, j * 128:(j + 1) * 128], h1T[:, kc, :],
                             kc == 0, kc == 7, r=[h1Tk] + Wxq.keys, w=[("ps", b)])
                P.add("act", lambda e, b=b, jg=jg: e.mul(qxT[:, jg * 4:jg * 4 + 4, :],
                      psum[b][:, :].rearrange("p (a t) -> p a t", t=128), 1.0 / 16.0), r=[("ps", b)], w=["qxT"])
            for hg in range(2):
                b = B.next_bank()
                for hh in range(2):
                    h = hg * 2 + hh
                    for mt in range(2):
                        col = (hh * 2 + mt) * 128
                        for c in range(2):
                            B.mm(psum[b][:, col:col + 128], memkT[:, s, h * 2 + c, mt * 128:(mt + 1) * 128], qxT[:, h * 2 + c, :],
                                 c == 0, c == 1, r=["memkT", "qxT"], w=[("ps", b)])
                B.act(PX[:, hg * 4:hg * 4 + 4, :], psum[b][:, :].rearrange("p (a t) -> p a t", t=128), AF.Exp,
                      r=[("ps", b)], w=["PX"])
            for h in range(4):
                b = B.next_bank()
                for mt in range(2):
                    B.mm(psum[b][:, 0:257], PX[:, h * 2 + mt, :], memv[:, s, mt, h, 0:257], mt == 0, mt == 1,
                         r=["PX", "memv"], w=[("ps", b)])
                P.add("dve", lambda e, b=b, h=h: e.reciprocal(rec8[:, h:h + 1], psum[b][:, 256:257]), r=[("ps", b)], w=[("rec8", "x", h)])
                B.ts(oxb[:, h, :], psum[b][:, 0:256], rec8[:, h:h + 1], None, ALU.mult, r=[("ps", b), ("rec8", "x", h)], w=["zqk"])
            for cg in range(2):
                b = B.next_bank()
                pb = psum[b][:, :].bitcast(BF16)
                oxf = zqk[:, :].bitcast(BF16)
                for cc in range(4):
                    c = cg * 4 + cc
                    B.tr(pb[:, cc * 128:(cc + 1) * 128], oxf[:, c * 128:(c + 1) * 128], identb[:], r=["zqk", "identb"], w=[("ps", b)])
                B.copy(oxT[:, cg * 4:cg * 4 + 4, :], pb[:, 0:512].rearrange("p (c t) -> p c t", t=128), r=[("ps", b)], w=["sr"])
            for hf in range(2):
                b = B.next_bank()
                for kc in range(8):
                    B.mm(psum[b][:, :], oxT[:, kc, :], Wxo.ap[:, kc, hf * 512:(hf + 1) * 512], kc == 0, kc == 7,
                         r=["sr"] + Wxo.keys, w=[("ps", b)])
                B.stt(pre[:, hf * 512:(hf + 1) * 512], h1[:, hf * 512:(hf + 1) * 512], float(ALPHA), psum[b][:, :],
                      ALU.mult, ALU.add, r=[h1k, ("ps", b)], w=[prek])
            layer_norm(pre, prek, 2, xin[i], xk)
            B.dma(sc_h2[tok0:tok0 + 128, :], xin[i][:], r=[xk], w=[("sc_h2", tok0 // 128)])
        B.bank_lim = 8


    SK = ["s_idx", "s_pg0", "s_pg1", "s_pg2", "s_pg3", "s_cT0", "s_cT1", "s_cT2", "s_cT3", "s_cb0", "s_cb1", "s_cb2", "s_cb3",
          "s_PS0", "s_PS1", "s_v32", "s_vm0", "s_vm1", "s_colT", "s_QM", "s_S0", "s_S1", "s_catS", "s_QLT", "s_qTs", "s_WukT",
          "s_ol", "s_olT", "s_om", "s_mk0", "s_mk1", "s_e16", "s_qx", "s_pxs", "s_om4", "s_den"]

    def sv(off, n, dt=F32, parts=128):
        a = arenaS[0:parts, off:off + n]
        return a.bitcast(dt) if dt != F32 else a
    s_idx = sv(0, 1040, I32)
    s_pg = [sv(1040 + k_ * 352, 352) for k_ in range(4)]
    s_cT = [sv(2448 + k_ * 192, 192, BF16).rearrange("p (c t) -> p c t", t=128) for k_ in range(4)]
    s_cb = [sv(3216 + k_ * 130, 130, BF16) for k_ in range(4)]
    s_PS = [sv(3736 + k_ * 16, 16, BF16) for k_ in range(2)]
    s_v32 = sv(3768, 512, parts=16)
    s_vm = [sv(4280 + k_ * 512, 512, parts=16) for k_ in range(2)]
    s_colT = sv(5304, 192, parts=64).rearrange("p (a b) -> p a b", b=16)
    s_QM = sv(5496, 1024, parts=64).rearrange("p (h b m) -> p h b m", b=16, m=16)
    s_S = [sv(6520 + k_ * 512, 512, parts=64).rearrange("p (h v) -> p h v", v=128) for k_ in range(2)]
    s_catS = sv(7544, 1024, parts=16)
    s_QLT = sv(8568, 128, BF16).rearrange("p (c h b) -> p c h b", h=8, b=16)
    s_qTs = sv(8696, 64, BF16, parts=96).rearrange("p (h b) -> p h b", b=16)
    s_WukT = sv(8760, 1024, BF16, parts=64).rearrange("p (h r) -> p h r", r=256)
    s_ol = sv(9784, 256, parts=8)
    s_olT = sv(10040, 8, BF16).rearrange("p (c h) -> p c h", h=8)
    s_om = sv(10048, 512, parts=8)
    s_mk = [sv(10560 + k_ * 1024, 1024) for k_ in range(2)]
    s_e16 = sv(1040, 128, parts=16)
    s_qx = sv(1168, 1024, parts=16)
    s_pxs = sv(2192, 4, BF16)
    s_om4 = sv(2200, 1024, parts=4)
    s_den = sv(3224, 8, parts=4)

    def barrier(keys):
        P.add("dve", lambda e: e.memset(rsq[:, 7:8], 0.0), r=[], w=list(keys) + ["rsq_bar"])

    def rms_rows(src, n, sscol, eps_n, gain, out, srck, outk, ntok):
        B.act(junk[:ntok, 0:n], src, AF.Square, r=[srck], w=["junk", "sskv"], accum_out=sskv[:ntok, sscol:sscol + 1])
        B.rsqrt(rskv[:ntok, sscol:sscol + 1], sskv[:ntok, sscol:sscol + 1], eps_n, rsq[:ntok, 0:1], r=["sskv"], w=["rskv"])
        B.stt(out, src, rskv[:ntok, sscol:sscol + 1], gain, ALU.mult, ALU.mult, r=[srck, "rskv"], w=[outk])

    def sampleA():
        n = NS
        barrier(kT_keys + V_keys + SK)
        load_A_weights()
        B.bank_lim = 6
        B.psn = 0
        i = cnt["x"] % 2
        cnt["x"] += 1
        xk, xtk = ("xin", i), ("xT", i)
        B.dma(xin[i][:n, :], xs, w=[xk])
        transpose_tile(xin[i], xk, xT[i], xtk, D, ntok=n)
        B.dma(s_idx[:, 0:NS * NPAGE], bass.AP(pt_d.tensor, pt_d.offset, [[0, 128], [1, NS * NPAGE]]), w=["s_idx"])
        B.ts(s_idx[:, 0:NS * NPAGE], s_idx[:, 0:NS * NPAGE], 128.0, pcol[:, 0:1], ALU.mult, ALU.add, r=["s_idx", "pcol"], w=["s_idx"])
        for k_ in range(4):
            P.add("dve", lambda e, k_=k_: e.memset(s_pg[k_], 0.0), w=["s_pg%d" % k_])
            P.add("dve", lambda e, k_=k_: e.memset(s_cb[k_], 1.0), w=["s_cb%d" % k_])

        def zgroup(av, nn):
            b = B.next_bank()
            for kc in range(8):
                B.mm(psum[b][:n, 0:nn], xT[i][:, kc, 0:n], av.ap[:, kc, :], kc == 0, kc == 7, r=[xtk] + av.keys, w=[("ps", b)])
            return b
        b = zgroup(Wqk, 512)
        B.copy(zqk[:n, :], psum[b][:n, :], r=[("ps", b)], w=["zqk"])
        b = zgroup(Wv, 512)
        B.copy(s_v32, psum[b][:n, :], r=[("ps", b)], w=["s_v32"])
        b = zgroup(Wr, 512)
        B.act(sr[:n, :], psum[b][:n, :], AF.Silu, r=[("ps", b)], w=["sr"])
        b = zgroup(Wcq, 384)
        B.copy(cq[:n, :], psum[b][:n, 0:384], r=[("ps", b)], w=["cq"])
        b = zgroup(We, 288)
        B.copy(ze[:n, :], psum[b][:n, 0:288], r=[("ps", b)], w=["ze"])
        b = B.next_bank()
        for kc in range(8):
            B.mm(psum[b][0:16, 0:n], Wa.ap[:, kc, :], xT[i][:, kc, 0:n], kc == 0, kc == 7, r=[xtk] + Wa.keys, w=[("ps", b)])
        B.copy(aT[0:16, 0:n], psum[b][0:16, 0:n], r=[("ps", b)], w=["aT"])
        b = B.next_bank()
        B.mm(psum[b][:n, 0:256], aT[0:17, 0:n], wgate[0:17, :], True, True, r=["aT", "wgate"], w=[("ps", b)])
        B.act(e1[:n, :], psum[b][:n, 0:256], AF.Exp, r=[("ps", b)], w=["e1"], scale=-1.0)
        B.act(l1[:n, :], e1[:n, :], AF.Ln, r=["e1"], w=["l1"], bias=1.0)
        B.act(eb[:n, :], l1[:n, :], AF.Exp, r=["l1"], w=["eb"], scale=-1.0 / 16.0)
        b = B.next_bank()
        for xi, (src, c0, key) in enumerate(((eb, 0, "eb"), (zqk, 256, "zqk"), (zqk, 0, "zqk"))):
            for h in range(4):
                B.tr(psum[b][0:64, (xi * 4 + h) * 16:(xi * 4 + h + 1) * 16], src[:n, c0 + h * 64:c0 + (h + 1) * 64], ident[:n, :n],
                     r=[key, "ident"], w=[("ps", b)])
        B.copy(s_colT[:, 0:8, :], psum[b][0:64, 0:128].rearrange("p (a b) -> p a b", b=16), r=[("ps", b)], w=["s_colT"], eng="dve")
        P.add("act", lambda e, b=b: e.mul(s_colT[:, 8:12, :], psum[b][0:64, 128:192].rearrange("p (a b) -> p a b", b=16), 0.125),
              r=[("ps", b)], w=["s_colT"])
        B.tt(s_QM, s_colT[:, 8:12, :].unsqueeze(3).to_broadcast([64, 4, 16, 16]),
             i16[:, :, :].unsqueeze(1).to_broadcast([64, 4, 16, 16]), ALU.mult, r=["s_colT", "i16"], w=["s_QM"])
        B.bank_lim = 4
        B.psn = 0
        for bb in range(n):
            sl = bb % 2
            Sk = "s_S%d" % sl
            B.dma(s_S[sl], sgla_s[bb].rearrange("h d v -> d h v"), w=[Sk])
            B.ts(s_vm[sl], s_v32, ident[:n, bb:bb + 1], None, ALU.mult, r=["s_v32", "ident"], w=["s_vm%d" % sl])
            b = B.next_bank()
            for h in range(4):
                B.mm(psum[b][0:64, h * 128:(h + 1) * 128], zqk[:n, 256 + h * 64:256 + (h + 1) * 64], s_vm[sl][:, h * 128:(h + 1) * 128],
                     True, True, r=["zqk", "s_vm%d" % sl], w=[("ps", b)])
            for h in range(4):
                B.stt(s_S[sl][:, h, :], s_S[sl][:, h, :], s_colT[:, h, bb:bb + 1], psum[b][0:64, h * 128:(h + 1) * 128], ALU.mult, ALU.add,
                      r=[Sk, "s_colT", ("ps", b)], w=[Sk])
            B.dma(o_sgla_s[bb].rearrange("h d v -> d h v"), s_S[sl], r=[Sk], w=[("o_sgla_s", bb)])
            for h in range(4):
                B.mm(psum[4 + h][:n, 0:128], s_QM[:, h, bb, :], s_S[sl][:, h, :], bb == 0, bb == n - 1,
                     r=["s_QM", Sk], w=[("ps", 4 + h)])
        for h in range(4):
            B.copy(osb[:n, h, :], psum[4 + h][:n, 0:128], r=[("ps", 4 + h)], w=["osb"], eng="act")
        B.bank_lim = 6
        B.psn = 0
        of = osb[:n].rearrange("p a b -> p (a b)")
        B.tt(junk[:n, :], of, of, ALU.mult, r=["osb"], w=["junk"])
        B.red(ss4[:n, :], junk[:n, :].rearrange("p (a b) -> p a b", b=128), ALU.add, r=["junk"], w=["ss4"])
        B.rsqrt(rs4[:n, :], ss4[:n, :], 128.0 * RMS_EPS, rsq[:n, 0:4], r=["ss4"], w=["rs4"])
        B.tt(t1[:n], osb[:n], rs4[:n, :].unsqueeze(2).to_broadcast([n, 4, 128]), ALU.mult, r=["osb", "rs4"], w=["t1"])
        B.tt(SG[:n], sr[:n, :].rearrange("p (a b) -> p a b", b=128), bc_h(ggla[:n, :], 4), ALU.mult, r=["sr", "ggla"], w=["SG"])
        B.tt(s_catS[:, 0:512].rearrange("p (a b) -> p a b", b=128), t1[:n], SG[:n], ALU.mult, r=["t1", "SG"], w=["s_catS"])
        rms_rows(ze[:n, 0:256], 256, 0, 256.0 * RMS_EPS, gkv[:n, :], ckvn[:n, :], "ze", "ckvn", n)
        B.dma(o_lat_s, ckvn[:n, :], r=["ckvn"], w=["o_lat_s"])
        rope(ze[:n, 256:272].unsqueeze(1), ze[:n, 272:288].unsqueeze(1), kpe[:n, 64:80].unsqueeze(1), kpe[:n, 80:96].unsqueeze(1),
             cos_s[:, :], sin_s[:, :], 1, ["ze"], ["kpe"], ntok=n, ck="cos_s", sk_="sin_s")
        B.dma(o_kr_s, kpe[:n, 64:96], r=["kpe"], w=["o_kr_s"])
        rms_rows(cq[:n, :], 384, 1, 384.0 * RMS_EPS, gq[:n, :], cqn[:n, :], "cq", "cqn", n)
        transpose_tile(cqn, "cqn", cqnT, "cqnT", 384, ntok=n)
        for (c0, nn, h0, nh) in ((0, 480, 0, 5), (480, 288, 5, 3)):
            b = B.next_bank()
            for kc in range(3):
                B.mm(psum[b][:n, 0:nn], cqnT[:, kc, 0:n], Wuq.ap[:, kc, c0:c0 + nn], kc == 0, kc == 2, r=["cqnT"] + Wuq.keys, w=[("ps", b)])
            P.add("act", lambda e, b=b, nn=nn, h0=h0, nh=nh: e.mul(
                qtm[:n, h0:h0 + nh, :], psum[b][:n, 0:nn].rearrange("p (h d) -> p h d", d=96), float(MLA_SCALE)),
                r=[("ps", b)], w=["qtm"])
        rope(qtm[:n, :, 64:80], qtm[:n, :, 80:96], qtm[:n, :, 64:80], qtm[:n, :, 80:96], cos_s[:, :], sin_s[:, :], 8, ["qtm"], ["qtm"],
             ntok=n, ck="cos_s", sk_="sin_s")
        b = B.next_bank()
        for h in range(8):
            B.tr(psum[b][0:96, h * 16:(h + 1) * 16], qtm[:n, h, :], ident[:n, :n], r=["qtm", "ident"], w=[("ps", b)])
        B.copy(s_qTs, psum[b][0:96, 0:128].rearrange("p (h b) -> p h b", b=16), r=[("ps", b)], w=["s_qTs"])
        for kc in range(2):
            b = B.next_bank()
            pb = psum[b][:, :].bitcast(BF16)
            for h in range(8):
                B.tr(pb[0:64, h * 128:(h + 1) * 128], Wuk.ap[:, kc, h * 64:(h + 1) * 64], identb[:], r=Wuk.keys + ["identb"], w=[("ps", b)])
            B.copy(s_WukT[:, :, kc * 128:(kc + 1) * 128], pb[0:64, :].rearrange("p (h r) -> p h r", r=128), r=[("ps", b)], w=["s_WukT"])
        b = B.next_bank()
        for rc in range(2):
            for h in range(8):
                B.mm(psum[b][:, (rc * 8 + h) * 16:(rc * 8 + h + 1) * 16], s_WukT[:, h, rc * 128:(rc + 1) * 128], s_qTs[0:64, h, :],
                     True, True, r=["s_WukT", "s_qTs"], w=[("ps", b)])
        B.copy(s_QLT, psum[b][:, 0:256].rearrange("p (c h b) -> p c h b", h=8, b=16), r=[("ps", b)], w=["s_QLT"])
        B.bank_lim = 5
        B.psn = 0
        slot = 0
        gcnt = 0
        for bb in range(n):
            pages = [("new", 0)] + [("page", j) for j in range(NPAGE)]
            for g0 in range(0, len(pages), 4):
                grp = pages[g0:g0 + 4]
                bs = B.next_bank()
                slots = []
                for jj, (kind, j) in enumerate(grp):
                    sl = slot % 4
                    slot += 1
                    slots.append(sl)
                    pgk, cTk, cbk = "s_pg%d" % sl, "s_cT%d" % sl, "s_cb%d" % sl
                    if kind == "new":
                        B.dma(s_pg[sl][0:1, 0:256], o_lat_s[bb:bb + 1, :], r=["o_lat_s"], w=[pgk])
                        B.dma(s_pg[sl][0:1, 320:352], o_kr_s[bb:bb + 1, :], r=["o_kr_s"], w=[pgk])
                    else:
                        col = bb * NPAGE + j
                        P.add("pool", lambda e, sl=sl, col=col: e.indirect_dma_start(
                            out=s_pg[sl][:, 0:256], out_offset=None, in_=c_lat[:, :],
                            in_offset=bass.IndirectOffsetOnAxis(ap=s_idx[:, col:col + 1], axis=0)), r=["s_idx"], w=[pgk], dma=True)
                        P.add("pool", lambda e, sl=sl, col=col: e.indirect_dma_start(
                            out=s_pg[sl][:, 320:352], out_offset=None, in_=c_kr[:, :],
                            in_offset=bass.IndirectOffsetOnAxis(ap=s_idx[:, col:col + 1], axis=0)), r=["s_idx"], w=[pgk], dma=True)
                    bt = B.next_bank()
                    B.tr(psum[bt][:, 0:128], s_pg[sl][:, 0:128], ident[:], r=[pgk, "ident"], w=[("ps", bt)])
                    B.tr(psum[bt][:, 128:256], s_pg[sl][:, 128:256], ident[:], r=[pgk, "ident"], w=[("ps", bt)])
                    B.tr(psum[bt][0:96, 256:384], s_pg[sl][:, 256:352], ident[:], r=[pgk, "ident"], w=[("ps", bt)])
                    B.copy(s_cT[sl], psum[bt][:, 0:384].rearrange("p (c t) -> p c t", t=128), r=[("ps", bt)], w=[cTk])
                    B.copy(s_cb[sl][:, 0:256], s_pg[sl][:, 0:256], r=[pgk], w=[cbk], eng="pool")
                    B.mm(psum[bs][:, jj * 8:(jj + 1) * 8], s_cT[sl][:, 0, :], s_QLT[:, 0, :, bb], True, False, r=[cTk, "s_QLT"], w=[("ps", bs)])
                    B.mm(psum[bs][:, jj * 8:(jj + 1) * 8], s_cT[sl][:, 1, :], s_QLT[:, 1, :, bb], False, False, r=[cTk, "s_QLT"], w=[("ps", bs)])
                    B.mm(psum[bs][:, jj * 8:(jj + 1) * 8], s_cT[sl][64:96, 2, :], s_qTs[64:96, :, bb], False, True, r=[cTk, "s_qTs"], w=[("ps", bs)])
                ng = len(grp)
                pi = gcnt % 2
                gcnt += 1
                PSk = "s_PS%d" % pi
                B.act(s_PS[pi][:, 0:ng * 8], psum[bs][:, 0:ng * 8], AF.Exp, r=[("ps", bs)], w=[PSk])
                if grp[0][0] == "new":
                    B.tt(s_PS[pi][:, 0:8], s_PS[pi][:, 0:8], maskb[:, 0:1].to_broadcast([128, 8]), ALU.mult, r=[PSk, "maskb"], w=[PSk])
                for jj, (kind, j) in enumerate(grp):
                    sl = slots[jj]
                    first = (g0 + jj == 0)
                    last = (g0 + jj == len(pages) - 1)
                    B.mm(psum[6][0:8, 0:257], s_PS[pi][:, jj * 8:(jj + 1) * 8], s_cb[sl][:, 0:257], first, last,
                         r=[PSk, "s_cb%d" % sl], w=[("ps", 6)])
            P.add("dve", lambda e: e.reciprocal(rec8[0:8, 0:1], psum[6][0:8, 256:257]), r=[("ps", 6)], w=[("rec8", 0)])
            B.ts(s_ol, psum[6][0:8, 0:256], rec8[0:8, 0:1], None, ALU.mult, r=[("ps", 6), ("rec8", 0)], w=["s_ol"])
            b = B.next_bank()
            for c in range(2):
                B.tr(psum[b][:, c * 8:(c + 1) * 8], s_ol[:, c * 128:(c + 1) * 128], ident[:8, :8], r=["s_ol", "ident"], w=[("ps", b)])
            B.copy(s_olT, psum[b][:, 0:16].rearrange("p (c h) -> p c h", h=8), r=[("ps", b)], w=["s_olT"], eng="dve")
            b = B.next_bank()
            for kc in range(2):
                B.mm(psum[b][0:8, :], s_olT[:, kc, :], Wuv.ap[:, kc, :], kc == 0, kc == 1, r=["s_olT"] + Wuv.keys, w=[("ps", b)])
            B.tt(s_om, psum[b][0:8, :], bm8[:, :], ALU.mult, r=[("ps", b), "bm8"], w=["s_om"])
            B.mm(psum[7][:n, :], i16[0:8, bb, :], s_om, bb == 0, bb == n - 1, r=["i16", "s_om"], w=[("ps", 7)])
        B.copy(s_catS[:, 512:1024], psum[7][:n, :], r=[("ps", 7)], w=["s_catS"], eng="dve")
        B.bank_lim = 8

    def sampleB():
        n = NS
        load_B_weights()
        B.dma(bm4, bm4_d, w=["memkT"])
        B.bank_lim = 6
        B.psn = 0
        i = cnt["x"] % 2
        cnt["x"] += 1
        xk = ("xin", i)
        B.dma(xin[i][:n, :], xs, w=[xk])
        transpose_tile(s_catS, "s_catS", catT, catk, D, ntok=n)
        pre, prek = stage[0], ("stage", 0)
        h1, h1k = stage[1], ("stage", 1)
        for hf in range(2):
            b = B.next_bank()
            for kc in range(8):
                B.mm(psum[b][:n, :], catT[:, kc, 0:n], Wout.ap[:, kc, hf * 512:(hf + 1) * 512], kc == 0, kc == 7,
                     r=[catk] + Wout.keys, w=[("ps", b)])
            B.stt(pre[:n, hf * 512:(hf + 1) * 512], xin[i][:n, hf * 512:(hf + 1) * 512], float(ALPHA), psum[b][:n, :],
                  ALU.mult, ALU.add, r=[xk, ("ps", b)], w=[prek])
        layer_norm(_Tile2(pre, n), prek, 0, _Tile2(h1, n), h1k, ntok=n)
        transpose_tile(h1, h1k, h1T, h1Tk, D, ntok=n)
        for hf in range(2):
            b = B.next_bank()
            for kc in range(8):
                B.mm(psum[b][:n, :], h1T[:, kc, 0:n], Wxq.ap[:, kc, hf * 512:(hf + 1) * 512], kc == 0, kc == 7,
                     r=[h1Tk] + Wxq.keys, w=[("ps", b)])
            P.add("act", lambda e, b=b, hf=hf: e.mul(s_qx[:, hf * 512:(hf + 1) * 512], psum[b][:n, :], 1.0 / 16.0), r=[("ps", b)], w=["s_qx"])
        for bb in range(n):
            B.ts(s_e16, ones16[:, :], ident[:n, bb:bb + 1], None, ALU.mult, r=["ones16", "ident"], w=["s_e16"])
            qb = []
            for hf in range(2):
                b = B.next_bank()
                B.mm(psum[b][:, :], s_e16, s_qx[:, hf * 512:(hf + 1) * 512], True, True, r=["s_e16", "s_qx"], w=[("ps", b)])
                qb.append(b)
            for mt in range(2):
                mk_, mkk = s_mk[0], "s_mk0"
                B.dma(mk_, cmk[bb, mt * 128:(mt + 1) * 128, :], w=[mkk])
                for hf in range(2):
                    B.tt(mk_[:, hf * 512:(hf + 1) * 512], mk_[:, hf * 512:(hf + 1) * 512], psum[qb[hf]][:, :], ALU.mult,
                         r=[mkk, ("ps", qb[hf])], w=[mkk])
                B.red(rsq[:, 0:4], mk_.rearrange("p (h d) -> p h d", d=256), ALU.add, r=[mkk], w=["rsq_tmp"])
                B.act(s_pxs[:, mt * 4:(mt + 1) * 4], rsq[:, 0:4], AF.Exp, r=["rsq_tmp"], w=["s_pxs"])
            bo = [B.next_bank(), B.next_bank()]
            bd = B.next_bank()
            for mt in range(2):
                mv_, mvk = s_mk[1], "s_mk1"
                B.dma(mv_, cmv[bb, mt * 128:(mt + 1) * 128, :], w=[mvk])
                mvb = oxT[:].rearrange("p c t -> p (c t)")
                B.copy(mvb, mv_, r=[mvk], w=["sr"])
                for hf in range(2):
                    B.mm(psum[bo[hf]][0:4, :], s_pxs[:, mt * 4:(mt + 1) * 4], mvb[:, hf * 512:(hf + 1) * 512], mt == 0, mt == 1,
                         r=["s_pxs", "sr"], w=[("ps", bo[hf])])
                B.mm(psum[bd][0:4, 0:1], s_pxs[:, mt * 4:(mt + 1) * 4], maskb[:, 127:128], mt == 0, mt == 1,
                     r=["s_pxs", "maskb"], w=[("ps", bd)])
            P.add("dve", lambda e, bd=bd: e.reciprocal(s_den[:, 0:1], psum[bd][0:4, 0:1]), r=[("ps", bd)], w=["s_den"])
            for hf in range(2):
                B.stt(s_om4[:, hf * 512:(hf + 1) * 512], psum[bo[hf]][0:4, :], s_den[:, 0:1], bm4[:, hf * 512:(hf + 1) * 512],
                      ALU.mult, ALU.mult, r=[("ps", bo[hf]), "s_den", "memkT"], w=["s_om4"])
                B.mm(psum[6 + hf][:n, :], i16[0:4, bb, :], s_om4[:, hf * 512:(hf + 1) * 512], bb == 0, bb == n - 1,
                     r=["i16", "s_om4"], w=[("ps", 6 + hf)])
        for hf in range(2):
            B.copy(s_catS[:, hf * 512:(hf + 1) * 512], psum[6 + hf][:n, :], r=[("ps", 6 + hf)], w=["s_catS"], eng="dve")
        transpose_tile(s_catS, "s_catS", catT, catk, D, ntok=n)
        for hf in range(2):
            b = B.next_bank()
            for kc in range(8):
                B.mm(psum[b][:n, :], catT[:, kc, 0:n], Wxo.ap[:, kc, hf * 512:(hf + 1) * 512], kc == 0, kc == 7,
                     r=[catk] + Wxo.keys, w=[("ps", b)])
            B.stt(pre[:n, hf * 512:(hf + 1) * 512], h1[:n, hf * 512:(hf + 1) * 512], float(ALPHA), psum[b][:n, :],
                  ALU.mult, ALU.add, r=[h1k, ("ps", b)], w=[prek])
        layer_norm(_Tile2(pre, n), prek, 2, _Tile2(xin[i], n), xk, ntok=n)
        B.dma(sc_h2[NT:NT + n, :], xin[i][:n, :], r=[xk], w=[("sc_h2", NT // 128)])
        B.bank_lim = 8

    if "A" in stages:
        for s in range(NSEQ):
            phaseA(s)
            if "B" in stages:
                phaseB(s)
    if "S" in stages:
        sampleA()
        sampleB()


    yacc_f, yacc_k = sview(0, 4096)
    yacc = yacc_f.rearrange("p (t d) -> p t d", d=D)
    h2Tf_f, h2Tf_k = sview(4096, 4096)
    h2Tf = h2Tf_f.rearrange("p (c t) -> p c t", t=512)
    h2Tb_f, h2Tb_k = sview(8192, 2048, BF16)
    h2Tb = h2Tb_f.rearrange("p (c t) -> p c t", t=512)
    hT_f, hT_k = sview(10240, 512, BF16)
    hT = hT_f.rearrange("p (f t) -> p f t", t=512)
    yout_f, yout_k = sview(10752, 2048)
    yout = [yout_f[:, 0:1024], yout_f[:, 1024:2048]]
    EW = []
    for k_ in range(4):
        o = k_ * 6144
        EW.append((AV(arena, o, [8, 256]), AV(arena, o + 2048, [8, 256]), AV(arena, o + 4096, [2, 1024])))

    def load_expert(e):
        g_, u_, d_ = EW[e % 4]
        load_w(g_.ap, w_eg[e], g_.keys)
        load_w(u_.ap, w_eu[e], u_.keys)
        load_w(d_.ap, w_ed[e], d_.keys, kcn=2)

    def router(ti, ntok):
        R_ = lambda i, n=8: rt[:ntok, i, 0:n]
        rk = lambda i: ("rt", i)
        b = B.next_bank()
        for kc in range(8):
            B.mm(psum[b][:ntok, 0:36], h2Tf[:, kc, ti * 128:ti * 128 + ntok], wrt[:, kc, :], kc == 0, kc == 7,
                 r=h2Tf_k + ["wrt"], w=[("ps", b)])
        lg = rt[:ntok, 0, :]
        lgb = rt[:ntok, 1, :]
        B.copy(lg[:, 0:36] if False else rt[:ntok, 0:2, :].rearrange("p a b -> p (a b)")[:, 0:36], psum[b][:ntok, 0:36],
               r=[("ps", b)], w=[rk(0), rk(1)], eng="dve")
        lgf = rt[:ntok, 0:2, :].rearrange("p a b -> p (a b)")
        lbf = rt[:ntok, 2:4, :].rearrange("p a b -> p (a b)")
        B.tt(lbf[:, 0:36], lgf[:, 0:36], brt[:ntok, :], ALU.add, r=[rk(0), rk(1), "brt"], w=[rk(2), rk(3)])
        gl, glb = lgf[:, 0:4], lbf[:, 0:4]
        el = lgf[:, 4:36].rearrange("p (g e) -> p g e", e=8)
        elb = lbf[:, 4:36].rearrange("p (g e) -> p g e", e=8)
        sc = rt[:ntok, 4, :]
        sk = rk(4)
        B.red(sc[:, 0:1], gl, ALU.max, r=[rk(0)], w=[sk])
        B.ts(sc[:, 1:2], sc[:, 0:1], -1.0, None, ALU.mult, r=[sk], w=[sk])
        ex = rt[:ntok, 5, 0:4]
        B.act(ex, gl, AF.Exp, r=[rk(0), sk], w=[rk(5), sk], bias=sc[:, 1:2], accum_out=sc[:, 2:3])
        P.add("dve", lambda e: e.reciprocal(sc[:, 3:4], sc[:, 2:3]), r=[sk], w=[sk])
        B.red(sc[:, 4:5], glb, ALU.max, r=[rk(2)], w=[sk])
        ohg = rt[:ntok, 6, 0:4]
        B.ts(ohg, glb, sc[:, 4:5], None, ALU.is_equal, r=[rk(2), sk], w=[rk(6)])
        B.tt(ex, ex, ohg, ALU.mult, r=[rk(5), rk(6)], w=[rk(5)])
        B.red(sc[:, 5:6], ex, ALU.add, r=[rk(5)], w=[sk])
        B.ts(sc[:, 5:6], sc[:, 5:6], sc[:, 3:4], None, ALU.mult, r=[sk], w=[sk])
        ohg3 = ohg.unsqueeze(2).to_broadcast([ntok, 4, 8])
        tmp = rt[:ntok, 7, :].rearrange("p (g e) -> p g e", e=8)
        ing = rt[:ntok, 8, 0:8]
        B.tt(tmp, el, ohg3, ALU.mult, r=[rk(0), rk(1), rk(6)], w=[rk(7)])
        B.red(ing, tmp.rearrange("p g e -> p e g"), ALU.add, r=[rk(7)], w=[rk(8)])
        selb = rt[:ntok, 9, 0:8]
        B.tt(tmp, elb, ohg3, ALU.mult, r=[rk(2), rk(3), rk(6)], w=[rk(7)])
        B.red(selb, tmp.rearrange("p g e -> p e g"), ALU.add, r=[rk(7)], w=[rk(9)])
        oh1, oh2, sel2 = rt[:ntok, 10, 0:8], rt[:ntok, 11, 0:8], rt[:ntok, 12, 0:8]
        B.red(sc[:, 6:7], selb, ALU.max, r=[rk(9)], w=[sk])
        B.ts(oh1, selb, sc[:, 6:7], None, ALU.is_equal, r=[rk(9), sk], w=[rk(10)])
        B.stt(sel2, oh1, -1.0e30, selb, ALU.mult, ALU.add, r=[rk(10), rk(9)], w=[rk(12)])
        B.red(sc[:, 7:8], sel2, ALU.max, r=[rk(12)], w=[sk])
        B.ts(oh2, sel2, sc[:, 7:8], None, ALU.is_equal, r=[rk(12), sk], w=[rk(11)])
        t8 = rt[:ntok, 13, 0:8]
        B.tt(t8, oh1, ing, ALU.mult, r=[rk(10), rk(8)], w=[rk(13)])
        B.red(sc[:, 8:9], t8, ALU.add, r=[rk(13)], w=[sk])
        B.tt(t8, oh2, ing, ALU.mult, r=[rk(11), rk(8)], w=[rk(13)])
        B.red(sc[:, 9:10], t8, ALU.add, r=[rk(13)], w=[sk])
        B.tt(sc[:, 10:11], sc[:, 9:10], sc[:, 8:9], ALU.subtract, r=[sk], w=[sk])
        B.act(sc[:, 11:12], sc[:, 10:11], AF.Exp, r=[sk], w=[sk])
        B.ts(sc[:, 11:12], sc[:, 11:12], 1.0, None, ALU.add, r=[sk], w=[sk])
        P.add("dve", lambda e: e.reciprocal(sc[:, 11:12], sc[:, 11:12]), r=[sk], w=[sk])
        B.ts(sc[:, 12:13], sc[:, 11:12], -1.0, 1.0, ALU.mult, ALU.add, r=[sk], w=[sk])
        wsel = rt[:ntok, 14, 0:8]
        B.ts(wsel, oh1, sc[:, 11:12], None, ALU.mult, r=[rk(10), sk], w=[rk(14)])
        B.stt(wsel, oh2, sc[:, 12:13], wsel, ALU.mult, ALU.add, r=[rk(11), rk(14), sk], w=[rk(14)])
        B.ts(wsel, wsel, sc[:, 5:6], None, ALU.mult, r=[rk(14), sk], w=[rk(14)])
        B.tt(gates[:ntok, ti, :].rearrange("p (g e) -> p g e", e=8), ohg3, wsel.unsqueeze(1).to_broadcast([ntok, 4, 8]),
             ALU.mult, r=[rk(6), rk(14)], w=["gates"])

    def phaseC(row0, ntile, ntok, odram, orow0):
        N = (ntile - 1) * 128 + ntok
        B.bank_lim = 4
        B.psn = 0
        for e in range(3):
            load_expert(e)
        for ti in range(ntile):
            B.dma(yacc[:ntok, ti, :], sc_h2[row0 + ti * 128: row0 + ti * 128 + ntok, :],
                  r=[("sc_h2", (row0 + ti * 128) // 128)], w=yacc_k)
        for ti in range(ntile):
            transpose_tile(yacc[:, ti, :], yacc_k[0], h2Tf, h2Tf_k[0], D, tok0=ti * 128, ntok=ntok)
        B.copy(h2Tb[:, :, 0:N], h2Tf[:, :, 0:N], r=h2Tf_k, w=h2Tb_k, eng="dve")
        for ti in range(ntile):
            router(ti, ntok)
        P.add("act", lambda e: e.mul(yacc[:ntok, 0:ntile, :], yacc[:ntok, 0:ntile, :], float(ALPHA)), r=yacc_k + h2Tf_k, w=yacc_k)
        for e in range(32):
            if e + 3 < 32:
                load_expert(e + 3)
            g_, u_, d_ = EW[e % 4]
            for f in range(2):
                bg, bu = 2 * f, 2 * f + 1
                for kc in range(8):
                    B.mm(psum[bg][:, 0:N], g_.ap[:, kc, f * 128:(f + 1) * 128], h2Tb[:, kc, 0:N], kc == 0, kc == 7,
                         r=h2Tb_k + g_.keys, w=[("ps", bg)])
                for kc in range(8):
                    B.mm(psum[bu][:, 0:N], u_.ap[:, kc, f * 128:(f + 1) * 128], h2Tb[:, kc, 0:N], kc == 0, kc == 7,
                         r=h2Tb_k + u_.keys, w=[("ps", bu)])
                sgt, sgk = (zqk, "zqk") if f == 0 else (sr, "sr")
                B.act(sgt[:, 0:N], psum[bg][:, 0:N], AF.Silu, r=[("ps", bg)], w=[sgk])
                B.tt(hT[:, f, 0:N], sgt[:, 0:N], psum[bu][:, 0:N], ALU.mult, r=[sgk, ("ps", bu)], w=hT_k)
            for ti in range(ntile):
                for hf in range(2):
                    b = 4 + (B.next_bank())
                    for f in range(2):
                        B.mm(psum[b][:ntok, :], hT[:, f, ti * 128:ti * 128 + ntok], d_.ap[:, f, hf * 512:(hf + 1) * 512],
                             f == 0, f == 1, r=hT_k + d_.keys, w=[("ps", b)])
                    B.stt(yacc[:ntok, ti, hf * 512:(hf + 1) * 512], psum[b][:ntok, :], gates[:ntok, ti, e:e + 1],
                          yacc[:ntok, ti, hf * 512:(hf + 1) * 512], ALU.mult, ALU.add,
                          r=[("ps", b), "gates"] + yacc_k, w=yacc_k)
        for ti in range(ntile):
            yo = yout[ti % 2]
            pre_t = _Tile(yacc, ti, ntok)
            layer_norm(pre_t, yacc_k[0], 0, _Tile2(yo, ntok), yout_k[0], ntok=ntok)
            B.dma(odram[orow0 + ti * 128: orow0 + ti * 128 + ntok, :], yo[:ntok, :], r=yout_k, w=[("o_y", row0, ti)])
        B.bank_lim = 8

    if "C" in stages:
        barrier(SK + kT_keys + V_keys + yacc_k + h2Tf_k + h2Tb_k + hT_k + yout_k
                + ["e1", "l1", "eb", "enb", "ekb", "wrt", "brt", "gates"] + [("rt", k_) for k_ in range(16)])
        B.dma(wrt[:], w_rt.rearrange("(kc p) n -> p kc n", p=128), w=["wrt"])
        B.dma(brt[:], _bc(b_rt, 36), w=["brt"])
        B.dma(lnp[0][:], _bc(ln_d[4], D), w=[("lnp", 0)])
        B.dma(lnp[1][:], _bc(ln_d[5], D), w=[("lnp", 1)])
        if "A" in stages:
            for blk in range(NT // 512):
                phaseC(blk * 512, 4, 128, o_y, blk * 512)
        if "S" in stages:
            phaseC(NT, 1, NS, o_ys, 0)

    if "dbg_h2" in stages:
        o_h2 = B.dout("o_h2", [NT, D])
        for tt_ in range(NT // 128):
            i = cnt["x"] % 2
            cnt["x"] += 1
            B.dma(xin[i][:], sc_h2[tt_ * 128:(tt_ + 1) * 128, :], r=[("sc_h2", tt_)], w=[("xin", i)])
            B.dma(o_h2[tt_ * 128:(tt_ + 1) * 128, :], xin[i][:], r=[("xin", i)], w=[("o_h2", tt_)])

    P.emit(nc, B.es)
    B.es.close()
    return nc


def make_consts():
    j = np.arange(128)[:, None]
    i = np.arange(128)[None, :]
    inv = (10000.0 ** (-np.arange(0, 32, 2, dtype=np.float32) / 32.0)).astype(np.float32)
    pos = (np.arange(16)[None, :] * 128 + np.arange(128)[:, None]).astype(np.float32)
    ang = pos[:, :, None] * inv[None, None, :]
    ang_s = (np.float32(8192.0) * inv)[None, :].repeat(NS, 0)
    return {
        "c_pcol": np.arange(128, dtype=np.float32).reshape(128, 1),
        "c_i16": np.tile(np.eye(16, dtype=np.float32).reshape(1, 256), (64, 1)),
        "c_cos_s": np.cos(ang_s).astype(np.float32),
        "c_sin_s": np.sin(ang_s).astype(np.float32),
        "c_bm8": (np.arange(512)[None, :] // 64 == np.arange(8)[:, None]).astype(np.float32),
        "c_bm4": (np.arange(1024)[None, :] // 256 == np.arange(4)[:, None]).astype(np.float32),
        "ident": np.eye(128, dtype=np.float32),
        "c_us": np.where(j <= i, -1.0 / 16.0, 0.0).astype(np.float32),
        "c_lw": np.where(j > i, -1.0 / 16.0, 0.0).astype(np.float32),
        "c_mask": (j <= i).astype(np.float32),
        "c_cos": np.cos(ang).astype(np.float32),
        "c_sin": np.sin(ang).astype(np.float32),
    }


_NC_CACHE = {}


def _program():
    if "nc" not in _NC_CACHE:
        _NC_CACHE["nc"] = build(("memkv", "A", "B", "S", "C"))
    return _NC_CACHE["nc"]


def kernel(**inputs):
    f32 = lambda a: np.ascontiguousarray(np.asarray(a, dtype=np.float32))
    g = {k: np.asarray(v) for k, v in inputs.items()}
    consts = make_consts()
    shared = dict(consts)
    shared["w_mk"] = f32(g["w_mk"][0])
    shared["w_mv"] = f32(g["w_mv"][0])
    shared["w_in"] = f32(g["w_in"][0])
    shared["w_gate_aug"] = f32(np.concatenate([g["w_gla_gate"][0], g["b_gla_gate"]], axis=0))
    shared["gla_norm_g"] = f32(g["gla_norm_g"])
    shared["mla_q_norm_g"] = f32(g["mla_q_norm_g"])
    shared["mla_kv_norm_g"] = f32(g["mla_kv_norm_g"])
    shared["w_uq"] = f32(g["w_uq"][0].reshape(384, 768))
    shared["w_uk"] = f32(g["w_uk"][0].reshape(256, 512))
    shared["w_uv"] = f32(g["w_uv"][0].reshape(256, 512))
    shared["w_out"] = f32(g["w_out"][0])
    shared["w_xq"] = f32(g["w_xq"][0])
    shared["w_xo"] = f32(g["w_xo"][0])
    for i, k in enumerate(("ln1_g", "ln1_b", "ln2_g", "ln2_b", "ln3_g", "ln3_b")):
        shared["ln%d" % i] = f32(g[k])
    shared["w_rt"] = f32(np.concatenate([g["w_grp"][0], g["w_rtr"][0]], axis=1))
    shared["b_rt"] = f32(np.concatenate([g["b_grp"], g["b_rtr"]], axis=1))
    shared["w_e_gate"] = f32(g["w_e_gate"][0])
    shared["w_e_up"] = f32(g["w_e_up"][0])
    shared["w_e_down"] = f32(g["w_e_down"][0])
    shared["cache_lat"] = f32(g["cache_latent"][0].reshape(-1, 256))
    shared["cache_kr"] = f32(g["cache_krope"][0].reshape(-1, 32))
    in_maps = []
    for c in range(NCORES):
        m = dict(shared)
        m["xp"] = f32(g["x_prompt"][NSEQ * c:NSEQ * (c + 1)].reshape(NSEQ * SEQ, D))
        m["memp"] = f32(g["mem_prompt"][NSEQ * c:NSEQ * (c + 1)].reshape(NSEQ * 256, D))
        m["xs"] = f32(g["x_sample"][NS * c:NS * (c + 1), 0])
        m["sgla_s"] = f32(g["state_gla"][0, NS * c:NS * (c + 1)])
        m["cmk"] = f32(g["cache_mem_k"][0, NS * c:NS * (c + 1)].reshape(NS, 256, D))
        m["cmv"] = f32(g["cache_mem_v"][0, NS * c:NS * (c + 1)].reshape(NS, 256, D))
        m["pt"] = np.ascontiguousarray(g["page_table"][NS * c:NS * (c + 1)].reshape(1, NS * NPAGE).astype(np.int32))
        in_maps.append(m)
    nc = _program()
    res = run_bass_kernel_spmd(nc, in_maps, core_ids=list(range(NCORES)))
    R = res.results
    cat = lambda name: np.concatenate([np.asarray(R[c][name]) for c in range(NCORES)], axis=0)
    y_prompt = cat("o_y").reshape(16, SEQ, D)
    y_sample = cat("o_ys").reshape(128, 1, D)
    sgla_p = cat("o_sgla").reshape(1, 16, 4, 64, 128)
    lat_p = cat("o_lat").reshape(1, 16, SEQ, 256)
    kr_p = cat("o_kr").reshape(1, 16, SEQ, 32)
    mk_p = cat("o_memk").reshape(1, 16, 256, 4, 256)
    mv_p = cat("o_memv").reshape(1, 16, 256, 4, 256)
    sgla_s = cat("o_sgla_s").reshape(1, 128, 4, 64, 128)
    lat_s = cat("o_lat_s").reshape(1, 128, 1, 256)
    kr_s = cat("o_kr_s").reshape(1, 128, 1, 32)
    outs = (y_prompt, y_sample, sgla_p, lat_p, kr_p, mk_p, mv_p, sgla_s, lat_s, kr_s)
    return tuple(np.ascontiguousarray(o.astype(np.float32)) for o in outs)
```

```python
import numpy as np
from contextlib import ExitStack
import concourse.bass as bass
import concourse.mybir as mybir
from concourse.bass_utils import run_bass_kernel_spmd

F32 = mybir.dt.float32
BF16 = mybir.dt.bfloat16
I32 = mybir.dt.int32
AF = mybir.ActivationFunctionType
ALU = mybir.AluOpType
AX = mybir.AxisListType

NCORES = 8
D = 1024
SEQ = 2048
NSEQ = 2
NS = 16
NPAGE = 64
ALPHA = 2.0 ** 0.25
LN_EPS = 1e-5
RMS_EPS = 1e-6
MLA_SCALE = 96.0 ** -0.5


class _Op:
    __slots__ = ("idx", "eng", "fn", "deps", "dma", "signal", "sem", "val", "prev")

    def __init__(self, idx, eng, fn, dma):
        self.idx, self.eng, self.fn, self.dma = idx, eng, fn, dma
        self.deps = set()
        self.signal = False
        self.sem = None
        self.val = 0
        self.prev = 0


class Prog:
    ENGS = ("pe", "act", "dve", "pool", "sp")
    NDS = 48

    def __init__(self):
        self.ops = []
        self.lastw = {}
        self.readers = {}

    def add(self, eng, fn, r=(), w=(), dma=False):
        op = _Op(len(self.ops), eng, fn, dma)
        xs = [k for k in r if isinstance(k, tuple) and k and k[0] == "ps"]
        if xs:
            r = [k for k in r if k not in xs]
            w = list(w) + [k for k in xs if k not in w]
        for k in r:
            lw = self.lastw.get(k)
            if lw is not None:
                op.deps.add(lw)
        for k in w:
            lw = self.lastw.get(k)
            if lw is not None:
                op.deps.add(lw)
            rs = self.readers.get(k)
            if rs:
                op.deps.update(rs)
        for k in r:
            self.readers.setdefault(k, []).append(op.idx)
        for k in w:
            self.lastw[k] = op.idx
            self.readers[k] = []
        op.deps.discard(op.idx)
        self.ops.append(op)
        return op

    def emit(self, nc, es):
        ops = self.ops
        esem = {e: es.enter_context(nc.semaphore("se_" + e)) for e in self.ENGS}
        dsem = [es.enter_context(nc.semaphore("sd_%d" % i)) for i in range(self.NDS)]
        qrange = {"sp": (0, self.NDS - 12), "act": (0, self.NDS - 12), "pool": (self.NDS - 12, self.NDS)}
        qnext = {q: lo for q, (lo, hi) in qrange.items()}

        def pruned(op, dop):
            return (not op.dma) and (not dop.dma) and op.eng == "pe" and dop.eng == "pe"

        for op in ops:
            for d in op.deps:
                if not pruned(op, ops[d]):
                    ops[d].signal = True
        cnt = {e: 0 for e in self.ENGS}
        dcnt = [0] * self.NDS
        dn = 0
        for op in ops:
            if op.dma:
                qk = "pool" if op.eng == "pool" else "sp"
                dn = qnext[qk]
                lo, hi = qrange[qk]
                qnext[qk] = lo + (dn + 1 - lo) % (hi - lo)
                op.sem = dsem[dn]
                op.prev = dcnt[dn]
                dcnt[dn] += 16
                op.val = dcnt[dn]
            elif op.signal:
                cnt[op.eng] += 1
                op.sem = esem[op.eng]
                op.val = cnt[op.eng]
        cuts = sorted(set([0] + [c for c in getattr(self, "cuts", []) if 0 < c < len(ops)] + [len(ops)]))
        waited = {e: {} for e in self.ENGS}

        def body_for(e, lo, hi, final):
            def body(engh):
                wd = waited[e]
                for op in ops[lo:hi]:
                    if op.eng != e:
                        continue
                    needs = {}
                    for d in op.deps:
                        dop = ops[d]
                        if pruned(op, dop):
                            continue
                        key = id(dop.sem)
                        if key not in needs or needs[key][1] < dop.val:
                            needs[key] = (dop.sem, dop.val)
                    if op.dma and op.prev > 0:
                        key = id(op.sem)
                        if key not in needs or needs[key][1] < op.prev:
                            needs[key] = (op.sem, op.prev)
                    for key, (sem, val) in needs.items():
                        if wd.get(key, 0) < val:
                            engh.wait_ge(sem, val)
                            wd[key] = val
                    ins = op.fn(engh)
                    if op.dma:
                        ins.then_inc(op.sem, 16)
                    elif op.signal:
                        ins.then_inc(op.sem, 1)
                if e == "sp" and final:
                    for i in range(self.NDS):
                        if dcnt[i] > 0:
                            engh.wait_ge(dsem[i], dcnt[i])
            return body

        for ci in range(len(cuts) - 1):
            lo, hi = cuts[ci], cuts[ci + 1]
            final = ci == len(cuts) - 2
            with nc.Block() as block:
                block.tensor(body_for("pe", lo, hi, final))
                block.scalar(body_for("act", lo, hi, final))
                block.vector(body_for("dve", lo, hi, final))
                block.gpsimd(body_for("pool", lo, hi, final))
                block.sync(body_for("sp", lo, hi, final))


class Builder:
    def __init__(self, stages):
        self.stages = stages
        self.nc = bass.Bass("TRN2", target_bir_lowering=False)
        self.es = ExitStack()
        self.P = Prog()
        self.psn = 0
        self.bank_lim = 8
        self.ptn = 0
        self.ev = 0

    def din(self, name, shape, dt=F32):
        return self.nc.dram_tensor(name, list(shape), dt, kind="ExternalInput").ap()

    def dout(self, name, shape, dt=F32):
        return self.nc.dram_tensor(name, list(shape), dt, kind="ExternalOutput").ap()

    def sb(self, name, shape, dt):
        return self.es.enter_context(self.nc.sbuf_tensor("sb_" + name, list(shape), dt))

    def next_bank(self):
        b = self.psn % self.bank_lim
        self.psn = (b + 1) % self.bank_lim
        return b

    def next_tbank(self):
        b = self.ptn
        self.ptn = (self.ptn + 1) % 2
        return b

    def dma(self, out, in_, r=(), w=(), q="sp"):
        self.P.add(q, lambda e: e.dma_start(out=out, in_=in_), r=r, w=w, dma=True)

    def mm(self, out, lhsT, rhs, start, stop, r=(), w=()):
        self.P.add("pe", lambda e: e.matmul(out, lhsT, rhs, start=start, stop=stop), r=r, w=w)

    def tr(self, out, in_, ident, r=(), w=()):
        self.P.add("pe", lambda e: e.transpose(out, in_, ident), r=r, w=w)

    def act(self, out, in_, func, r=(), w=(), **kw):
        self.P.add("act", lambda e: e.activation(out, in_, func, **kw), r=r, w=w)

    def copy(self, out, in_, r=(), w=(), eng=None):
        if eng is None:
            eng = ("dve", "act")[self.ev % 2]
            self.ev += 1
        if eng == "act":
            self.P.add("act", lambda e: e.copy(out, in_), r=r, w=w)
        else:
            self.P.add(eng, lambda e: e.tensor_copy(out, in_), r=r, w=w)

    def tt(self, out, in0, in1, op, r=(), w=(), eng="dve"):
        self.P.add(eng, lambda e: e.tensor_tensor(out, in0, in1, op), r=r, w=w)

    def ts(self, out, in0, s1, s2, op0, op1=ALU.bypass, r=(), w=(), eng="dve"):
        if s2 is None:
            self.P.add(eng, lambda e: e.tensor_scalar(out, in0, s1, None, op0), r=r, w=w)
        else:
            self.P.add(eng, lambda e: e.tensor_scalar(out, in0, s1, s2, op0, op1), r=r, w=w)

    def stt(self, out, in0, scalar, in1, op0, op1, r=(), w=()):
        self.P.add("dve", lambda e: e.scalar_tensor_tensor(out, in0, scalar, in1, op0, op1), r=r, w=w)

    def rsqrt(self, out, in_, c, tmp, r=(), w=(), tk="rsq_tmp"):
        self.P.add("act", lambda e: e.activation(tmp, in_, AF.Sqrt, bias=float(c)), r=r, w=[tk])
        self.P.add("dve", lambda e: e.reciprocal(out, tmp), r=[tk], w=w)

    def red(self, out, in_, op, r=(), w=()):
        self.P.add("dve", lambda e: e.tensor_reduce(out, in_, AX.X, op), r=r, w=w)


def _bc(ap, n):
    return bass.AP(ap.tensor, ap.offset, [[0, 128], [1, n]])


class AV:
    CH = 2048

    def __init__(self, arena, off, shape):
        n = int(np.prod(shape))
        a = arena[:, off:off + n]
        if len(shape) == 2:
            a = a.rearrange("p (a b) -> p a b", b=shape[1])
        self.ap = a
        self.keys = [("W", c) for c in range(off // self.CH, (off + n - 1) // self.CH + 1)]


C_Q, C_K, C_V, C_A, C_R, C_CQ, C_CKV, C_KR = 0, 256, 512, 1024, 1040, 1552, 1936, 2192


def build(stages=("memkv", "A")):
    B = Builder(stages)
    nc, P = B.nc, B.P
    NT = NSEQ * SEQ

    xp = B.din("xp", [NT, D])
    memp = B.din("memp", [NSEQ * 256, D])
    ident_d = B.din("ident", [128, 128])
    us_d = B.din("c_us", [128, 128])
    lw_d = B.din("c_lw", [128, 128])
    mask_d = B.din("c_mask", [128, 128])
    cos_d = B.din("c_cos", [128, 16, 16])
    sin_d = B.din("c_sin", [128, 16, 16])
    w_mk = B.din("w_mk", [D, D])
    w_mv = B.din("w_mv", [D, D])
    w_in = B.din("w_in", [D, 2224])
    wgate_d = B.din("w_gate_aug", [17, 256])
    ggla_d = B.din("gla_norm_g", [1, 128])
    gq_d = B.din("mla_q_norm_g", [1, 384])
    gkv_d = B.din("mla_kv_norm_g", [1, 256])
    w_uq = B.din("w_uq", [384, 768])
    w_uk = B.din("w_uk", [256, 512])
    w_uv = B.din("w_uv", [256, 512])
    w_out = B.din("w_out", [D, D])
    w_xq = B.din("w_xq", [D, D])
    w_xo = B.din("w_xo", [D, D])
    ln_d = [B.din("ln%d" % i, [1, D]) for i in range(6)]
    sc_h2 = nc.dram_tensor("sc_h2", [NT + NS, D], F32).ap()
    w_rt = B.din("w_rt", [D, 36])
    b_rt = B.din("b_rt", [1, 36])
    w_eg = B.din("w_e_gate", [32, D, 256])
    w_eu = B.din("w_e_up", [32, D, 256])
    w_ed = B.din("w_e_down", [32, 256, D])
    xs = B.din("xs", [NS, D])
    sgla_s = B.din("sgla_s", [NS, 4, 64, 128])
    NPHYS = 10240
    c_lat = B.din("cache_lat", [NPHYS * 128, 256])
    c_kr = B.din("cache_kr", [NPHYS * 128, 32])
    cmk = B.din("cmk", [NS, 256, D])
    cmv = B.din("cmv", [NS, 256, D])
    pt_d = B.din("pt", [1, NS * NPAGE], I32)
    pcol_d = B.din("c_pcol", [128, 1])
    i16_d = B.din("c_i16", [64, 256])
    coss_d = B.din("c_cos_s", [NS, 16])
    sins_d = B.din("c_sin_s", [NS, 16])
    bm8_d = B.din("c_bm8", [8, 512])
    bm4_d = B.din("c_bm4", [4, 1024])
    r8_d = B.din("c_r8", [16, 128])
    sc_lat = nc.dram_tensor("sc_lat", [NS * NPAGE * 128, 256], F32).ap()
    sc_kr = nc.dram_tensor("sc_kr", [NS * NPAGE * 128, 32], F32).ap()
    o_sgla_s = B.dout("o_sgla_s", [NS, 4, 64, 128])
    o_lat_s = B.dout("o_lat_s", [NS, 256])
    o_kr_s = B.dout("o_kr_s", [NS, 32])
    o_y = B.dout("o_y", [NT, D])
    o_ys = B.dout("o_ys", [NS, D])
    o_memk = B.dout("o_memk", [NSEQ * 256, D])
    o_memv = B.dout("o_memv", [NSEQ * 256, D])
    o_lat = B.dout("o_lat", [NT, 256])
    o_kr = B.dout("o_kr", [NT, 32])
    o_sgla = B.dout("o_sgla", [NSEQ, 4, 64, 128])
    sc_q = nc.dram_tensor("sc_q", [NT, 768], BF16).ap()
    sc_go = nc.dram_tensor("sc_go", [NT, 512], BF16).ap()

    psum = [B.es.enter_context(nc.psum_tensor("ps%d" % i, [128, 512], F32)) for i in range(8)]
    ident = B.sb("ident", [128, 128], F32)
    identb = B.sb("identb", [128, 128], BF16)
    c_us = B.sb("c_us", [128, 128], F32)
    c_lw = B.sb("c_lw", [128, 128], F32)
    c_mask = B.sb("c_mask", [128, 128], F32)
    cosT = B.sb("cosT", [128, 16, 16], F32)
    sinT = B.sb("sinT", [128, 16, 16], F32)
    arena = B.sb("arena", [128, 24576], BF16)
    WmkA = AV(arena, 0, [8, 1024])
    WmvA = AV(arena, 8192, [8, 1024])
    wA, wB = WmkA.ap, WmvA.ap
    xin = [B.sb("xin%d" % i, [128, D], F32) for i in range(2)]
    xT = [B.sb("xT%d" % i, [128, 8, 128], BF16) for i in range(2)]
    stage = [B.sb("stage%d" % i, [128, D], F32) for i in range(2)]
    memT = B.sb("memT", [128, 8, 256], BF16)
    memkT = B.sb("memkT", [128, NSEQ, 8, 256], BF16)
    memv = B.sb("memv", [128, NSEQ, 2, 4, 260], BF16)
    arenaS = B.sb("arenaS", [128, 12800], F32)

    def sview(off, n, dt=F32, parts=128):
        a = arenaS[0:parts, off:off + n]
        if dt != F32:
            a = a.bitcast(dt)
        return a, [("S", c) for c in range(off // 1024, (off + n - 1) // 1024 + 1)]
    kT_flat, kT_keys = sview(0, 8192, BF16, 96)
    kT_seq = kT_flat.rearrange("p (h t) -> p h t", t=SEQ)
    V_flat, V_keys = sview(8192, 4224, BF16)
    V_seq = V_flat.rearrange("p (a h d) -> p a h d", h=8, d=66)
    Sst = B.sb("Sst", [64, 4, 128], F32)
    Sbf = B.sb("Sbf", [64, 4, 128], BF16)
    wgate = B.sb("wgate", [17, 256], F32)
    ggla = B.sb("ggla", [128, 128], F32)
    gq = B.sb("gq", [128, 384], F32)
    gkv = B.sb("gkv", [128, 256], F32)
    aT = B.sb("aT", [32, 128], F32)
    zqk = B.sb("zqk", [128, 512], F32)
    vbf = B.sb("vbf", [128, 512], BF16)
    sr = B.sb("sr", [128, 512], F32)
    cq = B.sb("cq", [128, 384], F32)
    ze = B.sb("ze", [128, 288], F32)
    gtmp = B.sb("gtmp", [128, 5, 256], F32)
    e1, l1, eb, enb, ekb = [gtmp[:, k_, :] for k_ in range(5)]
    eblT = B.sb("eblT", [64, 4], F32)
    qt = B.sb("qt", [128, 256], BF16)
    kt = B.sb("kt", [128, 256], BF16)
    kp = B.sb("kp", [128, 256], BF16)
    qkT = B.sb("qkT", [64, 8, 128], BF16)
    ATm = B.sb("ATm", [128, 4, 128], BF16)
    osb = B.sb("osb", [128, 4, 128], F32)
    junk = B.sb("junk", [128, 512], F32)
    ss4 = B.sb("ss4", [128, 4], F32)
    rs4 = B.sb("rs4", [128, 4], F32)
    big1 = B.sb("big1", [128, 1024], F32)
    t1 = big1[:, 0:512].rearrange("p (a b) -> p a b", b=128)
    SG = big1[:, 512:1024].rearrange("p (a b) -> p a b", b=128)
    gobf = B.sb("gobf", [128, 512], BF16)
    rsq = B.sb("rsq", [128, 8], F32)
    sskv = B.sb("sskv", [128, 2], F32)
    rskv = B.sb("rskv", [128, 2], F32)
    ckvn = B.sb("ckvn", [128, 256], F32)
    ckvT = B.sb("ckvT", [128, 2, 128], BF16)
    cqn = B.sb("cqn", [128, 384], F32)
    cqnT = B.sb("cqnT", [128, 3, 128], BF16)
    qtm = B.sb("qtm", [128, 8, 96], F32)
    qbf = B.sb("qbf", [128, 768], BF16)
    kpe = B.sb("kpe", [128, 96], F32)
    rp = [B.sb("rp%d" % i, [128, 8, 16], F32) for i in range(4)]

    lnp = [B.sb("lnp%d" % i, [128, D], F32) for i in range(4)]
    maskb = B.sb("maskb", [128, 128], BF16)
    rec8 = B.sb("rec8", [128, 8], F32)
    lnst = B.sb("lnst", [128, 2, 6], F32)
    lnmv = B.sb("lnmv", [128, 2], F32)
    lnrs = B.sb("lnrs", [128, 2], F32)
    r8 = B.sb("r8", [16, 128], F32)
    ptq = B.sb("ptq", [16, 64], I32)
    ptqf = B.sb("ptqf", [16, 64], F32)
    idxq = B.sb("idxq", [128, 64], I32)
    idxk = B.sb("idxk", [64, 16], I32)
    pcol = B.sb("pcol", [128, 1], F32)
    i16 = B.sb("i16", [64, 16, 16], F32)
    cos_s = B.sb("cos_s", [NS, 16], F32)
    sin_s = B.sb("sin_s", [NS, 16], F32)
    bm8 = B.sb("bm8", [8, 512], F32)
    bm4 = memkT[:].rearrange("p a b c -> p (a b c)")[0:4, 0:2048].bitcast(F32)
    rt = gtmp[:, 0:2, :].rearrange("p a (b c) -> p (a b) c", c=32)
    wrt = gtmp[:, 2:4, :].rearrange("p a b -> p (a b)")[:, 0:288].rearrange("p (k n) -> p k n", n=36)
    gates = gtmp[:, 4, 0:128].rearrange("p (t e) -> p t e", e=32)
    brt = gtmp[:, 4, 128:164]
    B.dma(ident[:], ident_d, w=["ident"])
    B.dma(c_us[:], us_d, w=["c_us"])
    B.dma(c_lw[:], lw_d, w=["c_lw"])
    B.dma(c_mask[:], mask_d, w=["c_mask"])
    B.dma(cosT[:], cos_d, w=["cosT"])
    B.dma(sinT[:], sin_d, w=["sinT"])
    B.dma(wgate[:], wgate_d, w=["wgate"])
    B.dma(ggla[:], _bc(ggla_d, 128), w=["ggla"])
    B.dma(gq[:], _bc(gq_d, 384), w=["gq"])
    B.dma(gkv[:], _bc(gkv_d, 256), w=["gkv"])
    B.dma(pcol[:], pcol_d, w=["pcol"])
    B.dma(i16[:].rearrange("p a b -> p (a b)"), i16_d, w=["i16"])
    B.dma(cos_s[:], coss_d, w=["cos_s"])
    B.dma(sin_s[:], sins_d, w=["sin_s"])
    B.dma(bm8[:], bm8_d, w=["bm8"])
    B.copy(identb[:], ident[:], r=["ident"], w=["identb"], eng="dve")
    B.copy(maskb[:], c_mask[:], r=["c_mask"], w=["maskb"], eng="dve")
    for i in range(4):
        B.dma(lnp[i][:], _bc(ln_d[i], D), w=[("lnp", i)])
    P.add("act", lambda e: e.mul(ggla[:], ggla[:], float(128.0 ** 0.5)), r=["ggla"], w=["ggla"])
    P.add("act", lambda e: e.mul(gq[:], gq[:], float(384.0 ** 0.5)), r=["gq"], w=["gq"])
    P.add("act", lambda e: e.mul(gkv[:], gkv[:], float(256.0 ** 0.5)), r=["gkv"], w=["gkv"])
    P.add("dve", lambda e: e.memset(aT[:], 1.0), w=["aT"])
    P.add("dve", lambda e: e.memset(kpe[:], 0.0), w=["kpe"])

    def load_w(dst, src, keys, q="pool", kcn=8, step=4):
        v = src.rearrange("(kc p) n -> p kc n", p=128)
        for kc0 in range(0, kcn, step):
            kc1 = min(kcn, kc0 + step)
            B.dma(dst[:, kc0:kc1, :], v[:, kc0:kc1, :], w=keys, q=q)

    cnt = {"x": 0, "st": 0}

    def transpose_tile(src_tile, src_key, dstT, dst_key, ncols, tok0=0, ntok=128):
        nchunk = ncols // 128
        for g0 in range(0, nchunk, 4):
            g1 = min(nchunk, g0 + 4)
            b = B.next_bank()
            for c in range(g0, g1):
                B.tr(psum[b][:, (c - g0) * 128:(c - g0) * 128 + ntok],
                     src_tile[:ntok, c * 128:(c + 1) * 128], ident[:ntok, :ntok],
                     r=[src_key, "ident"], w=[("ps", b)])
            pv = psum[b][:, 0:(g1 - g0) * 128].rearrange("p (c t) -> p c t", t=128)
            B.copy(dstT[:, g0:g1, tok0:tok0 + ntok], pv[:, :, :ntok], r=[("ps", b)], w=[dst_key])


    def barrier(keys):
        P.add("dve", lambda e: e.memset(rsq[:, 7:8], 0.0), r=[], w=list(keys) + ["rsq_bar"])

    def pregather():
        P.add("sp", lambda e: e.dma_start(out=ptq[:], in_=bass.AP(pt_d.tensor, pt_d.offset, [[1, 16], [16, 64]]),
                                          allow_slow_non_contiguous=True), w=["ptq"], dma=True)
        P.add("sp", lambda e: e.dma_start(out=idxk[:], in_=bass.AP(pt_d.tensor, pt_d.offset, [[1, 64], [64, 16]]),
                                          allow_slow_non_contiguous=True), w=["idxk"], dma=True)
        B.dma(r8[:], r8_d, w=["r8"])
        B.copy(ptqf[:], ptq[:], r=["ptq"], w=["ptqf"], eng="dve")
        B.mm(psum[0][:, 0:64], r8[:], ptqf[:], True, True, r=["r8", "ptqf"], w=[("ps", 0)])
        B.ts(idxq[:], psum[0][:, 0:64], 8.0, pcol[:, 0:1], ALU.mult, ALU.add, r=[("ps", 0), "pcol"], w=["idxq"])
        LB = [arenaS[:, 0:4096], arenaS[:, 4096:8192]]
        KB = arenaS[0:64, 8192:12288]
        lat8 = c_lat.rearrange("(n t) r -> n (t r)", t=16)
        kr1 = c_kr.rearrange("(n t) d -> n (t d)", t=128)
        k_ = 0
        for bb in range(NS):
            for g_ in range(4):
                sl = k_ % 2
                k_ += 1
                col = bb * 4 + g_
                P.add("pool", lambda e, sl=sl, col=col: e.indirect_dma_start(
                    out=LB[sl], out_offset=None, in_=lat8,
                    in_offset=bass.IndirectOffsetOnAxis(ap=idxq[:, col:col + 1], axis=0)), r=["idxq"], w=[("pre_LB", sl)], dma=True)
                row0 = (bb * NPAGE + g_ * 16) * 128
                B.dma(sc_lat[row0:row0 + 2048, :].rearrange("(p t) r -> p (t r)", t=16), LB[sl], r=[("pre_LB", sl)], w=[("sc_lat", bb)])
            P.add("pool", lambda e, bb=bb: e.indirect_dma_start(
                out=KB, out_offset=None, in_=kr1,
                in_offset=bass.IndirectOffsetOnAxis(ap=idxk[:, bb:bb + 1], axis=0)), r=["idxk"], w=["pre_KB"], dma=True)
            row0 = bb * NPAGE * 128
            B.dma(sc_kr[row0:row0 + NPAGE * 128, :].rearrange("(p t) d -> p (t d)", t=128), KB, r=["pre_KB"], w=[("sc_kr", bb)])
        barrier([("pre_LB", 0), ("pre_LB", 1), "pre_KB"] + kT_keys + V_keys)

    if "S" in stages:
        pregather()

    if "memkv" in stages:
        P.add("dve", lambda e: e.memset(memv[:].rearrange("p a b c d -> p (a b c d)"), 1.0), w=["memv"])
        load_w(wA, w_mk, WmkA.keys)
        load_w(wB, w_mv, WmvA.keys)
        for s in range(NSEQ):
            for mt in range(2):
                i = cnt["x"] % 2
                cnt["x"] += 1
                B.dma(xin[i][:], memp[s * 256 + mt * 128: s * 256 + (mt + 1) * 128, :], w=[("xin", i)])
                transpose_tile(xin[i], ("xin", i), memT, "memT", D, tok0=mt * 128)
            for wi, (wt, wkey, odram) in enumerate(((wA, WmkA.keys, o_memk), (wB, WmvA.keys, o_memv))):
                for mt in range(2):
                    si = cnt["st"] % 2
                    cnt["st"] += 1
                    for half in range(2):
                        b = B.next_bank()
                        for kc in range(8):
                            B.mm(psum[b][:, :], memT[:, kc, mt * 128:(mt + 1) * 128],
                                 wt[:, kc, half * 512:(half + 1) * 512], kc == 0, kc == 7,
                                 r=["memT"] + wkey, w=[("ps", b)])
                        B.copy(stage[si][:, half * 512:(half + 1) * 512], psum[b][:, :],
                               r=[("ps", b)], w=[("stage", si)])
                        if wi == 1:
                            B.copy(memv[:, s, mt, half * 2:half * 2 + 2, 0:256],
                                   psum[b][:, :].rearrange("p (h d) -> p h d", d=256),
                                   r=[("ps", b)], w=["memv"])
                    B.dma(odram[s * 256 + mt * 128: s * 256 + (mt + 1) * 128, :], stage[si][:],
                          r=[("stage", si)], w=[("o", wi, s, mt)])
            for j in range(8):
                b = B.next_bank()
                for kc in range(8):
                    B.mm(psum[b][:, 0:256], wA[:, kc, j * 128:(j + 1) * 128], memT[:, kc, :],
                         kc == 0, kc == 7, r=["memT"] + WmkA.keys, w=[("ps", b)])
                B.copy(memkT[:, s, j, :], psum[b][:, 0:256], r=[("ps", b)], w=["memkT"])

    Wqk = AV(arena, 0, [8, 512])
    Wv = AV(arena, 4096, [8, 512])
    Wr = AV(arena, 8192, [8, 512])
    Wcq = AV(arena, 12288, [8, 384])
    We = AV(arena, 15360, [8, 288])
    Wa = AV(arena, 17664, [8, 16])
    Wuq = AV(arena, 17792, [3, 768])
    Wuk = AV(arena, 20096, [2, 512])
    Wuv = AV(arena, 21120, [2, 512])

    def bc_h(ap2d, nh):
        return ap2d.unsqueeze(1).to_broadcast([ap2d.shape[0], nh, ap2d.shape[1]])

    def rope(x1, x2, o1, o2, cs, sn, nh, rk, wk, ntok=128, ck="cosT", sk_="sinT"):
        c3, s3 = bc_h(cs, nh), bc_h(sn, nh)
        t = [rp[i][:ntok, 0:nh, :] for i in range(4)]
        B.tt(t[0], x1, c3, ALU.mult, r=rk + [ck], w=["rp0"])
        B.tt(t[1], x2, s3, ALU.mult, r=rk + [sk_], w=["rp1"])
        B.tt(t[2], x1, s3, ALU.mult, r=rk + [sk_], w=["rp2"])
        B.tt(t[3], x2, c3, ALU.mult, r=rk + [ck], w=["rp3"])
        B.tt(o1, t[0], t[1], ALU.subtract, r=["rp0", "rp1"], w=wk)
        B.tt(o2, t[2], t[3], ALU.add, r=["rp2", "rp3"], w=wk)

    def load_A_weights():
        wi = w_in
        for (av, c0, n) in ((Wqk, 0, 512), (Wv, C_V, 512), (Wr, C_R, 512), (Wcq, C_CQ, 384),
                            (We, C_CKV, 288), (Wa, C_A, 16)):
            load_w(av.ap, wi[:, c0:c0 + n], av.keys)
        load_w(Wuq.ap, w_uq, Wuq.keys, kcn=3)
        load_w(Wuk.ap, w_uk, Wuk.keys, kcn=2)
        load_w(Wuv.ap, w_uv, Wuv.keys, kcn=2)

    def phaseA(s):
        load_A_weights()
        P.add("dve", lambda e: e.memset(Sst[:].rearrange("p a b -> p (a b)"), 0.0), w=["Sst"])
        P.add("dve", lambda e: e.memset(Sbf[:].rearrange("p a b -> p (a b)"), 0.0), w=["Sbf"])
        P.add("dve", lambda e: e.memset(V_flat, 1.0), w=V_keys)
        for t in range(16):
            tok0 = s * SEQ + t * 128
            tsl = slice(t * 128, (t + 1) * 128)
            i = cnt["x"] % 2
            cnt["x"] += 1
            xk, xtk = ("xin", i), ("xT", i)
            B.dma(xin[i][:], xp[tok0:tok0 + 128, :], w=[xk])
            transpose_tile(xin[i], xk, xT[i], xtk, D)

            def zgroup(av, n):
                b = B.next_bank()
                for kc in range(8):
                    B.mm(psum[b][:, 0:n], xT[i][:, kc, :], av.ap[:, kc, :], kc == 0, kc == 7,
                         r=[xtk] + av.keys, w=[("ps", b)])
                return b
            b = zgroup(Wqk, 512)
            B.copy(zqk[:], psum[b][:, :], r=[("ps", b)], w=["zqk"])
            b = zgroup(Wv, 512)
            B.copy(vbf[:], psum[b][:, :], r=[("ps", b)], w=["vbf"])
            b = zgroup(Wr, 512)
            B.act(sr[:], psum[b][:, :], AF.Silu, r=[("ps", b)], w=["sr"])
            b = zgroup(Wcq, 384)
            B.copy(cq[:], psum[b][:, 0:384], r=[("ps", b)], w=["cq"])
            b = zgroup(We, 288)
            B.copy(ze[:], psum[b][:, 0:288], r=[("ps", b)], w=["ze"])
            b = B.next_bank()
            for kc in range(8):
                B.mm(psum[b][0:16, 0:128], Wa.ap[:, kc, :], xT[i][:, kc, :], kc == 0, kc == 7,
                     r=[xtk] + Wa.keys, w=[("ps", b)])
            B.copy(aT[0:16, :], psum[b][0:16, 0:128], r=[("ps", b)], w=["aT"])
            b = B.next_bank()
            B.mm(psum[b][:, 0:256], aT[0:17, :], wgate[0:17, :], True, True, r=["aT", "wgate"], w=[("ps", b)])
            B.act(e1[:], psum[b][:, 0:256], AF.Exp, r=[("ps", b)], w=["e1"], scale=-1.0)
            B.act(l1[:], e1[:], AF.Ln, r=["e1"], w=["l1"], bias=1.0)
            b = B.next_bank()
            B.mm(psum[b][:, 0:256], c_us[:], l1[:], True, True, r=["c_us", "l1"], w=[("ps", b)])
            B.act(eb[:], psum[b][:, 0:256], AF.Exp, r=[("ps", b)], w=["eb"])
            B.act(enb[:], psum[b][:, 0:256], AF.Exp, r=[("ps", b)], w=["enb"], scale=-1.0)
            b = B.next_bank()
            B.mm(psum[b][:, 0:256], c_lw[:], l1[:], True, True, r=["c_lw", "l1"], w=[("ps", b)])
            B.act(ekb[:], psum[b][:, 0:256], AF.Exp, r=[("ps", b)], w=["ekb"])
            b = B.next_bank()
            for h in range(4):
                B.mm(psum[b][0:64, h:h + 1], l1[:, h * 64:(h + 1) * 64], c_us[:, 127:128], True, True,
                     r=["c_us", "l1"], w=[("ps", b)])
            B.act(eblT[:], psum[b][0:64, 0:4], AF.Exp, r=[("ps", b)], w=["eblT"])
            B.stt(qt[:], zqk[:, 0:256], 0.125, eb[:], ALU.mult, ALU.mult, r=["zqk", "eb"], w=["qt"])
            B.tt(kt[:], zqk[:, 256:512], enb[:], ALU.mult, r=["zqk", "enb"], w=["kt"])
            B.tt(kp[:], zqk[:, 256:512], ekb[:], ALU.mult, r=["zqk", "ekb"], w=["kp"])
            b = B.next_bank()
            pb = psum[b][:, :].bitcast(BF16)
            for h in range(4):
                B.tr(pb[0:64, h * 128:(h + 1) * 128], qt[:, h * 64:(h + 1) * 64], identb[:], r=["qt", "identb"], w=[("ps", b)])
                B.tr(pb[0:64, (4 + h) * 128:(5 + h) * 128], kt[:, h * 64:(h + 1) * 64], identb[:], r=["kt", "identb"], w=[("ps", b)])
            B.copy(qkT[:].rearrange("p a b -> p (a b)"), pb[0:64, :], r=[("ps", b)], w=["qkT"])
            b = B.next_bank()
            for h in range(4):
                B.mm(psum[b][:, h * 128:(h + 1) * 128], qkT[:, 4 + h, :], qkT[:, h, :], True, True, r=["qkT"], w=[("ps", b)])
            B.tt(ATm[:], psum[b][:, :].rearrange("p (h i) -> p h i", i=128), bc_h(c_mask[:, :], 4), ALU.mult,
                 r=[("ps", b), "c_mask"], w=["ATm"])
            bo = B.next_bank()
            for h in range(4):
                B.mm(psum[bo][:, h * 128:(h + 1) * 128], ATm[:, h, :], vbf[:, h * 128:(h + 1) * 128], True, False,
                     r=["ATm", "vbf"], w=[("ps", bo)])
                B.mm(psum[bo][:, h * 128:(h + 1) * 128], qkT[:, h, :], Sbf[:, h, :], False, True,
                     r=["qkT", "Sbf"], w=[("ps", bo)])
            B.copy(osb[:].rearrange("p a b -> p (a b)"), psum[bo][:, :], r=[("ps", bo)], w=["osb"], eng="act")
            b = B.next_bank()
            for h in range(4):
                B.mm(psum[b][0:64, h * 128:(h + 1) * 128], kp[:, h * 64:(h + 1) * 64], vbf[:, h * 128:(h + 1) * 128], True, True,
                     r=["kp", "vbf"], w=[("ps", b)])
            for h in range(4):
                B.stt(Sst[:, h, :], Sst[:, h, :], eblT[:, h:h + 1], psum[b][0:64, h * 128:(h + 1) * 128], ALU.mult, ALU.add,
                      r=["eblT", ("ps", b), "Sst"], w=["Sst"])
            B.copy(Sbf[:].rearrange("p a b -> p (a b)"), Sst[:].rearrange("p a b -> p (a b)"), r=["Sst"], w=["Sbf"], eng="dve")
            of = osb[:].rearrange("p a b -> p (a b)")
            B.tt(junk[:], of, of, ALU.mult, r=["osb"], w=["junk"])
            B.red(ss4[:], junk[:].rearrange("p (a b) -> p a b", b=128), ALU.add, r=["junk"], w=["ss4"])
            B.rsqrt(rs4[:], ss4[:], 128.0 * RMS_EPS, rsq[:, 0:4], r=["ss4"], w=["rs4"])
            B.tt(t1, osb[:], rs4[:, :].unsqueeze(2).to_broadcast([128, 4, 128]), ALU.mult, r=["osb", "rs4"], w=["t1"])
            B.tt(SG, sr[:].rearrange("p (a b) -> p a b", b=128), bc_h(ggla[:, :], 4), ALU.mult, r=["sr", "ggla"], w=["SG"])
            B.tt(gobf[:].rearrange("p (a b) -> p a b", b=128), t1, SG, ALU.mult, r=["t1", "SG"], w=["gobf"])
            B.dma(sc_go[tok0:tok0 + 128, :], gobf[:], r=["gobf"], w=[("sc_go", s, t)])
            B.act(junk[:, 0:256], ze[:, 0:256], AF.Square, r=["ze"], w=["junk", "sskv"], accum_out=sskv[:, 0:1])
            B.rsqrt(rskv[:, 0:1], sskv[:, 0:1], 256.0 * RMS_EPS, rsq[:, 0:1], r=["sskv"], w=["rskv"])
            B.stt(ckvn[:], ze[:, 0:256], rskv[:, 0:1], gkv[:], ALU.mult, ALU.mult, r=["ze", "rskv", "gkv"], w=["ckvn"])
            B.dma(o_lat[tok0:tok0 + 128, :], ckvn[:], r=["ckvn"], w=[("o_lat", s, t)])
            transpose_tile(ckvn, "ckvn", ckvT, "ckvT", 256)
            for hg in range(2):
                b = B.next_bank()
                for hh in range(4):
                    h = hg * 4 + hh
                    for kc in range(2):
                        B.mm(psum[b][0:64, hh * 128:(hh + 1) * 128], Wuk.ap[:, kc, h * 64:(h + 1) * 64], ckvT[:, kc, :],
                             kc == 0, kc == 1, r=["ckvT"] + Wuk.keys, w=[("ps", b)])
                B.copy(kT_seq[0:64, hg * 4:hg * 4 + 4, tsl], psum[b][0:64, :].rearrange("p (h t) -> p h t", t=128),
                       r=[("ps", b)], w=kT_keys)
            b = B.next_bank()
            for kc in range(2):
                B.mm(psum[b][:, :], ckvT[:, kc, :], Wuv.ap[:, kc, :], kc == 0, kc == 1, r=["ckvT"] + Wuv.keys, w=[("ps", b)])
            B.copy(V_seq[:, t, :, 0:64], psum[b][:, :].rearrange("p (h d) -> p h d", d=64), r=[("ps", b)], w=V_keys)
            rope(ze[:, 256:272].unsqueeze(1), ze[:, 272:288].unsqueeze(1), kpe[:, 64:80].unsqueeze(1), kpe[:, 80:96].unsqueeze(1),
                 cosT[:, t, :], sinT[:, t, :], 1, ["ze"], ["kpe"])
            B.dma(o_kr[tok0:tok0 + 128, :], kpe[:, 64:96], r=["kpe"], w=[("o_kr", s, t)])
            b = B.next_bank()
            B.tr(psum[b][0:96, 0:128], kpe[:, :], ident[:], r=["kpe", "ident"], w=[("ps", b)])
            B.copy(kT_seq[64:96, :, tsl], psum[b][64:96, 0:128].unsqueeze(1).to_broadcast([32, 8, 128]),
                   r=[("ps", b)], w=kT_keys, eng="dve")
            B.act(junk[:, 0:384], cq[:], AF.Square, r=["cq"], w=["junk", "sskv"], accum_out=sskv[:, 1:2])
            B.rsqrt(rskv[:, 1:2], sskv[:, 1:2], 384.0 * RMS_EPS, rsq[:, 0:1], r=["sskv"], w=["rskv"])
            B.stt(cqn[:], cq[:], rskv[:, 1:2], gq[:], ALU.mult, ALU.mult, r=["cq", "rskv", "gq"], w=["cqn"])
            transpose_tile(cqn, "cqn", cqnT, "cqnT", 384)
            for (c0, n, h0, nh) in ((0, 480, 0, 5), (480, 288, 5, 3)):
                b = B.next_bank()
                for kc in range(3):
                    B.mm(psum[b][:, 0:n], cqnT[:, kc, :], Wuq.ap[:, kc, c0:c0 + n], kc == 0, kc == 2,
                         r=["cqnT"] + Wuq.keys, w=[("ps", b)])
                P.add("act", lambda e, b=b, n=n, h0=h0, nh=nh: e.mul(
                    qtm[:, h0:h0 + nh, :], psum[b][:, 0:n].rearrange("p (h d) -> p h d", d=96), float(MLA_SCALE)),
                    r=[("ps", b)], w=["qtm"])
            rope(qtm[:, :, 64:80], qtm[:, :, 80:96], qtm[:, :, 64:80], qtm[:, :, 80:96],
                 cosT[:, t, :], sinT[:, t, :], 8, ["qtm"], ["qtm"])
            B.copy(qbf[:].rearrange("p (h d) -> p h d", d=96), qtm[:], r=["qtm"], w=["qbf"], eng="dve")
            B.dma(sc_q[tok0:tok0 + 128, :], qbf[:], r=["qbf"], w=[("sc_q", s, t)])
        B.dma(o_sgla[s].rearrange("h d v -> d h v"), Sst[:], r=["Sst"], w=[("o_sgla", s)])


    oxb = zqk[:, :].bitcast(BF16).rearrange("p (h d) -> p h d", d=256)
    oxT = sr[:, :].bitcast(BF16).rearrange("p (c t) -> p c t", t=128)
    qT = osb[:].rearrange("p a b -> p (a b)").bitcast(BF16)[0:96, :].rearrange("p (h t) -> p h t", t=128)
    _jb = junk[:, :].bitcast(BF16)
    _qb = qtm[:].rearrange("p a b -> p (a b)").bitcast(BF16)
    PT = [_jb[:, 0:512].rearrange("p (a b) -> p a b", b=128), _jb[:, 512:1024].rearrange("p (a b) -> p a b", b=128),
          _qb[:, 0:512].rearrange("p (a b) -> p a b", b=128), _qb[:, 512:1024].rearrange("p (a b) -> p a b", b=128)]
    PTk = ["junk", "junk", "qtm", "qtm"]
    Wout = AV(arena, 0, [8, 1024])
    Wxq = AV(arena, 8192, [8, 1024])
    Wxo = AV(arena, 16384, [8, 1024])
    catT, h1T = xT[0], xT[1]
    catk, h1Tk = ("xT", 0), ("xT", 1)
    qxT = memT[:, :, 0:128]
    PX = memT[:, :, 128:256]
    mlao = vbf

    class _Tile:
        def __init__(self, v, ti, ntok):
            self.v, self.ti, self.n = v, ti, ntok

        def __getitem__(self, key):
            if isinstance(key, tuple):
                return self.v[:self.n, self.ti, key[1]]
            return self.v[:self.n, self.ti, :]

    class _Tile2:
        def __init__(self, v, ntok):
            self.v, self.n = v, ntok

        def __getitem__(self, key):
            if isinstance(key, tuple):
                return self.v[:self.n, key[1]]
            return self.v[:self.n, :]

    def layer_norm(pre, prek, gi, out, outk, ntok=128):
        n = ntok
        for hf in range(2):
            P.add("dve", lambda e, hf=hf: e.bn_stats(lnst[:n, hf, :], pre[:, hf * 512:(hf + 1) * 512]), r=[prek], w=["lnst"])
        P.add("dve", lambda e: e.bn_aggr(lnmv[:n, :], lnst[:n, :, :]), r=["lnst"], w=["lnmv"])
        B.rsqrt(lnrs[:n, 0:1], lnmv[:n, 1:2], LN_EPS, rsq[:n, 4:5], r=["lnmv"], w=["lnrs"], tk="rsq_ln")
        B.stt(lnrs[:n, 1:2], lnmv[:n, 0:1], -1.0, lnrs[:n, 0:1], ALU.mult, ALU.mult, r=["lnmv", "lnrs"], w=["lnrs"])
        B.act(big1[:n, :], pre[:], AF.Identity, r=[prek, "lnrs"], w=["t1", "SG"], scale=lnrs[:n, 0:1], bias=lnrs[:n, 1:2])
        B.tt(big1[:n, :], big1[:n, :], lnp[gi][:n, :], ALU.mult, r=["t1", "SG", ("lnp", gi)], w=["t1", "SG"])
        B.tt(out[:], big1[:n, :], lnp[gi + 1][:n, :], ALU.add, r=["t1", "SG", ("lnp", gi + 1)], w=[outk])

    def load_B_weights():
        load_w(Wout.ap, w_out, Wout.keys)
        load_w(Wxq.ap, w_xq, Wxq.keys)
        load_w(Wxo.ap, w_xo, Wxo.keys)

    catT_alt = gtmp[:, 0:2, :].rearrange("p a b -> p (a b)").bitcast(BF16).rearrange("p (c t) -> p c t", t=128)

    def phaseB(s):
        load_B_weights()
        B.bank_lim = 6
        B.psn = 0

        def front(t):
            tok0 = s * SEQ + t * 128
            i = cnt["x"] % 2
            cnt["x"] += 1
            xk = ("xin", i)
            catT, catk = (xT[0], [("xT", 0)]) if t % 2 == 0 else (catT_alt, ["e1", "l1"])
            B.dma(xin[i][:], xp[tok0:tok0 + 128, :], w=[xk])
            B.dma(qbf[:], sc_q[tok0:tok0 + 128, :], r=[("sc_q", s, t)], w=["qbf"])
            B.dma(gobf[:], sc_go[tok0:tok0 + 128, :], r=[("sc_go", s, t)], w=["gobf"])
            b = B.next_bank()
            pb = psum[b][:, :].bitcast(BF16)
            for h in range(8):
                B.tr(pb[0:96, h * 128:(h + 1) * 128], qbf[:, h * 96:(h + 1) * 96], identb[:], r=["qbf", "identb"], w=[("ps", b)])
            B.copy(qT, pb[0:96, :].rearrange("p (h t) -> p h t", t=128), r=[("ps", b)], w=["osb"])
            b = B.next_bank()
            pb = psum[b][:, :].bitcast(BF16)
            for c in range(4):
                B.tr(pb[:, c * 128:(c + 1) * 128], gobf[:, c * 128:(c + 1) * 128], identb[:], r=["gobf", "identb"], w=[("ps", b)])
            B.copy(catT[:, 0:4, :], pb[:, 0:512].rearrange("p (c t) -> p c t", t=128), r=[("ps", b)], w=catk)
            nk = t + 1

            def att_front(h, g0, g1):
                b = B.next_bank()
                for kt_ in range(g0, g1):
                    j = kt_ - g0
                    B.mm(psum[b][:, j * 128:(j + 1) * 128], kT_seq[0:96, h, kt_ * 128:(kt_ + 1) * 128], qT[:, h, :],
                         True, True, r=["osb"] + kT_keys, w=[("ps", b)])
                pi = cnt["st"] % 4
                cnt["st"] += 1
                n = g1 - g0
                B.act(PT[pi][:, 0:n, :], psum[b][:, 0:n * 128].rearrange("p (a b) -> p a b", b=128), AF.Exp,
                      r=[("ps", b)], w=[PTk[pi]])
                if g1 == nk:
                    B.tt(PT[pi][:, n - 1, :], PT[pi][:, n - 1, :], maskb[:], ALU.mult, r=[PTk[pi], "maskb"], w=[PTk[pi]])
                return pi

            def att_back(h, g0, g1, pi):
                ob = 6 + h // 4
                oc = (h % 4) * 65
                for kt_ in range(g0, g1):
                    j = kt_ - g0
                    B.mm(psum[ob][:, oc:oc + 65], PT[pi][:, j, :], V_seq[:, kt_, h, 0:65], kt_ == 0, kt_ == nk - 1,
                         r=[PTk[pi]] + V_keys, w=[("ps", ob)])

            pend = None
            for h in range(8):
                for g0 in range(0, nk, 4):
                    g1 = min(nk, g0 + 4)
                    pi = att_front(h, g0, g1)
                    if pend is not None:
                        att_back(*pend)
                    pend = (h, g0, g1, pi)
            att_back(*pend)
            for hg in range(2):
                ov = psum[6 + hg][:, 0:260].rearrange("p (h d) -> p h d", d=65)
                P.add("dve", lambda e, ov=ov, hg=hg: e.reciprocal(rec8[:, hg * 4:hg * 4 + 4], ov[:, :, 64]),
                      r=[("ps", 6 + hg)], w=[("rec8", hg)])
                B.tt(mlao[:, hg * 256:(hg + 1) * 256].rearrange("p (h d) -> p h d", d=64), ov[:, :, 0:64],
                     rec8[:, hg * 4:hg * 4 + 4].unsqueeze(2).to_broadcast([128, 4, 64]), ALU.mult,
                     r=[("ps", 6 + hg), ("rec8", hg)], w=["vbf"])
            b = B.next_bank()
            pb = psum[b][:, :].bitcast(BF16)
            for c in range(4):
                B.tr(pb[:, c * 128:(c + 1) * 128], mlao[:, c * 128:(c + 1) * 128], identb[:], r=["vbf", "identb"], w=[("ps", b)])
            B.copy(catT[:, 4:8, :], pb[:, 0:512].rearrange("p (c t) -> p c t", t=128), r=[("ps", b)], w=catk)
            return (t, tok0, i, xk, catT, catk)

        def tail(t, tok0, i, xk, catT, catk):
            pre, prek = stage[0], ("stage", 0)
            h1, h1k = stage[1], ("stage", 1)
            for hf in range(2):
                b = B.next_bank()
                for kc in range(8):
                    B.mm(psum[b][:, :], catT[:, kc, :], Wout.ap[:, kc, hf * 512:(hf + 1) * 512], kc == 0, kc == 7,
                         r=catk + Wout.keys, w=[("ps", b)])
                B.stt(pre[:, hf * 512:(hf + 1) * 512], xin[i][:, hf * 512:(hf + 1) * 512], float(ALPHA), psum[b][:, :],
                      ALU.mult, ALU.add, r=[xk, ("ps", b)], w=[prek])
            layer_norm(pre, prek, 0, h1, h1k)
            transpose_tile(h1, h1k, h1T, h1Tk, D)
            for jg in range(2):
                b = B.next_bank()
                for jj in range(4):
                    j = jg * 4 + jj
                    for kc in range(8):
                        B.mm(psum[b][:, jj * 128:(jj + 1) * 128], Wxq.ap[:, kc, j * 128:(j + 1) * 128], h1T[:, kc, :],
                             kc == 0, kc == 7, r=[h1Tk] + Wxq.keys, w=[("ps", b)])
                P.add("act", lambda e, b=b, jg=jg: e.mul(qxT[:, jg * 4:jg * 4 + 4, :],
                      psum[b][:, :].rearrange("p (a t) -> p a t", t=128), 1.0 / 16.0), r=[("ps", b)], w=["qxT"])
            for hg in range(2):
                b = B.next_bank()
                for hh in range(2):
                    h = hg * 2 + hh
                    for mt in range(2):
                        col = (hh * 2 + mt) * 128
                        for c in range(2):
                            B.mm(psum[b][:, col:col + 128], memkT[:, s, h * 2 + c, mt * 128:(mt + 1) * 128], qxT[:, h * 2 + c, :],
                                 c == 0, c == 1, r=["memkT", "qxT"], w=[("ps", b)])
                B.act(PX[:, hg * 4:hg * 4 + 4, :], psum[b][:, :].rearrange("p (a t) -> p a t", t=128), AF.Exp,
                      r=[("ps", b)], w=["PX"])
            for h in range(4):
                b = B.next_bank()
                for mt in range(2):
                    B.mm(psum[b][:, 0:257], PX[:, h * 2 + mt, :], memv[:, s, mt, h, 0:257], mt == 0, mt == 1,
                         r=["PX", "memv"], w=[("ps", b)])
                P.add("dve", lambda e, b=b, h=h: e.reciprocal(rec8[:, h:h + 1], psum[b][:, 256:257]), r=[("ps", b)], w=[("rec8", "x", h)])
                B.ts(oxb[:, h, :], psum[b][:, 0:256], rec8[:, h:h + 1], None, ALU.mult, r=[("ps", b), ("rec8", "x", h)], w=["zqk"])
            for cg in range(2):
                b = B.next_bank()
                pb = psum[b][:, :].bitcast(BF16)
                oxf = zqk[:, :].bitcast(BF16)
                for cc in range(4):
                    c = cg * 4 + cc
                    B.tr(pb[:, cc * 128:(cc + 1) * 128], oxf[:, c * 128:(c + 1) * 128], identb[:], r=["zqk", "identb"], w=[("ps", b)])
                B.copy(oxT[:, cg * 4:cg * 4 + 4, :], pb[:, 0:512].rearrange("p (c t) -> p c t", t=128), r=[("ps", b)], w=["sr"])
            for hf in range(2):
                b = B.next_bank()
                for kc in range(8):
                    B.mm(psum[b][:, :], oxT[:, kc, :], Wxo.ap[:, kc, hf * 512:(hf + 1) * 512], kc == 0, kc == 7,
                         r=["sr"] + Wxo.keys, w=[("ps", b)])
                B.stt(pre[:, hf * 512:(hf + 1) * 512], h1[:, hf * 512:(hf + 1) * 512], float(ALPHA), psum[b][:, :],
                      ALU.mult, ALU.add, r=[h1k, ("ps", b)], w=[prek])
            layer_norm(pre, prek, 2, xin[i], xk)
            B.dma(sc_h2[tok0:tok0 + 128, :], xin[i][:], r=[xk], w=[("sc_h2", tok0 // 128)])

        pend = None
        for t in range(16):
            st = front(t)
            if pend is not None:
                tail(*pend)
            pend = st
        tail(*pend)
        B.bank_lim = 8


    SK = ["s_pg%d" % k_ for k_ in range(9)] + ["s_cT%d" % k_ for k_ in range(9)] + ["s_cb%d" % k_ for k_ in range(9)] + [
          "s_PS0", "s_PS1", "s_v32", "s_vm0", "s_vm1", "s_colT", "s_QM", "s_S0", "s_S1", "s_catS", "s_QLT", "s_qTs", "s_WukT",
          "s_ol", "s_olT", "s_om", "s_mk0", "s_mk1", "s_e16", "s_qx", "s_pxs", "s_om4", "s_den"]

    def sv(off, n, dt=F32, parts=128):
        a = arenaS[0:parts, off:off + n]
        return a.bitcast(dt) if dt != F32 else a
    NSL = 9
    s_pg = [sv(1040 + k_ * 352, 352) for k_ in range(8)] + [sv(0, 352)]
    s_cT = [sv(3856 + k_ * 192, 192, BF16).rearrange("p (c t) -> p c t", t=128) for k_ in range(8)] + [
        sv(352, 192, BF16).rearrange("p (c t) -> p c t", t=128)]
    s_cb = [sv(5392 + k_ * 130, 130, BF16) for k_ in range(8)] + [sv(544, 130, BF16)]
    s_PS = [sv(6432 + k_ * 16, 16, BF16) for k_ in range(2)]
    s_v32 = sv(6464, 512, parts=16)
    s_vm = [sv(6976 + k_ * 512, 512, parts=16) for k_ in range(2)]
    s_colT = sv(8000, 192, parts=64).rearrange("p (a b) -> p a b", b=16)
    s_QM = sv(8192, 1024, parts=64).rearrange("p (h b m) -> p h b m", b=16, m=16)
    s_S = [sv(9216 + k_ * 512, 512, parts=64).rearrange("p (h v) -> p h v", v=128) for k_ in range(2)]
    s_catS = sv(10240, 1024, parts=16)
    s_QLT = sv(11264, 128, BF16).rearrange("p (c h b) -> p c h b", h=8, b=16)
    s_qTs = sv(11392, 64, BF16, parts=96).rearrange("p (h b) -> p h b", b=16)
    s_WukT = sv(11456, 1024, BF16, parts=64).rearrange("p (h r) -> p h r", r=256)
    s_ol = sv(12480, 256, parts=8)
    s_olT = sv(12736, 8, BF16).rearrange("p (c h) -> p c h", h=8)
    s_om = sv(6976, 512, parts=8)
    s_mk = [sv(1040 + k_ * 1024, 1024) for k_ in range(2)]
    s_e16 = sv(3088, 128, parts=16)
    s_qx = sv(3216, 1024, parts=16)
    s_pxs = sv(4240, 4, BF16)
    s_om4 = sv(4244, 1024, parts=4)
    s_den = sv(5268, 8, parts=4)

    def rms_rows(src, n, sscol, eps_n, gain, out, srck, outk, ntok):
        B.act(junk[:ntok, 0:n], src, AF.Square, r=[srck], w=["junk", "sskv"], accum_out=sskv[:ntok, sscol:sscol + 1])
        B.rsqrt(rskv[:ntok, sscol:sscol + 1], sskv[:ntok, sscol:sscol + 1], eps_n, rsq[:ntok, 0:1], r=["sskv"], w=["rskv"])
        B.stt(out, src, rskv[:ntok, sscol:sscol + 1], gain, ALU.mult, ALU.mult, r=[srck, "rskv"], w=[outk])

    def sampleA():
        n = NS
        barrier(kT_keys + V_keys + SK)
        load_A_weights()
        B.bank_lim = 6
        B.psn = 0
        i = cnt["x"] % 2
        cnt["x"] += 1
        xk, xtk = ("xin", i), ("xT", i)
        B.dma(xin[i][:n, :], xs, w=[xk])
        transpose_tile(xin[i], xk, xT[i], xtk, D, ntok=n)
        for k_ in range(NSL):
            P.add("dve", lambda e, k_=k_: e.memset(s_pg[k_], 0.0), w=["s_pg%d" % k_])
            P.add("dve", lambda e, k_=k_: e.memset(s_cb[k_], 1.0), w=["s_cb%d" % k_])

        def zgroup(av, nn):
            b = B.next_bank()
            for kc in range(8):
                B.mm(psum[b][:n, 0:nn], xT[i][:, kc, 0:n], av.ap[:, kc, :], kc == 0, kc == 7, r=[xtk] + av.keys, w=[("ps", b)])
            return b
        b = zgroup(Wqk, 512)
        B.copy(zqk[:n, :], psum[b][:n, :], r=[("ps", b)], w=["zqk"])
        b = zgroup(Wv, 512)
        B.copy(s_v32, psum[b][:n, :], r=[("ps", b)], w=["s_v32"])
        b = zgroup(Wr, 512)
        B.act(sr[:n, :], psum[b][:n, :], AF.Silu, r=[("ps", b)], w=["sr"])
        b = zgroup(Wcq, 384)
        B.copy(cq[:n, :], psum[b][:n, 0:384], r=[("ps", b)], w=["cq"])
        b = zgroup(We, 288)
        B.copy(ze[:n, :], psum[b][:n, 0:288], r=[("ps", b)], w=["ze"])
        b = B.next_bank()
        for kc in range(8):
            B.mm(psum[b][0:16, 0:n], Wa.ap[:, kc, :], xT[i][:, kc, 0:n], kc == 0, kc == 7, r=[xtk] + Wa.keys, w=[("ps", b)])
        B.copy(aT[0:16, 0:n], psum[b][0:16, 0:n], r=[("ps", b)], w=["aT"])
        b = B.next_bank()
        B.mm(psum[b][:n, 0:256], aT[0:17, 0:n], wgate[0:17, :], True, True, r=["aT", "wgate"], w=[("ps", b)])
        B.act(e1[:n, :], psum[b][:n, 0:256], AF.Exp, r=[("ps", b)], w=["e1"], scale=-1.0)
        B.act(l1[:n, :], e1[:n, :], AF.Ln, r=["e1"], w=["l1"], bias=1.0)
        B.act(eb[:n, :], l1[:n, :], AF.Exp, r=["l1"], w=["eb"], scale=-1.0 / 16.0)
        b = B.next_bank()
        for xi, (src, c0, key) in enumerate(((eb, 0, "eb"), (zqk, 256, "zqk"), (zqk, 0, "zqk"))):
            for h in range(4):
                B.tr(psum[b][0:64, (xi * 4 + h) * 16:(xi * 4 + h + 1) * 16], src[:n, c0 + h * 64:c0 + (h + 1) * 64], ident[:n, :n],
                     r=[key, "ident"], w=[("ps", b)])
        B.copy(s_colT[:, 0:8, :], psum[b][0:64, 0:128].rearrange("p (a b) -> p a b", b=16), r=[("ps", b)], w=["s_colT"], eng="dve")
        P.add("act", lambda e, b=b: e.mul(s_colT[:, 8:12, :], psum[b][0:64, 128:192].rearrange("p (a b) -> p a b", b=16), 0.125),
              r=[("ps", b)], w=["s_colT"])
        B.tt(s_QM, s_colT[:, 8:12, :].unsqueeze(3).to_broadcast([64, 4, 16, 16]),
             i16[:, :, :].unsqueeze(1).to_broadcast([64, 4, 16, 16]), ALU.mult, r=["s_colT", "i16"], w=["s_QM"])
        B.bank_lim = 4
        B.psn = 0
        for bb in range(n):
            sl = bb % 2
            Sk = "s_S%d" % sl
            B.dma(s_S[sl], sgla_s[bb].rearrange("h d v -> d h v"), w=[Sk])
            B.ts(s_vm[sl], s_v32, ident[:n, bb:bb + 1], None, ALU.mult, r=["s_v32", "ident"], w=["s_vm%d" % sl])
            b = B.next_bank()
            for h in range(4):
                B.mm(psum[b][0:64, h * 128:(h + 1) * 128], zqk[:n, 256 + h * 64:256 + (h + 1) * 64], s_vm[sl][:, h * 128:(h + 1) * 128],
                     True, True, r=["zqk", "s_vm%d" % sl], w=[("ps", b)])
            for h in range(4):
                B.stt(s_S[sl][:, h, :], s_S[sl][:, h, :], s_colT[:, h, bb:bb + 1], psum[b][0:64, h * 128:(h + 1) * 128], ALU.mult, ALU.add,
                      r=[Sk, "s_colT", ("ps", b)], w=[Sk])
            B.dma(o_sgla_s[bb].rearrange("h d v -> d h v"), s_S[sl], r=[Sk], w=[("o_sgla_s", bb)])
            for h in range(4):
                B.mm(psum[4 + h][:n, 0:128], s_QM[:, h, bb, :], s_S[sl][:, h, :], bb == 0, bb == n - 1,
                     r=["s_QM", Sk], w=[("ps", 4 + h)])
        for h in range(4):
            B.copy(osb[:n, h, :], psum[4 + h][:n, 0:128], r=[("ps", 4 + h)], w=["osb"], eng="act")
        B.bank_lim = 6
        B.psn = 0
        of = osb[:n].rearrange("p a b -> p (a b)")
        B.tt(junk[:n, :], of, of, ALU.mult, r=["osb"], w=["junk"])
        B.red(ss4[:n, :], junk[:n, :].rearrange("p (a b) -> p a b", b=128), ALU.add, r=["junk"], w=["ss4"])
        B.rsqrt(rs4[:n, :], ss4[:n, :], 128.0 * RMS_EPS, rsq[:n, 0:4], r=["ss4"], w=["rs4"])
        B.tt(t1[:n], osb[:n], rs4[:n, :].unsqueeze(2).to_broadcast([n, 4, 128]), ALU.mult, r=["osb", "rs4"], w=["t1"])
        B.tt(SG[:n], sr[:n, :].rearrange("p (a b) -> p a b", b=128), bc_h(ggla[:n, :], 4), ALU.mult, r=["sr", "ggla"], w=["SG"])
        B.tt(s_catS[:, 0:512].rearrange("p (a b) -> p a b", b=128), t1[:n], SG[:n], ALU.mult, r=["t1", "SG"], w=["s_catS"])
        rms_rows(ze[:n, 0:256], 256, 0, 256.0 * RMS_EPS, gkv[:n, :], ckvn[:n, :], "ze", "ckvn", n)
        B.dma(o_lat_s, ckvn[:n, :], r=["ckvn"], w=["o_lat_s"])
        rope(ze[:n, 256:272].unsqueeze(1), ze[:n, 272:288].unsqueeze(1), kpe[:n, 64:80].unsqueeze(1), kpe[:n, 80:96].unsqueeze(1),
             cos_s[:, :], sin_s[:, :], 1, ["ze"], ["kpe"], ntok=n, ck="cos_s", sk_="sin_s")
        B.dma(o_kr_s, kpe[:n, 64:96], r=["kpe"], w=["o_kr_s"])
        rms_rows(cq[:n, :], 384, 1, 384.0 * RMS_EPS, gq[:n, :], cqn[:n, :], "cq", "cqn", n)
        transpose_tile(cqn, "cqn", cqnT, "cqnT", 384, ntok=n)
        for (c0, nn, h0, nh) in ((0, 480, 0, 5), (480, 288, 5, 3)):
            b = B.next_bank()
            for kc in range(3):
                B.mm(psum[b][:n, 0:nn], cqnT[:, kc, 0:n], Wuq.ap[:, kc, c0:c0 + nn], kc == 0, kc == 2, r=["cqnT"] + Wuq.keys, w=[("ps", b)])
            P.add("act", lambda e, b=b, nn=nn, h0=h0, nh=nh: e.mul(
                qtm[:n, h0:h0 + nh, :], psum[b][:n, 0:nn].rearrange("p (h d) -> p h d", d=96), float(MLA_SCALE)),
                r=[("ps", b)], w=["qtm"])
        rope(qtm[:n, :, 64:80], qtm[:n, :, 80:96], qtm[:n, :, 64:80], qtm[:n, :, 80:96], cos_s[:, :], sin_s[:, :], 8, ["qtm"], ["qtm"],
             ntok=n, ck="cos_s", sk_="sin_s")
        b = B.next_bank()
        for h in range(8):
            B.tr(psum[b][0:96, h * 16:(h + 1) * 16], qtm[:n, h, :], ident[:n, :n], r=["qtm", "ident"], w=[("ps", b)])
        B.copy(s_qTs, psum[b][0:96, 0:128].rearrange("p (h b) -> p h b", b=16), r=[("ps", b)], w=["s_qTs"])
        for kc in range(2):
            b = B.next_bank()
            pb = psum[b][:, :].bitcast(BF16)
            for h in range(8):
                B.tr(pb[0:64, h * 128:(h + 1) * 128], Wuk.ap[:, kc, h * 64:(h + 1) * 64], identb[:], r=Wuk.keys + ["identb"], w=[("ps", b)])
            B.copy(s_WukT[:, :, kc * 128:(kc + 1) * 128], pb[0:64, :].rearrange("p (h r) -> p h r", r=128), r=[("ps", b)], w=["s_WukT"])
        b = B.next_bank()
        for rc in range(2):
            for h in range(8):
                B.mm(psum[b][:, (rc * 8 + h) * 16:(rc * 8 + h + 1) * 16], s_WukT[:, h, rc * 128:(rc + 1) * 128], s_qTs[0:64, h, :],
                     True, True, r=["s_WukT", "s_qTs"], w=[("ps", b)])
        B.copy(s_QLT, psum[b][:, 0:256].rearrange("p (c h b) -> p c h b", h=8, b=16), r=[("ps", b)], w=["s_QLT"])
        B.bank_lim = 5
        B.psn = 0
        gcnt = [0]
        pgv = arenaS[:, 1040:1040 + 8 * 352].rearrange("p (s c) -> p s c", c=352)
        npg_tot = NPAGE + 1

        def pg_front(bb, grp):
            bs = B.next_bank()
            if grp[0][0] == "new":
                B.dma(s_pg[8][0:1, 0:256], o_lat_s[bb:bb + 1, :], r=["o_lat_s"], w=["s_pg8"])
                B.dma(s_pg[8][0:1, 320:352], o_kr_s[bb:bb + 1, :], r=["o_kr_s"], w=["s_pg8"])
            else:
                j0, s0 = grp[0][1], grp[0][2]
                row0 = (bb * NPAGE + j0) * 128
                wk4 = ["s_pg%d" % (s0 + k_) for k_ in range(4)]
                B.dma(pgv[:, s0:s0 + 4, 0:256], sc_lat[row0:row0 + 512, :].rearrange("(j p) r -> p j r", p=128),
                      r=[("sc_lat", bb)], w=wk4)
                B.dma(pgv[:, s0:s0 + 4, 320:352], sc_kr[row0:row0 + 512, :].rearrange("(j p) r -> p j r", p=128),
                      r=[("sc_kr", bb)], w=wk4)
            for jj, (kind, j, sl) in enumerate(grp):
                pgk, cTk, cbk = "s_pg%d" % sl, "s_cT%d" % sl, "s_cb%d" % sl
                bt = B.next_bank()
                B.tr(psum[bt][:, 0:128], s_pg[sl][:, 0:128], ident[:], r=[pgk, "ident"], w=[("ps", bt)])
                B.tr(psum[bt][:, 128:256], s_pg[sl][:, 128:256], ident[:], r=[pgk, "ident"], w=[("ps", bt)])
                B.tr(psum[bt][0:96, 256:384], s_pg[sl][:, 256:352], ident[:], r=[pgk, "ident"], w=[("ps", bt)])
                B.copy(s_cT[sl], psum[bt][:, 0:384].rearrange("p (c t) -> p c t", t=128), r=[("ps", bt)], w=[cTk])
                B.copy(s_cb[sl][:, 0:256], s_pg[sl][:, 0:256], r=[pgk], w=[cbk])
                B.mm(psum[bs][:, jj * 8:(jj + 1) * 8], s_cT[sl][:, 0, :], s_QLT[:, 0, :, bb], True, False, r=[cTk, "s_QLT"], w=[("ps", bs)])
                B.mm(psum[bs][:, jj * 8:(jj + 1) * 8], s_cT[sl][:, 1, :], s_QLT[:, 1, :, bb], False, False, r=[cTk, "s_QLT"], w=[("ps", bs)])
                B.mm(psum[bs][:, jj * 8:(jj + 1) * 8], s_cT[sl][64:96, 2, :], s_qTs[64:96, :, bb], False, True, r=[cTk, "s_qTs"], w=[("ps", bs)])
            ng = len(grp)
            pi = gcnt[0] % 2
            gcnt[0] += 1
            PSk = "s_PS%d" % pi
            B.act(s_PS[pi][:, 0:ng * 8], psum[bs][:, 0:ng * 8], AF.Exp, r=[("ps", bs)], w=[PSk])
            if grp[0][0] == "new":
                B.tt(s_PS[pi][:, 0:8], s_PS[pi][:, 0:8], maskb[:, 0:1].to_broadcast([128, 8]), ALU.mult, r=[PSk, "maskb"], w=[PSk])
            return pi

        def pg_back(bb, grp, pi, first, last):
            PSk = "s_PS%d" % pi
            for jj, (kind, j, sl) in enumerate(grp):
                B.mm(psum[6][0:8, 0:257], s_PS[pi][:, jj * 8:(jj + 1) * 8], s_cb[sl][:, 0:257],
                     first and jj == 0, last and jj == len(grp) - 1, r=[PSk, "s_cb%d" % sl], w=[("ps", 6)])
            if last:
                sample_post(bb)

        def sample_post(bb):
            P.add("dve", lambda e: e.reciprocal(rec8[0:8, 0:1], psum[6][0:8, 256:257]), r=[("ps", 6)], w=[("rec8", 0)])
            B.ts(s_ol, psum[6][0:8, 0:256], rec8[0:8, 0:1], None, ALU.mult, r=[("ps", 6), ("rec8", 0)], w=["s_ol"])
            b = B.next_bank()
            for c in range(2):
                B.tr(psum[b][:, c * 8:(c + 1) * 8], s_ol[:, c * 128:(c + 1) * 128], ident[:8, :8], r=["s_ol", "ident"], w=[("ps", b)])
            B.copy(s_olT, psum[b][:, 0:16].rearrange("p (c h) -> p c h", h=8), r=[("ps", b)], w=["s_olT"], eng="dve")
            b = B.next_bank()
            for kc in range(2):
                B.mm(psum[b][0:8, :], s_olT[:, kc, :], Wuv.ap[:, kc, :], kc == 0, kc == 1, r=["s_olT"] + Wuv.keys, w=[("ps", b)])
            B.tt(s_om, psum[b][0:8, :], bm8[:, :], ALU.mult, r=[("ps", b), "bm8"], w=["s_vm0"])
            B.mm(psum[7][:n, :], i16[0:8, bb, :], s_om, bb == 0, bb == n - 1, r=["i16", "s_vm0"], w=[("ps", 7)])

        all_groups = []
        for bb in range(n):
            all_groups.append((bb, [("new", 0, 8)], True, False))
            for gi in range(NPAGE // 4):
                s0 = (gi % 2) * 4
                all_groups.append((bb, [("page", gi * 4 + k_, s0 + k_) for k_ in range(4)], False, gi == NPAGE // 4 - 1))
        pend = None
        for (bb, grp, first, last) in all_groups:
            pi = pg_front(bb, grp)
            if pend is not None:
                pg_back(*pend)
            pend = (bb, grp, pi, first, last)
        pg_back(*pend)
        B.copy(s_catS[:, 512:1024], psum[7][:n, :], r=[("ps", 7)], w=["s_catS"], eng="dve")
        B.bank_lim = 8

    def sampleB():
        n = NS
        barrier(SK)
        load_B_weights()
        B.dma(bm4, bm4_d, w=["memkT"])
        B.bank_lim = 6
        B.psn = 0
        i = cnt["x"] % 2
        cnt["x"] += 1
        xk = ("xin", i)
        B.dma(xin[i][:n, :], xs, w=[xk])
        transpose_tile(s_catS, "s_catS", catT, catk, D, ntok=n)
        pre, prek = stage[0], ("stage", 0)
        h1, h1k = stage[1], ("stage", 1)
        for hf in range(2):
            b = B.next_bank()
            for kc in range(8):
                B.mm(psum[b][:n, :], catT[:, kc, 0:n], Wout.ap[:, kc, hf * 512:(hf + 1) * 512], kc == 0, kc == 7,
                     r=[catk] + Wout.keys, w=[("ps", b)])
            B.stt(pre[:n, hf * 512:(hf + 1) * 512], xin[i][:n, hf * 512:(hf + 1) * 512], float(ALPHA), psum[b][:n, :],
                  ALU.mult, ALU.add, r=[xk, ("ps", b)], w=[prek])
        layer_norm(_Tile2(pre, n), prek, 0, _Tile2(h1, n), h1k, ntok=n)
        transpose_tile(h1, h1k, h1T, h1Tk, D, ntok=n)
        for hf in range(2):
            b = B.next_bank()
            for kc in range(8):
                B.mm(psum[b][:n, :], h1T[:, kc, 0:n], Wxq.ap[:, kc, hf * 512:(hf + 1) * 512], kc == 0, kc == 7,
                     r=[h1Tk] + Wxq.keys, w=[("ps", b)])
            P.add("act", lambda e, b=b, hf=hf: e.mul(s_qx[:, hf * 512:(hf + 1) * 512], psum[b][:n, :], 1.0 / 16.0), r=[("ps", b)], w=["s_qx"])
        for bb in range(n):
            B.ts(s_e16, c_mask[0:n, :], 0.0, ident[:n, bb:bb + 1], ALU.mult, ALU.add, r=["c_mask", "ident"], w=["s_e16"])
            qb = []
            for hf in range(2):
                b = B.next_bank()
                B.mm(psum[b][:, :], s_e16, s_qx[:, hf * 512:(hf + 1) * 512], True, True, r=["s_e16", "s_qx"], w=[("ps", b)])
                qb.append(b)
            for mt in range(2):
                mk_, mkk = s_mk[0], "s_mk0"
                B.dma(mk_, cmk[bb, mt * 128:(mt + 1) * 128, :], w=[mkk])
                for hf in range(2):
                    B.tt(mk_[:, hf * 512:(hf + 1) * 512], mk_[:, hf * 512:(hf + 1) * 512], psum[qb[hf]][:, :], ALU.mult,
                         r=[mkk, ("ps", qb[hf])], w=[mkk])
                B.red(rsq[:, 0:4], mk_.rearrange("p (h d) -> p h d", d=256), ALU.add, r=[mkk], w=["rsq_tmp"])
                B.act(s_pxs[:, mt * 4:(mt + 1) * 4], rsq[:, 0:4], AF.Exp, r=["rsq_tmp"], w=["s_pxs"])
            bo = [B.next_bank(), B.next_bank()]
            bd = B.next_bank()
            for mt in range(2):
                mv_, mvk = s_mk[1], "s_mk1"
                B.dma(mv_, cmv[bb, mt * 128:(mt + 1) * 128, :], w=[mvk])
                mvb = oxT[:].rearrange("p c t -> p (c t)")
                B.copy(mvb, mv_, r=[mvk], w=["sr"])
                for hf in range(2):
                    B.mm(psum[bo[hf]][0:4, :], s_pxs[:, mt * 4:(mt + 1) * 4], mvb[:, hf * 512:(hf + 1) * 512], mt == 0, mt == 1,
                         r=["s_pxs", "sr"], w=[("ps", bo[hf])])
                B.mm(psum[bd][0:4, 0:1], s_pxs[:, mt * 4:(mt + 1) * 4], maskb[:, 127:128], mt == 0, mt == 1,
                     r=["s_pxs", "maskb"], w=[("ps", bd)])
            P.add("dve", lambda e, bd=bd: e.reciprocal(s_den[:, 0:1], psum[bd][0:4, 0:1]), r=[("ps", bd)], w=["s_den"])
            for hf in range(2):
                B.stt(s_om4[:, hf * 512:(hf + 1) * 512], psum[bo[hf]][0:4, :], s_den[:, 0:1], bm4[:, hf * 512:(hf + 1) * 512],
                      ALU.mult, ALU.mult, r=[("ps", bo[hf]), "s_den", "memkT"], w=["s_om4"])
                B.mm(psum[6 + hf][:n, :], i16[0:4, bb, :], s_om4[:, hf * 512:(hf + 1) * 512], bb == 0, bb == n - 1,
                     r=["i16", "s_om4"], w=[("ps", 6 + hf)])
        for hf in range(2):
            B.copy(s_catS[:, hf * 512:(hf + 1) * 512], psum[6 + hf][:n, :], r=[("ps", 6 + hf)], w=["s_catS"], eng="dve")
        transpose_tile(s_catS, "s_catS", catT, catk, D, ntok=n)
        for hf in range(2):
            b = B.next_bank()
            for kc in range(8):
                B.mm(psum[b][:n, :], catT[:, kc, 0:n], Wxo.ap[:, kc, hf * 512:(hf + 1) * 512], kc == 0, kc == 7,
                     r=[catk] + Wxo.keys, w=[("ps", b)])
            B.stt(pre[:n, hf * 512:(hf + 1) * 512], h1[:n, hf * 512:(hf + 1) * 512], float(ALPHA), psum[b][:n, :],
                  ALU.mult, ALU.add, r=[h1k, ("ps", b)], w=[prek])
        layer_norm(_Tile2(pre, n), prek, 2, _Tile2(xin[i], n), xk, ntok=n)
        B.dma(sc_h2[NT:NT + n, :], xin[i][:n, :], r=[xk], w=[("sc_h2", NT // 128)])
        B.bank_lim = 8

    if "A" in stages:
        for s in range(NSEQ):
            phaseA(s)
            if "B" in stages:
                phaseB(s)
    if "S" in stages:
        sampleA()
        sampleB()


    yacc_f, yacc_k = sview(0, 4096)
    yacc = yacc_f.rearrange("p (t d) -> p t d", d=D)
    h2Tf_f, h2Tf_k = sview(4096, 4096)
    h2Tf = h2Tf_f.rearrange("p (c t) -> p c t", t=512)
    h2Tb_f, h2Tb_k = sview(8192, 2048, BF16)
    h2Tb = h2Tb_f.rearrange("p (c t) -> p c t", t=512)
    hT_f, hT_k = sview(10240, 512, BF16)
    hT = hT_f.rearrange("p (f t) -> p f t", t=512)
    hT2_f, hT2_k = sview(11776, 512, BF16)
    hT2 = hT2_f.rearrange("p (f t) -> p f t", t=512)
    hTs = [(hT, ["hT_a"]), (hT2, ["hT_b"])]
    yout_f, yout_k = sview(10752, 1024)
    yout = [yout_f[:, 0:1024], yout_f[:, 0:1024]]
    EW = []
    for k_ in range(4):
        o = k_ * 6144
        EW.append((AV(arena, o, [8, 256]), AV(arena, o + 2048, [8, 256]), AV(arena, o + 4096, [2, 1024])))

    def load_expert(e):
        g_, u_, d_ = EW[e % 4]
        load_w(g_.ap, w_eg[e], g_.keys, step=8)
        load_w(u_.ap, w_eu[e], u_.keys, step=8)
        load_w(d_.ap, w_ed[e], d_.keys, kcn=2)

    def router(ti, ntok):
        R_ = lambda i, n=8: rt[:ntok, i, 0:n]
        rk = lambda i: ("rt", i)
        b = B.next_bank()
        for kc in range(8):
            B.mm(psum[b][:ntok, 0:36], h2Tf[:, kc, ti * 128:ti * 128 + ntok], wrt[:, kc, :], kc == 0, kc == 7,
                 r=h2Tf_k + ["wrt"], w=[("ps", b)])
        lg = rt[:ntok, 0, :]
        lgb = rt[:ntok, 1, :]
        B.copy(lg[:, 0:36] if False else rt[:ntok, 0:2, :].rearrange("p a b -> p (a b)")[:, 0:36], psum[b][:ntok, 0:36],
               r=[("ps", b)], w=[rk(0), rk(1)], eng="dve")
        lgf = rt[:ntok, 0:2, :].rearrange("p a b -> p (a b)")
        lbf = rt[:ntok, 2:4, :].rearrange("p a b -> p (a b)")
        B.tt(lbf[:, 0:36], lgf[:, 0:36], brt[:ntok, :], ALU.add, r=[rk(0), rk(1), "brt"], w=[rk(2), rk(3)])
        gl, glb = lgf[:, 0:4], lbf[:, 0:4]
        el = lgf[:, 4:36].rearrange("p (g e) -> p g e", e=8)
        elb = lbf[:, 4:36].rearrange("p (g e) -> p g e", e=8)
        sc = rt[:ntok, 4, :]
        sk = rk(4)
        B.red(sc[:, 0:1], gl, ALU.max, r=[rk(0)], w=[sk])
        B.ts(sc[:, 1:2], sc[:, 0:1], -1.0, None, ALU.mult, r=[sk], w=[sk])
        ex = rt[:ntok, 5, 0:4]
        B.act(ex, gl, AF.Exp, r=[rk(0), sk], w=[rk(5), sk], bias=sc[:, 1:2], accum_out=sc[:, 2:3])
        P.add("dve", lambda e: e.reciprocal(sc[:, 3:4], sc[:, 2:3]), r=[sk], w=[sk])
        B.red(sc[:, 4:5], glb, ALU.max, r=[rk(2)], w=[sk])
        ohg = rt[:ntok, 6, 0:4]
        B.ts(ohg, glb, sc[:, 4:5], None, ALU.is_equal, r=[rk(2), sk], w=[rk(6)])
        B.tt(ex, ex, ohg, ALU.mult, r=[rk(5), rk(6)], w=[rk(5)])
        B.red(sc[:, 5:6], ex, ALU.add, r=[rk(5)], w=[sk])
        B.ts(sc[:, 5:6], sc[:, 5:6], sc[:, 3:4], None, ALU.mult, r=[sk], w=[sk])
        ohg3 = ohg.unsqueeze(2).to_broadcast([ntok, 4, 8])
        tmp = rt[:ntok, 7, :].rearrange("p (g e) -> p g e", e=8)
        ing = rt[:ntok, 8, 0:8]
        B.tt(tmp, el, ohg3, ALU.mult, r=[rk(0), rk(1), rk(6)], w=[rk(7)])
        B.red(ing, tmp.rearrange("p g e -> p e g"), ALU.add, r=[rk(7)], w=[rk(8)])
        selb = rt[:ntok, 9, 0:8]
        B.tt(tmp, elb, ohg3, ALU.mult, r=[rk(2), rk(3), rk(6)], w=[rk(7)])
        B.red(selb, tmp.rearrange("p g e -> p e g"), ALU.add, r=[rk(7)], w=[rk(9)])
        oh1, oh2, sel2 = rt[:ntok, 10, 0:8], rt[:ntok, 11, 0:8], rt[:ntok, 12, 0:8]
        B.red(sc[:, 6:7], selb, ALU.max, r=[rk(9)], w=[sk])
        B.ts(oh1, selb, sc[:, 6:7], None, ALU.is_equal, r=[rk(9), sk], w=[rk(10)])
        B.stt(sel2, oh1, -1.0e30, selb, ALU.mult, ALU.add, r=[rk(10), rk(9)], w=[rk(12)])
        B.red(sc[:, 7:8], sel2, ALU.max, r=[rk(12)], w=[sk])
        B.ts(oh2, sel2, sc[:, 7:8], None, ALU.is_equal, r=[rk(12), sk], w=[rk(11)])
        t8 = rt[:ntok, 13, 0:8]
        B.tt(t8, oh1, ing, ALU.mult, r=[rk(10), rk(8)], w=[rk(13)])
        B.red(sc[:, 8:9], t8, ALU.add, r=[rk(13)], w=[sk])
        B.tt(t8, oh2, ing, ALU.mult, r=[rk(11), rk(8)], w=[rk(13)])
        B.red(sc[:, 9:10], t8, ALU.add, r=[rk(13)], w=[sk])
        B.tt(sc[:, 10:11], sc[:, 9:10], sc[:, 8:9], ALU.subtract, r=[sk], w=[sk])
        B.act(sc[:, 11:12], sc[:, 10:11], AF.Exp, r=[sk], w=[sk])
        B.ts(sc[:, 11:12], sc[:, 11:12], 1.0, None, ALU.add, r=[sk], w=[sk])
        P.add("dve", lambda e: e.reciprocal(sc[:, 11:12], sc[:, 11:12]), r=[sk], w=[sk])
        B.ts(sc[:, 12:13], sc[:, 11:12], -1.0, 1.0, ALU.mult, ALU.add, r=[sk], w=[sk])
        wsel = rt[:ntok, 14, 0:8]
        B.ts(wsel, oh1, sc[:, 11:12], None, ALU.mult, r=[rk(10), sk], w=[rk(14)])
        B.stt(wsel, oh2, sc[:, 12:13], wsel, ALU.mult, ALU.add, r=[rk(11), rk(14), sk], w=[rk(14)])
        B.ts(wsel, wsel, sc[:, 5:6], None, ALU.mult, r=[rk(14), sk], w=[rk(14)])
        B.tt(gates[:ntok, ti, :].rearrange("p (g e) -> p g e", e=8), ohg3, wsel.unsqueeze(1).to_broadcast([ntok, 4, 8]),
             ALU.mult, r=[rk(6), rk(14)], w=["gates"])

    def phaseC(row0, ntile, ntok, odram, orow0):
        N = (ntile - 1) * 128 + ntok
        B.bank_lim = 4
        B.psn = 0
        for e in range(3):
            load_expert(e)
        for ti in range(ntile):
            B.dma(yacc[:ntok, ti, :], sc_h2[row0 + ti * 128: row0 + ti * 128 + ntok, :],
                  r=[("sc_h2", (row0 + ti * 128) // 128)], w=yacc_k)
        for ti in range(ntile):
            transpose_tile(yacc[:, ti, :], yacc_k[0], h2Tf, h2Tf_k[0], D, tok0=ti * 128, ntok=ntok)
        B.copy(h2Tb[:, :, 0:N], h2Tf[:, :, 0:N], r=h2Tf_k, w=h2Tb_k, eng="dve")
        for ti in range(ntile):
            router(ti, ntok)
        P.add("act", lambda e: e.mul(yacc[:ntok, 0:ntile, :], yacc[:ntok, 0:ntile, :], float(ALPHA)), r=yacc_k + h2Tf_k, w=yacc_k)
        def gate_up(e):
            g_, u_, d_ = EW[e % 4]
            hTe, hTk = hTs[e % 2]
            for f in range(2):
                bg, bu = 2 * f, 2 * f + 1
                for kc in range(8):
                    B.mm(psum[bg][:, 0:N], g_.ap[:, kc, f * 128:(f + 1) * 128], h2Tb[:, kc, 0:N], kc == 0, kc == 7,
                         r=h2Tb_k + g_.keys, w=[("ps", bg)])
                for kc in range(8):
                    B.mm(psum[bu][:, 0:N], u_.ap[:, kc, f * 128:(f + 1) * 128], h2Tb[:, kc, 0:N], kc == 0, kc == 7,
                         r=h2Tb_k + u_.keys, w=[("ps", bu)])
                sgt, sgk = (zqk, "zqk") if f == 0 else (sr, "sr")
                B.act(sgt[:, 0:N], psum[bg][:, 0:N], AF.Silu, r=[("ps", bg)], w=[sgk])
                B.tt(hTe[:, f, 0:N], sgt[:, 0:N], psum[bu][:, 0:N], ALU.mult, r=[sgk, ("ps", bu)], w=hTk)

        def down(e):
            g_, u_, d_ = EW[e % 4]
            hTe, hTk = hTs[e % 2]
            for ti in range(ntile):
                for hf in range(2):
                    b = 4 + (B.next_bank())
                    for f in range(2):
                        B.mm(psum[b][:ntok, :], hTe[:, f, ti * 128:ti * 128 + ntok], d_.ap[:, f, hf * 512:(hf + 1) * 512],
                             f == 0, f == 1, r=hTk + d_.keys, w=[("ps", b)])
                    B.stt(yacc[:ntok, ti, hf * 512:(hf + 1) * 512], psum[b][:ntok, :], gates[:ntok, ti, e:e + 1],
                          yacc[:ntok, ti, hf * 512:(hf + 1) * 512], ALU.mult, ALU.add,
                          r=[("ps", b), "gates"] + yacc_k, w=yacc_k)

        gate_up(0)
        for e in range(32):
            if e + 3 < 32:
                load_expert(e + 3)
            if e + 1 < 32:
                gate_up(e + 1)
            down(e)
        for ti in range(ntile):
            yo = yout[ti % 2]
            pre_t = _Tile(yacc, ti, ntok)
            layer_norm(pre_t, yacc_k[0], 0, _Tile2(yo, ntok), yout_k[0], ntok=ntok)
            B.dma(odram[orow0 + ti * 128: orow0 + ti * 128 + ntok, :], yo[:ntok, :], r=yout_k, w=[("o_y", row0, ti)])
        B.bank_lim = 8

    if "C" in stages:
        barrier(SK + kT_keys + V_keys + yacc_k + h2Tf_k + h2Tb_k + hT_k + hT2_k + yout_k + ["hT_a", "hT_b"]
                + ["e1", "l1", "eb", "enb", "ekb", "wrt", "brt", "gates"] + [("rt", k_) for k_ in range(16)])
        B.dma(wrt[:], w_rt.rearrange("(kc p) n -> p kc n", p=128), w=["wrt"])
        B.dma(brt[:], _bc(b_rt, 36), w=["brt"])
        B.dma(lnp[0][:], _bc(ln_d[4], D), w=[("lnp", 0)])
        B.dma(lnp[1][:], _bc(ln_d[5], D), w=[("lnp", 1)])
        if "A" in stages:
            for blk in range(NT // 512):
                phaseC(blk * 512, 4, 128, o_y, blk * 512)
        if "S" in stages:
            phaseC(NT, 1, NS, o_ys, 0)

    if "dbg_h2" in stages:
        o_h2 = B.dout("o_h2", [NT, D])
        for tt_ in range(NT // 128):
            i = cnt["x"] % 2
            cnt["x"] += 1
            B.dma(xin[i][:], sc_h2[tt_ * 128:(tt_ + 1) * 128, :], r=[("sc_h2", tt_)], w=[("xin", i)])
            B.dma(o_h2[tt_ * 128:(tt_ + 1) * 128, :], xin[i][:], r=[("xin", i)], w=[("o_h2", tt_)])

    P.emit(nc, B.es)
    B.es.close()
    return nc


def make_consts():
    j = np.arange(128)[:, None]
    i = np.arange(128)[None, :]
    inv = (10000.0 ** (-np.arange(0, 32, 2, dtype=np.float32) / 32.0)).astype(np.float32)
    pos = (np.arange(16)[None, :] * 128 + np.arange(128)[:, None]).astype(np.float32)
    ang = pos[:, :, None] * inv[None, None, :]
    ang_s = (np.float32(8192.0) * inv)[None, :].repeat(NS, 0)
    return {
        "c_pcol": (np.arange(128) % 8).astype(np.float32).reshape(128, 1),
        "c_r8": (np.arange(128)[None, :] // 8 == np.arange(16)[:, None]).astype(np.float32),
        "c_i16": np.tile(np.eye(16, dtype=np.float32).reshape(1, 256), (64, 1)),
        "c_cos_s": np.cos(ang_s).astype(np.float32),
        "c_sin_s": np.sin(ang_s).astype(np.float32),
        "c_bm8": (np.arange(512)[None, :] // 64 == np.arange(8)[:, None]).astype(np.float32),
        "c_bm4": (np.arange(1024)[None, :] // 256 == np.arange(4)[:, None]).astype(np.float32),
        "ident": np.eye(128, dtype=np.float32),
        "c_us": np.where(j <= i, -1.0 / 16.0, 0.0).astype(np.float32),
        "c_lw": np.where(j > i, -1.0 / 16.0, 0.0).astype(np.float32),
        "c_mask": (j <= i).astype(np.float32),
        "c_cos": np.cos(ang).astype(np.float32),
        "c_sin": np.sin(ang).astype(np.float32),
    }


_NC_CACHE = {}


def _program():
    if "nc" not in _NC_CACHE:
        _NC_CACHE["nc"] = build(("memkv", "A", "B", "S", "C"))
    return _NC_CACHE["nc"]


def kernel(**inputs):
    f32 = lambda a: np.ascontiguousarray(np.asarray(a, dtype=np.float32))
    g = {k: np.asarray(v) for k, v in inputs.items()}
    consts = make_consts()
    shared = dict(consts)
    shared["w_mk"] = f32(g["w_mk"][0])
    shared["w_mv"] = f32(g["w_mv"][0])
    shared["w_in"] = f32(g["w_in"][0])
    shared["w_gate_aug"] = f32(np.concatenate([g["w_gla_gate"][0], g["b_gla_gate"]], axis=0))
    shared["gla_norm_g"] = f32(g["gla_norm_g"])
    shared["mla_q_norm_g"] = f32(g["mla_q_norm_g"])
    shared["mla_kv_norm_g"] = f32(g["mla_kv_norm_g"])
    shared["w_uq"] = f32(g["w_uq"][0].reshape(384, 768))
    shared["w_uk"] = f32(g["w_uk"][0].reshape(256, 512))
    shared["w_uv"] = f32(g["w_uv"][0].reshape(256, 512))
    shared["w_out"] = f32(g["w_out"][0])
    shared["w_xq"] = f32(g["w_xq"][0])
    shared["w_xo"] = f32(g["w_xo"][0])
    for i, k in enumerate(("ln1_g", "ln1_b", "ln2_g", "ln2_b", "ln3_g", "ln3_b")):
        shared["ln%d" % i] = f32(g[k])
    shared["w_rt"] = f32(np.concatenate([g["w_grp"][0], g["w_rtr"][0]], axis=1))
    shared["b_rt"] = f32(np.concatenate([g["b_grp"], g["b_rtr"]], axis=1))
    shared["w_e_gate"] = f32(g["w_e_gate"][0])
    shared["w_e_up"] = f32(g["w_e_up"][0])
    shared["w_e_down"] = f32(g["w_e_down"][0])
    shared["cache_lat"] = f32(g["cache_latent"][0].reshape(-1, 256))
    shared["cache_kr"] = f32(g["cache_krope"][0].reshape(-1, 32))
    in_maps = []
    for c in range(NCORES):
        m = dict(shared)
        m["xp"] = f32(g["x_prompt"][NSEQ * c:NSEQ * (c + 1)].reshape(NSEQ * SEQ, D))
        m["memp"] = f32(g["mem_prompt"][NSEQ * c:NSEQ * (c + 1)].reshape(NSEQ * 256, D))
        m["xs"] = f32(g["x_sample"][NS * c:NS * (c + 1), 0])
        m["sgla_s"] = f32(g["state_gla"][0, NS * c:NS * (c + 1)])
        m["cmk"] = f32(g["cache_mem_k"][0, NS * c:NS * (c + 1)].reshape(NS, 256, D))
        m["cmv"] = f32(g["cache_mem_v"][0, NS * c:NS * (c + 1)].reshape(NS, 256, D))
        m["pt"] = np.ascontiguousarray(g["page_table"][NS * c:NS * (c + 1)].reshape(1, NS * NPAGE).astype(np.int32))
        in_maps.append(m)
    nc = _program()
    res = run_bass_kernel_spmd(nc, in_maps, core_ids=list(range(NCORES)))
    R = res.results
    cat = lambda name: np.concatenate([np.asarray(R[c][name]) for c in range(NCORES)], axis=0)
    y_prompt = cat("o_y").reshape(16, SEQ, D)
    y_sample = cat("o_ys").reshape(128, 1, D)
    sgla_p = cat("o_sgla").reshape(1, 16, 4, 64, 128)
    lat_p = cat("o_lat").reshape(1, 16, SEQ, 256)
    kr_p = cat("o_kr").reshape(1, 16, SEQ, 32)
    mk_p = cat("o_memk").reshape(1, 16, 256, 4, 256)
    mv_p = cat("o_memv").reshape(1, 16, 256, 4, 256)
    sgla_s = cat("o_sgla_s").reshape(1, 128, 4, 64, 128)
    lat_s = cat("o_lat_s").reshape(1, 128, 1, 256)
    kr_s = cat("o_kr_s").reshape(1, 128, 1, 32)
    outs = (y_prompt, y_sample, sgla_p, lat_p, kr_p, mk_p, mv_p, sgla_s, lat_s, kr_s)
    return tuple(np.ascontiguousarray(o.astype(np.float32)) for o in outs)
```

```python
import numpy as np
from contextlib import ExitStack
import concourse.bass as bass
import concourse.mybir as mybir
from concourse.bass_utils import run_bass_kernel_spmd

F32 = mybir.dt.float32
BF16 = mybir.dt.bfloat16
I32 = mybir.dt.int32
AF = mybir.ActivationFunctionType
ALU = mybir.AluOpType
AX = mybir.AxisListType

NCORES = 8
D = 1024
SEQ = 2048
NSEQ = 2
NS = 16
NPAGE = 64
ALPHA = 2.0 ** 0.25
LN_EPS = 1e-5
RMS_EPS = 1e-6
MLA_SCALE = 96.0 ** -0.5


class _Op:
    __slots__ = ("idx", "eng", "fn", "deps", "dma", "signal", "sem", "val", "prev")

    def __init__(self, idx, eng, fn, dma):
        self.idx, self.eng, self.fn, self.dma = idx, eng, fn, dma
        self.deps = set()
        self.signal = False
        self.sem = None
        self.val = 0
        self.prev = 0


class Prog:
    ENGS = ("pe", "act", "dve", "pool", "sp")
    NDS = 48

    def __init__(self):
        self.ops = []
        self.lastw = {}
        self.readers = {}

    def add(self, eng, fn, r=(), w=(), dma=False):
        op = _Op(len(self.ops), eng, fn, dma)
        xs = [k for k in r if isinstance(k, tuple) and k and k[0] == "ps"]
        if xs:
            r = [k for k in r if k not in xs]
            w = list(w) + [k for k in xs if k not in w]
        for k in r:
            lw = self.lastw.get(k)
            if lw is not None:
                op.deps.add(lw)
        for k in w:
            lw = self.lastw.get(k)
            if lw is not None:
                op.deps.add(lw)
            rs = self.readers.get(k)
            if rs:
                op.deps.update(rs)
        for k in r:
            self.readers.setdefault(k, []).append(op.idx)
        for k in w:
            self.lastw[k] = op.idx
            self.readers[k] = []
        op.deps.discard(op.idx)
        self.ops.append(op)
        return op

    def emit(self, nc, es):
        ops = self.ops
        esem = {e: es.enter_context(nc.semaphore("se_" + e)) for e in self.ENGS}
        dsem = [es.enter_context(nc.semaphore("sd_%d" % i)) for i in range(self.NDS)]
        qrange = {"sp": (0, self.NDS - 12), "act": (0, self.NDS - 12), "pool": (self.NDS - 12, self.NDS)}
        qnext = {q: lo for q, (lo, hi) in qrange.items()}

        def pruned(op, dop):
            return (not op.dma) and (not dop.dma) and op.eng == "pe" and dop.eng == "pe"

        for op in ops:
            for d in op.deps:
                if not pruned(op, ops[d]):
                    ops[d].signal = True
        cnt = {e: 0 for e in self.ENGS}
        dcnt = [0] * self.NDS
        dn = 0
        for op in ops:
            if op.dma:
                qk = "pool" if op.eng == "pool" else "sp"
                dn = qnext[qk]
                lo, hi = qrange[qk]
                qnext[qk] = lo + (dn + 1 - lo) % (hi - lo)
                op.sem = dsem[dn]
                op.prev = dcnt[dn]
                dcnt[dn] += 16
                op.val = dcnt[dn]
            elif op.signal:
                cnt[op.eng] += 1
                op.sem = esem[op.eng]
                op.val = cnt[op.eng]
        cuts = sorted(set([0] + [c for c in getattr(self, "cuts", []) if 0 < c < len(ops)] + [len(ops)]))
        waited = {e: {} for e in self.ENGS}

        def body_for(e, lo, hi, final):
            def body(engh):
                wd = waited[e]
                for op in ops[lo:hi]:
                    if op.eng != e:
                        continue
                    needs = {}
                    for d in op.deps:
                        dop = ops[d]
                        if pruned(op, dop):
                            continue
                        key = id(dop.sem)
                        if key not in needs or needs[key][1] < dop.val:
                            needs[key] = (dop.sem, dop.val)
                    if op.dma and op.prev > 0:
                        key = id(op.sem)
                        if key not in needs or needs[key][1] < op.prev:
                            needs[key] = (op.sem, op.prev)
                    for key, (sem, val) in needs.items():
                        if wd.get(key, 0) < val:
                            engh.wait_ge(sem, val)
                            wd[key] = val
                    ins = op.fn(engh)
                    if op.dma:
                        ins.then_inc(op.sem, 16)
                    elif op.signal:
                        ins.then_inc(op.sem, 1)
                if e == "sp" and final:
                    for i in range(self.NDS):
                        if dcnt[i] > 0:
                            engh.wait_ge(dsem[i], dcnt[i])
            return body

        for ci in range(len(cuts) - 1):
            lo, hi = cuts[ci], cuts[ci + 1]
            final = ci == len(cuts) - 2
            with nc.Block() as block:
                block.tensor(body_for("pe", lo, hi, final))
                block.scalar(body_for("act", lo, hi, final))
                block.vector(body_for("dve", lo, hi, final))
                block.gpsimd(body_for("pool", lo, hi, final))
                block.sync(body_for("sp", lo, hi, final))


class Builder:
    def __init__(self, stages):
        self.stages = stages
        self.nc = bass.Bass("TRN2", target_bir_lowering=False)
        self.es = ExitStack()
        self.P = Prog()
        self.psn = 0
        self.bank_lim = 8
        self.ptn = 0
        self.ev = 0

    def din(self, name, shape, dt=F32):
        return self.nc.dram_tensor(name, list(shape), dt, kind="ExternalInput").ap()

    def dout(self, name, shape, dt=F32):
        return self.nc.dram_tensor(name, list(shape), dt, kind="ExternalOutput").ap()

    def sb(self, name, shape, dt):
        return self.es.enter_context(self.nc.sbuf_tensor("sb_" + name, list(shape), dt))

    def next_bank(self):
        b = self.psn % self.bank_lim
        self.psn = (b + 1) % self.bank_lim
        return b

    def next_tbank(self):
        b = self.ptn
        self.ptn = (self.ptn + 1) % 2
        return b

    def dma(self, out, in_, r=(), w=(), q="sp"):
        self.P.add(q, lambda e: e.dma_start(out=out, in_=in_), r=r, w=w, dma=True)

    def mm(self, out, lhsT, rhs, start, stop, r=(), w=()):
        self.P.add("pe", lambda e: e.matmul(out, lhsT, rhs, start=start, stop=stop), r=r, w=w)

    def tr(self, out, in_, ident, r=(), w=()):
        self.P.add("pe", lambda e: e.transpose(out, in_, ident), r=r, w=w)

    def act(self, out, in_, func, r=(), w=(), **kw):
        self.P.add("act", lambda e: e.activation(out, in_, func, **kw), r=r, w=w)

    def copy(self, out, in_, r=(), w=(), eng=None):
        if eng is None:
            eng = ("dve", "act")[self.ev % 2]
            self.ev += 1
        if eng == "act":
            self.P.add("act", lambda e: e.copy(out, in_), r=r, w=w)
        else:
            self.P.add(eng, lambda e: e.tensor_copy(out, in_), r=r, w=w)

    def tt(self, out, in0, in1, op, r=(), w=(), eng="dve"):
        self.P.add(eng, lambda e: e.tensor_tensor(out, in0, in1, op), r=r, w=w)

    def ts(self, out, in0, s1, s2, op0, op1=ALU.bypass, r=(), w=(), eng="dve"):
        if s2 is None:
            self.P.add(eng, lambda e: e.tensor_scalar(out, in0, s1, None, op0), r=r, w=w)
        else:
            self.P.add(eng, lambda e: e.tensor_scalar(out, in0, s1, s2, op0, op1), r=r, w=w)

    def stt(self, out, in0, scalar, in1, op0, op1, r=(), w=()):
        self.P.add("dve", lambda e: e.scalar_tensor_tensor(out, in0, scalar, in1, op0, op1), r=r, w=w)

    def rsqrt(self, out, in_, c, tmp, r=(), w=(), tk="rsq_tmp"):
        self.P.add("act", lambda e: e.activation(tmp, in_, AF.Sqrt, bias=float(c)), r=r, w=[tk])
        self.P.add("dve", lambda e: e.reciprocal(out, tmp), r=[tk], w=w)

    def red(self, out, in_, op, r=(), w=()):
        self.P.add("dve", lambda e: e.tensor_reduce(out, in_, AX.X, op), r=r, w=w)


def _bc(ap, n):
    return bass.AP(ap.tensor, ap.offset, [[0, 128], [1, n]])


class AV:
    CH = 2048

    def __init__(self, arena, off, shape):
        n = int(np.prod(shape))
        a = arena[:, off:off + n]
        if len(shape) == 2:
            a = a.rearrange("p (a b) -> p a b", b=shape[1])
        self.ap = a
        self.keys = [("W", c) for c in range(off // self.CH, (off + n - 1) // self.CH + 1)]


C_Q, C_K, C_V, C_A, C_R, C_CQ, C_CKV, C_KR = 0, 256, 512, 1024, 1040, 1552, 1936, 2192


def build(stages=("memkv", "A")):
    B = Builder(stages)
    nc, P = B.nc, B.P
    NT = NSEQ * SEQ

    xp = B.din("xp", [NT, D])
    memp = B.din("memp", [NSEQ * 256, D])
    ident_d = B.din("ident", [128, 128])
    us_d = B.din("c_us", [128, 128])
    lw_d = B.din("c_lw", [128, 128])
    mask_d = B.din("c_mask", [128, 128])
    cos_d = B.din("c_cos", [128, 16, 16])
    sin_d = B.din("c_sin", [128, 16, 16])
    w_mk = B.din("w_mk", [D, D])
    w_mv = B.din("w_mv", [D, D])
    w_in = B.din("w_in", [D, 2224])
    wgate_d = B.din("w_gate_aug", [17, 256])
    ggla_d = B.din("gla_norm_g", [1, 128])
    gq_d = B.din("mla_q_norm_g", [1, 384])
    gkv_d = B.din("mla_kv_norm_g", [1, 256])
    w_uq = B.din("w_uq", [384, 768])
    w_uk = B.din("w_uk", [256, 512])
    w_uv = B.din("w_uv", [256, 512])
    w_out = B.din("w_out", [D, D])
    w_xq = B.din("w_xq", [D, D])
    w_xo = B.din("w_xo", [D, D])
    ln_d = [B.din("ln%d" % i, [1, D]) for i in range(6)]
    sc_h2 = nc.dram_tensor("sc_h2", [NT + NS, D], F32).ap()
    w_rt = B.din("w_rt", [D, 36])
    b_rt = B.din("b_rt", [1, 36])
    w_eg = B.din("w_e_gate", [32, D, 256])
    w_eu = B.din("w_e_up", [32, D, 256])
    w_ed = B.din("w_e_down", [32, 256, D])
    xs = B.din("xs", [NS, D])
    sgla_s = B.din("sgla_s", [NS, 4, 64, 128])
    NPHYS = 10240
    c_lat = B.din("cache_lat", [NPHYS * 128, 256])
    c_kr = B.din("cache_kr", [NPHYS * 128, 32])
    cmk = B.din("cmk", [NS, 256, D])
    cmv = B.din("cmv", [NS, 256, D])
    pt_d = B.din("pt", [1, NS * NPAGE], I32)
    pcol_d = B.din("c_pcol", [128, 1])
    i16_d = B.din("c_i16", [64, 256])
    coss_d = B.din("c_cos_s", [NS, 16])
    sins_d = B.din("c_sin_s", [NS, 16])
    bm8_d = B.din("c_bm8", [8, 512])
    bm4_d = B.din("c_bm4", [4, 1024])
    r8_d = B.din("c_r8", [16, 128])
    sc_lat = nc.dram_tensor("sc_lat", [NS * NPAGE * 128, 256], F32).ap()
    sc_kr = nc.dram_tensor("sc_kr", [NS * NPAGE * 128, 32], F32).ap()
    o_sgla_s = B.dout("o_sgla_s", [NS, 4, 64, 128])
    o_lat_s = B.dout("o_lat_s", [NS, 256])
    o_kr_s = B.dout("o_kr_s", [NS, 32])
    o_y = B.dout("o_y", [NT, D])
    o_ys = B.dout("o_ys", [NS, D])
    o_memk = B.dout("o_memk", [NSEQ * 256, D])
    o_memv = B.dout("o_memv", [NSEQ * 256, D])
    o_lat = B.dout("o_lat", [NT, 256])
    o_kr = B.dout("o_kr", [NT, 32])
    o_sgla = B.dout("o_sgla", [NSEQ, 4, 64, 128])
    sc_q = nc.dram_tensor("sc_q", [NT, 768], BF16).ap()
    sc_go = nc.dram_tensor("sc_go", [NT, 512], BF16).ap()

    psum = [B.es.enter_context(nc.psum_tensor("ps%d" % i, [128, 512], F32)) for i in range(8)]
    ident = B.sb("ident", [128, 128], F32)
    identb = B.sb("identb", [128, 128], BF16)
    c_us = B.sb("c_us", [128, 128], F32)
    c_lw = B.sb("c_lw", [128, 128], F32)
    c_mask = B.sb("c_mask", [128, 128], F32)
    cosT = B.sb("cosT", [128, 16, 16], F32)
    sinT = B.sb("sinT", [128, 16, 16], F32)
    arena = B.sb("arena", [128, 24576], BF16)
    WmkA = AV(arena, 0, [8, 1024])
    WmvA = AV(arena, 8192, [8, 1024])
    wA, wB = WmkA.ap, WmvA.ap
    xin = [B.sb("xin%d" % i, [128, D], F32) for i in range(2)]
    xT = [B.sb("xT%d" % i, [128, 8, 128], BF16) for i in range(2)]
    stage = [B.sb("stage%d" % i, [128, D], F32) for i in range(2)]
    memT = B.sb("memT", [128, 8, 256], BF16)
    memkT = B.sb("memkT", [128, NSEQ, 8, 256], BF16)
    memv = B.sb("memv", [128, NSEQ, 2, 4, 260], BF16)
    arenaS = B.sb("arenaS", [128, 12800], F32)

    def sview(off, n, dt=F32, parts=128):
        a = arenaS[0:parts, off:off + n]
        if dt != F32:
            a = a.bitcast(dt)
        return a, [("S", c) for c in range(off // 1024, (off + n - 1) // 1024 + 1)]
    kT_flat, kT_keys = sview(0, 8192, BF16, 96)
    kT_seq = kT_flat.rearrange("p (h t) -> p h t", t=SEQ)
    V_flat, V_keys = sview(8192, 4224, BF16)
    V_seq = V_flat.rearrange("p (a h d) -> p a h d", h=8, d=66)
    Sst = B.sb("Sst", [64, 4, 128], F32)
    Sbf = B.sb("Sbf", [64, 4, 128], BF16)
    wgate = B.sb("wgate", [17, 256], F32)
    ggla = B.sb("ggla", [128, 128], F32)
    gq = B.sb("gq", [128, 384], F32)
    gkv = B.sb("gkv", [128, 256], F32)
    aT = B.sb("aT", [32, 128], F32)
    zqk = B.sb("zqk", [128, 512], F32)
    vbf = B.sb("vbf", [128, 512], BF16)
    sr = B.sb("sr", [128, 512], F32)
    cq = B.sb("cq", [128, 384], F32)
    ze = B.sb("ze", [128, 288], F32)
    gtmp = B.sb("gtmp", [128, 5, 256], F32)
    e1, l1, eb, enb, ekb = [gtmp[:, k_, :] for k_ in range(5)]
    eblT = B.sb("eblT", [64, 4], F32)
    qt = B.sb("qt", [128, 256], BF16)
    kt = B.sb("kt", [128, 256], BF16)
    kp = B.sb("kp", [128, 256], BF16)
    qkT = B.sb("qkT", [64, 8, 128], BF16)
    ATm = B.sb("ATm", [128, 4, 128], BF16)
    osb = B.sb("osb", [128, 4, 128], F32)
    junk = B.sb("junk", [128, 512], F32)
    ss4 = B.sb("ss4", [128, 4], F32)
    rs4 = B.sb("rs4", [128, 4], F32)
    big1 = B.sb("big1", [128, 1024], F32)
    t1 = big1[:, 0:512].rearrange("p (a b) -> p a b", b=128)
    SG = big1[:, 512:1024].rearrange("p (a b) -> p a b", b=128)
    gobf = B.sb("gobf", [128, 512], BF16)
    rsq = B.sb("rsq", [128, 8], F32)
    sskv = B.sb("sskv", [128, 2], F32)
    rskv = B.sb("rskv", [128, 2], F32)
    ckvn = B.sb("ckvn", [128, 256], F32)
    ckvT = B.sb("ckvT", [128, 2, 128], BF16)
    cqn = B.sb("cqn", [128, 384], F32)
    cqnT = B.sb("cqnT", [128, 3, 128], BF16)
    qtm = B.sb("qtm", [128, 8, 96], F32)
    qbf = B.sb("qbf", [128, 768], BF16)
    kpe = B.sb("kpe", [128, 96], F32)
    rp = [B.sb("rp%d" % i, [128, 8, 16], F32) for i in range(4)]

    lnp = [B.sb("lnp%d" % i, [128, D], F32) for i in range(4)]
    maskb = B.sb("maskb", [128, 128], BF16)
    rec8 = B.sb("rec8", [128, 8], F32)
    lnst = B.sb("lnst", [128, 2, 6], F32)
    lnmv = B.sb("lnmv", [128, 2], F32)
    lnrs = B.sb("lnrs", [128, 2], F32)
    r8 = B.sb("r8", [16, 128], F32)
    ptq = B.sb("ptq", [16, 64], I32)
    ptqf = B.sb("ptqf", [16, 64], F32)
    idxq = B.sb("idxq", [128, 64], I32)
    idxk = B.sb("idxk", [64, 16], I32)
    pcol = B.sb("pcol", [128, 1], F32)
    i16 = B.sb("i16", [64, 16, 16], F32)
    cos_s = B.sb("cos_s", [NS, 16], F32)
    sin_s = B.sb("sin_s", [NS, 16], F32)
    bm8 = B.sb("bm8", [8, 512], F32)
    bm4 = memkT[:].rearrange("p a b c -> p (a b c)")[0:4, 0:2048].bitcast(F32)
    rt = gtmp[:, 0:2, :].rearrange("p a (b c) -> p (a b) c", c=32)
    wrt = gtmp[:, 2:4, :].rearrange("p a b -> p (a b)")[:, 0:288].rearrange("p (k n) -> p k n", n=36)
    gates = gtmp[:, 4, 0:128].rearrange("p (t e) -> p t e", e=32)
    brt = gtmp[:, 4, 128:164]
    B.dma(ident[:], ident_d, w=["ident"])
    B.dma(c_us[:], us_d, w=["c_us"])
    B.dma(c_lw[:], lw_d, w=["c_lw"])
    B.dma(c_mask[:], mask_d, w=["c_mask"])
    B.dma(cosT[:], cos_d, w=["cosT"])
    B.dma(sinT[:], sin_d, w=["sinT"])
    B.dma(wgate[:], wgate_d, w=["wgate"])
    B.dma(ggla[:], _bc(ggla_d, 128), w=["ggla"])
    B.dma(gq[:], _bc(gq_d, 384), w=["gq"])
    B.dma(gkv[:], _bc(gkv_d, 256), w=["gkv"])
    B.dma(pcol[:], pcol_d, w=["pcol"])
    B.dma(i16[:].rearrange("p a b -> p (a b)"), i16_d, w=["i16"])
    B.dma(cos_s[:], coss_d, w=["cos_s"])
    B.dma(sin_s[:], sins_d, w=["sin_s"])
    B.dma(bm8[:], bm8_d, w=["bm8"])
    B.copy(identb[:], ident[:], r=["ident"], w=["identb"], eng="dve")
    B.copy(maskb[:], c_mask[:], r=["c_mask"], w=["maskb"], eng="dve")
    for i in range(4):
        B.dma(lnp[i][:], _bc(ln_d[i], D), w=[("lnp", i)])
    P.add("act", lambda e: e.mul(ggla[:], ggla[:], float(128.0 ** 0.5)), r=["ggla"], w=["ggla"])
    P.add("act", lambda e: e.mul(gq[:], gq[:], float(384.0 ** 0.5)), r=["gq"], w=["gq"])
    P.add("act", lambda e: e.mul(gkv[:], gkv[:], float(256.0 ** 0.5)), r=["gkv"], w=["gkv"])
    P.add("dve", lambda e: e.memset(aT[:], 1.0), w=["aT"])
    P.add("dve", lambda e: e.memset(kpe[:], 0.0), w=["kpe"])

    def load_w(dst, src, keys, q="pool", kcn=8, step=4):
        v = src.rearrange("(kc p) n -> p kc n", p=128)
        for kc0 in range(0, kcn, step):
            kc1 = min(kcn, kc0 + step)
            B.dma(dst[:, kc0:kc1, :], v[:, kc0:kc1, :], w=keys, q=q)

    cnt = {"x": 0, "st": 0}

    def transpose_tile(src_tile, src_key, dstT, dst_key, ncols, tok0=0, ntok=128):
        nchunk = ncols // 128
        for g0 in range(0, nchunk, 4):
            g1 = min(nchunk, g0 + 4)
            b = B.next_bank()
            for c in range(g0, g1):
                B.tr(psum[b][:, (c - g0) * 128:(c - g0) * 128 + ntok],
                     src_tile[:ntok, c * 128:(c + 1) * 128], ident[:ntok, :ntok],
                     r=[src_key, "ident"], w=[("ps", b)])
            pv = psum[b][:, 0:(g1 - g0) * 128].rearrange("p (c t) -> p c t", t=128)
            B.copy(dstT[:, g0:g1, tok0:tok0 + ntok], pv[:, :, :ntok], r=[("ps", b)], w=[dst_key])


    def barrier(keys):
        P.add("dve", lambda e: e.memset(rsq[:, 7:8], 0.0), r=[], w=list(keys) + ["rsq_bar"])

    def pregather():
        P.add("sp", lambda e: e.dma_start(out=ptq[:], in_=bass.AP(pt_d.tensor, pt_d.offset, [[1, 16], [16, 64]]),
                                          allow_slow_non_contiguous=True), w=["ptq"], dma=True)
        P.add("sp", lambda e: e.dma_start(out=idxk[:], in_=bass.AP(pt_d.tensor, pt_d.offset, [[1, 64], [64, 16]]),
                                          allow_slow_non_contiguous=True), w=["idxk"], dma=True)
        B.dma(r8[:], r8_d, w=["r8"])
        B.copy(ptqf[:], ptq[:], r=["ptq"], w=["ptqf"], eng="dve")
        B.mm(psum[0][:, 0:64], r8[:], ptqf[:], True, True, r=["r8", "ptqf"], w=[("ps", 0)])
        B.ts(idxq[:], psum[0][:, 0:64], 8.0, pcol[:, 0:1], ALU.mult, ALU.add, r=[("ps", 0), "pcol"], w=["idxq"])
        LB = [arenaS[:, 0:4096], arenaS[:, 4096:8192]]
        KB = arenaS[0:64, 8192:12288]
        lat8 = c_lat.rearrange("(n t) r -> n (t r)", t=16)
        kr1 = c_kr.rearrange("(n t) d -> n (t d)", t=128)
        k_ = 0
        for bb in range(NS):
            for g_ in range(4):
                sl = k_ % 2
                k_ += 1
                col = bb * 4 + g_
                P.add("pool", lambda e, sl=sl, col=col: e.indirect_dma_start(
                    out=LB[sl], out_offset=None, in_=lat8,
                    in_offset=bass.IndirectOffsetOnAxis(ap=idxq[:, col:col + 1], axis=0)), r=["idxq"], w=[("pre_LB", sl)], dma=True)
                row0 = (bb * NPAGE + g_ * 16) * 128
                B.dma(sc_lat[row0:row0 + 2048, :].rearrange("(p t) r -> p (t r)", t=16), LB[sl], r=[("pre_LB", sl)], w=[("sc_lat", bb)])
            P.add("pool", lambda e, bb=bb: e.indirect_dma_start(
                out=KB, out_offset=None, in_=kr1,
                in_offset=bass.IndirectOffsetOnAxis(ap=idxk[:, bb:bb + 1], axis=0)), r=["idxk"], w=["pre_KB"], dma=True)
            row0 = bb * NPAGE * 128
            B.dma(sc_kr[row0:row0 + NPAGE * 128, :].rearrange("(p t) d -> p (t d)", t=128), KB, r=["pre_KB"], w=[("sc_kr", bb)])
        barrier([("pre_LB", 0), ("pre_LB", 1), "pre_KB"] + kT_keys + V_keys)

    if "S" in stages:
        pregather()

    if "memkv" in stages:
        P.add("dve", lambda e: e.memset(memv[:].rearrange("p a b c d -> p (a b c d)"), 1.0), w=["memv"])
        load_w(wA, w_mk, WmkA.keys)
        load_w(wB, w_mv, WmvA.keys)
        for s in range(NSEQ):
            for mt in range(2):
                i = cnt["x"] % 2
                cnt["x"] += 1
                B.dma(xin[i][:], memp[s * 256 + mt * 128: s * 256 + (mt + 1) * 128, :], w=[("xin", i)])
                transpose_tile(xin[i], ("xin", i), memT, "memT", D, tok0=mt * 128)
            for wi, (wt, wkey, odram) in enumerate(((wA, WmkA.keys, o_memk), (wB, WmvA.keys, o_memv))):
                for mt in range(2):
                    si = cnt["st"] % 2
                    cnt["st"] += 1
                    for half in range(2):
                        b = B.next_bank()
                        for kc in range(8):
                            B.mm(psum[b][:, :], memT[:, kc, mt * 128:(mt + 1) * 128],
                                 wt[:, kc, half * 512:(half + 1) * 512], kc == 0, kc == 7,
                                 r=["memT"] + wkey, w=[("ps", b)])
                        B.copy(stage[si][:, half * 512:(half + 1) * 512], psum[b][:, :],
                               r=[("ps", b)], w=[("stage", si)])
                        if wi == 1:
                            B.copy(memv[:, s, mt, half * 2:half * 2 + 2, 0:256],
                                   psum[b][:, :].rearrange("p (h d) -> p h d", d=256),
                                   r=[("ps", b)], w=["memv"])
                    B.dma(odram[s * 256 + mt * 128: s * 256 + (mt + 1) * 128, :], stage[si][:],
                          r=[("stage", si)], w=[("o", wi, s, mt)])
            for j in range(8):
                b = B.next_bank()
                for kc in range(8):
                    B.mm(psum[b][:, 0:256], wA[:, kc, j * 128:(j + 1) * 128], memT[:, kc, :],
                         kc == 0, kc == 7, r=["memT"] + WmkA.keys, w=[("ps", b)])
                B.copy(memkT[:, s, j, :], psum[b][:, 0:256], r=[("ps", b)], w=["memkT"])

    Wqk = AV(arena, 0, [8, 512])
    Wv = AV(arena, 4096, [8, 512])
    Wr = AV(arena, 8192, [8, 512])
    Wcq = AV(arena, 12288, [8, 384])
    We = AV(arena, 15360, [8, 288])
    Wa = AV(arena, 17664, [8, 16])
    Wuq = AV(arena, 17792, [3, 768])
    Wuk = AV(arena, 20096, [2, 512])
    Wuv = AV(arena, 21120, [2, 512])

    def bc_h(ap2d, nh):
        return ap2d.unsqueeze(1).to_broadcast([ap2d.shape[0], nh, ap2d.shape[1]])

    def rope(x1, x2, o1, o2, cs, sn, nh, rk, wk, ntok=128, ck="cosT", sk_="sinT"):
        c3, s3 = bc_h(cs, nh), bc_h(sn, nh)
        t = [rp[i][:ntok, 0:nh, :] for i in range(4)]
        B.tt(t[0], x1, c3, ALU.mult, r=rk + [ck], w=["rp0"])
        B.tt(t[1], x2, s3, ALU.mult, r=rk + [sk_], w=["rp1"])
        B.tt(t[2], x1, s3, ALU.mult, r=rk + [sk_], w=["rp2"])
        B.tt(t[3], x2, c3, ALU.mult, r=rk + [ck], w=["rp3"])
        B.tt(o1, t[0], t[1], ALU.subtract, r=["rp0", "rp1"], w=wk)
        B.tt(o2, t[2], t[3], ALU.add, r=["rp2", "rp3"], w=wk)

    def load_A_weights():
        wi = w_in
        for (av, c0, n) in ((Wqk, 0, 512), (Wv, C_V, 512), (Wr, C_R, 512), (Wcq, C_CQ, 384),
                            (We, C_CKV, 288), (Wa, C_A, 16)):
            load_w(av.ap, wi[:, c0:c0 + n], av.keys)
        load_w(Wuq.ap, w_uq, Wuq.keys, kcn=3)
        load_w(Wuk.ap, w_uk, Wuk.keys, kcn=2)
        load_w(Wuv.ap, w_uv, Wuv.keys, kcn=2)

    def phaseA(s):
        load_A_weights()
        P.add("dve", lambda e: e.memset(Sst[:].rearrange("p a b -> p (a b)"), 0.0), w=["Sst"])
        P.add("dve", lambda e: e.memset(Sbf[:].rearrange("p a b -> p (a b)"), 0.0), w=["Sbf"])
        P.add("dve", lambda e: e.memset(V_flat, 1.0), w=V_keys)
        for t in range(16):
            tok0 = s * SEQ + t * 128
            tsl = slice(t * 128, (t + 1) * 128)
            i = cnt["x"] % 2
            cnt["x"] += 1
            xk, xtk = ("xin", i), ("xT", i)
            B.dma(xin[i][:], xp[tok0:tok0 + 128, :], w=[xk])
            transpose_tile(xin[i], xk, xT[i], xtk, D)

            def zgroup(av, n):
                b = B.next_bank()
                for kc in range(8):
                    B.mm(psum[b][:, 0:n], xT[i][:, kc, :], av.ap[:, kc, :], kc == 0, kc == 7,
                         r=[xtk] + av.keys, w=[("ps", b)])
                return b
            b = zgroup(Wqk, 512)
            B.copy(zqk[:], psum[b][:, :], r=[("ps", b)], w=["zqk"])
            b = zgroup(Wv, 512)
            B.copy(vbf[:], psum[b][:, :], r=[("ps", b)], w=["vbf"])
            b = zgroup(Wr, 512)
            B.act(sr[:], psum[b][:, :], AF.Silu, r=[("ps", b)], w=["sr"])
            b = zgroup(Wcq, 384)
            B.copy(cq[:], psum[b][:, 0:384], r=[("ps", b)], w=["cq"])
            b = zgroup(We, 288)
            B.copy(ze[:], psum[b][:, 0:288], r=[("ps", b)], w=["ze"])
            b = B.next_bank()
            for kc in range(8):
                B.mm(psum[b][0:16, 0:128], Wa.ap[:, kc, :], xT[i][:, kc, :], kc == 0, kc == 7,
                     r=[xtk] + Wa.keys, w=[("ps", b)])
            B.copy(aT[0:16, :], psum[b][0:16, 0:128], r=[("ps", b)], w=["aT"])
            b = B.next_bank()
            B.mm(psum[b][:, 0:256], aT[0:17, :], wgate[0:17, :], True, True, r=["aT", "wgate"], w=[("ps", b)])
            B.act(e1[:], psum[b][:, 0:256], AF.Exp, r=[("ps", b)], w=["e1"], scale=-1.0)
            B.act(l1[:], e1[:], AF.Ln, r=["e1"], w=["l1"], bias=1.0)
            b = B.next_bank()
            B.mm(psum[b][:, 0:256], c_us[:], l1[:], True, True, r=["c_us", "l1"], w=[("ps", b)])
            B.act(eb[:], psum[b][:, 0:256], AF.Exp, r=[("ps", b)], w=["eb"])
            B.act(enb[:], psum[b][:, 0:256], AF.Exp, r=[("ps", b)], w=["enb"], scale=-1.0)
            b = B.next_bank()
            B.mm(psum[b][:, 0:256], c_lw[:], l1[:], True, True, r=["c_lw", "l1"], w=[("ps", b)])
            B.act(ekb[:], psum[b][:, 0:256], AF.Exp, r=[("ps", b)], w=["ekb"])
            b = B.next_bank()
            for h in range(4):
                B.mm(psum[b][0:64, h:h + 1], l1[:, h * 64:(h + 1) * 64], c_us[:, 127:128], True, True,
                     r=["c_us", "l1"], w=[("ps", b)])
            B.act(eblT[:], psum[b][0:64, 0:4], AF.Exp, r=[("ps", b)], w=["eblT"])
            B.stt(qt[:], zqk[:, 0:256], 0.125, eb[:], ALU.mult, ALU.mult, r=["zqk", "eb"], w=["qt"])
            B.tt(kt[:], zqk[:, 256:512], enb[:], ALU.mult, r=["zqk", "enb"], w=["kt"])
            B.tt(kp[:], zqk[:, 256:512], ekb[:], ALU.mult, r=["zqk", "ekb"], w=["kp"])
            b = B.next_bank()
            pb = psum[b][:, :].bitcast(BF16)
            for h in range(4):
                B.tr(pb[0:64, h * 128:(h + 1) * 128], qt[:, h * 64:(h + 1) * 64], identb[:], r=["qt", "identb"], w=[("ps", b)])
                B.tr(pb[0:64, (4 + h) * 128:(5 + h) * 128], kt[:, h * 64:(h + 1) * 64], identb[:], r=["kt", "identb"], w=[("ps", b)])
            B.copy(qkT[:].rearrange("p a b -> p (a b)"), pb[0:64, :], r=[("ps", b)], w=["qkT"])
            b = B.next_bank()
            for h in range(4):
                B.mm(psum[b][:, h * 128:(h + 1) * 128], qkT[:, 4 + h, :], qkT[:, h, :], True, True, r=["qkT"], w=[("ps", b)])
            B.tt(ATm[:], psum[b][:, :].rearrange("p (h i) -> p h i", i=128), bc_h(c_mask[:, :], 4), ALU.mult,
                 r=[("ps", b), "c_mask"], w=["ATm"])
            bo = B.next_bank()
            for h in range(4):
                B.mm(psum[bo][:, h * 128:(h + 1) * 128], ATm[:, h, :], vbf[:, h * 128:(h + 1) * 128], True, False,
                     r=["ATm", "vbf"], w=[("ps", bo)])
                B.mm(psum[bo][:, h * 128:(h + 1) * 128], qkT[:, h, :], Sbf[:, h, :], False, True,
                     r=["qkT", "Sbf"], w=[("ps", bo)])
            B.copy(osb[:].rearrange("p a b -> p (a b)"), psum[bo][:, :], r=[("ps", bo)], w=["osb"], eng="act")
            b = B.next_bank()
            for h in range(4):
                B.mm(psum[b][0:64, h * 128:(h + 1) * 128], kp[:, h * 64:(h + 1) * 64], vbf[:, h * 128:(h + 1) * 128], True, True,
                     r=["kp", "vbf"], w=[("ps", b)])
            for h in range(4):
                B.stt(Sst[:, h, :], Sst[:, h, :], eblT[:, h:h + 1], psum[b][0:64, h * 128:(h + 1) * 128], ALU.mult, ALU.add,
                      r=["eblT", ("ps", b), "Sst"], w=["Sst"])
            B.copy(Sbf[:].rearrange("p a b -> p (a b)"), Sst[:].rearrange("p a b -> p (a b)"), r=["Sst"], w=["Sbf"], eng="dve")
            of = osb[:].rearrange("p a b -> p (a b)")
            B.tt(junk[:], of, of, ALU.mult, r=["osb"], w=["junk"])
            B.red(ss4[:], junk[:].rearrange("p (a b) -> p a b", b=128), ALU.add, r=["junk"], w=["ss4"])
            B.rsqrt(rs4[:], ss4[:], 128.0 * RMS_EPS, rsq[:, 0:4], r=["ss4"], w=["rs4"])
            B.tt(t1, osb[:], rs4[:, :].unsqueeze(2).to_broadcast([128, 4, 128]), ALU.mult, r=["osb", "rs4"], w=["t1"])
            B.tt(SG, sr[:].rearrange("p (a b) -> p a b", b=128), bc_h(ggla[:, :], 4), ALU.mult, r=["sr", "ggla"], w=["SG"])
            B.tt(gobf[:].rearrange("p (a b) -> p a b", b=128), t1, SG, ALU.mult, r=["t1", "SG"], w=["gobf"])
            B.dma(sc_go[tok0:tok0 + 128, :], gobf[:], r=["gobf"], w=[("sc_go", s, t)], q="pool")
            B.act(junk[:, 0:256], ze[:, 0:256], AF.Square, r=["ze"], w=["junk", "sskv"], accum_out=sskv[:, 0:1])
            B.rsqrt(rskv[:, 0:1], sskv[:, 0:1], 256.0 * RMS_EPS, rsq[:, 0:1], r=["sskv"], w=["rskv"])
            B.stt(ckvn[:], ze[:, 0:256], rskv[:, 0:1], gkv[:], ALU.mult, ALU.mult, r=["ze", "rskv", "gkv"], w=["ckvn"])
            B.dma(o_lat[tok0:tok0 + 128, :], ckvn[:], r=["ckvn"], w=[("o_lat", s, t)], q="pool")
            transpose_tile(ckvn, "ckvn", ckvT, "ckvT", 256)
            for hg in range(2):
                b = B.next_bank()
                for hh in range(4):
                    h = hg * 4 + hh
                    for kc in range(2):
                        B.mm(psum[b][0:64, hh * 128:(hh + 1) * 128], Wuk.ap[:, kc, h * 64:(h + 1) * 64], ckvT[:, kc, :],
                             kc == 0, kc == 1, r=["ckvT"] + Wuk.keys, w=[("ps", b)])
                B.copy(kT_seq[0:64, hg * 4:hg * 4 + 4, tsl], psum[b][0:64, :].rearrange("p (h t) -> p h t", t=128),
                       r=[("ps", b)], w=kT_keys)
            b = B.next_bank()
            for kc in range(2):
                B.mm(psum[b][:, :], ckvT[:, kc, :], Wuv.ap[:, kc, :], kc == 0, kc == 1, r=["ckvT"] + Wuv.keys, w=[("ps", b)])
            B.copy(V_seq[:, t, :, 0:64], psum[b][:, :].rearrange("p (h d) -> p h d", d=64), r=[("ps", b)], w=V_keys)
            rope(ze[:, 256:272].unsqueeze(1), ze[:, 272:288].unsqueeze(1), kpe[:, 64:80].unsqueeze(1), kpe[:, 80:96].unsqueeze(1),
                 cosT[:, t, :], sinT[:, t, :], 1, ["ze"], ["kpe"])
            B.dma(o_kr[tok0:tok0 + 128, :], kpe[:, 64:96], r=["kpe"], w=[("o_kr", s, t)], q="pool")
            b = B.next_bank()
            B.tr(psum[b][0:96, 0:128], kpe[:, :], ident[:], r=["kpe", "ident"], w=[("ps", b)])
            B.copy(kT_seq[64:96, :, tsl], psum[b][64:96, 0:128].unsqueeze(1).to_broadcast([32, 8, 128]),
                   r=[("ps", b)], w=kT_keys, eng="dve")
            B.act(junk[:, 0:384], cq[:], AF.Square, r=["cq"], w=["junk", "sskv"], accum_out=sskv[:, 1:2])
            B.rsqrt(rskv[:, 1:2], sskv[:, 1:2], 384.0 * RMS_EPS, rsq[:, 0:1], r=["sskv"], w=["rskv"])
            B.stt(cqn[:], cq[:], rskv[:, 1:2], gq[:], ALU.mult, ALU.mult, r=["cq", "rskv", "gq"], w=["cqn"])
            transpose_tile(cqn, "cqn", cqnT, "cqnT", 384)
            for (c0, n, h0, nh) in ((0, 480, 0, 5), (480, 288, 5, 3)):
                b = B.next_bank()
                for kc in range(3):
                    B.mm(psum[b][:, 0:n], cqnT[:, kc, :], Wuq.ap[:, kc, c0:c0 + n], kc == 0, kc == 2,
                         r=["cqnT"] + Wuq.keys, w=[("ps", b)])
                P.add("act", lambda e, b=b, n=n, h0=h0, nh=nh: e.mul(
                    qtm[:, h0:h0 + nh, :], psum[b][:, 0:n].rearrange("p (h d) -> p h d", d=96), float(MLA_SCALE)),
                    r=[("ps", b)], w=["qtm"])
            rope(qtm[:, :, 64:80], qtm[:, :, 80:96], qtm[:, :, 64:80], qtm[:, :, 80:96],
                 cosT[:, t, :], sinT[:, t, :], 8, ["qtm"], ["qtm"])
            B.copy(qbf[:].rearrange("p (h d) -> p h d", d=96), qtm[:], r=["qtm"], w=["qbf"], eng="dve")
            B.dma(sc_q[tok0:tok0 + 128, :], qbf[:], r=["qbf"], w=[("sc_q", s, t)], q="pool")
        B.dma(o_sgla[s].rearrange("h d v -> d h v"), Sst[:], r=["Sst"], w=[("o_sgla", s)])


    oxb = zqk[:, :].bitcast(BF16).rearrange("p (h d) -> p h d", d=256)
    oxT = sr[:, :].bitcast(BF16).rearrange("p (c t) -> p c t", t=128)
    qT = osb[:].rearrange("p a b -> p (a b)").bitcast(BF16)[0:96, :].rearrange("p (h t) -> p h t", t=128)
    _jb = junk[:, :].bitcast(BF16)
    _qb = qtm[:].rearrange("p a b -> p (a b)").bitcast(BF16)
    PT = [_jb[:, 0:512].rearrange("p (a b) -> p a b", b=128), _jb[:, 512:1024].rearrange("p (a b) -> p a b", b=128),
          _qb[:, 0:512].rearrange("p (a b) -> p a b", b=128), _qb[:, 512:1024].rearrange("p (a b) -> p a b", b=128)]
    PTk = ["junk", "junk", "qtm", "qtm"]
    Wout = AV(arena, 0, [8, 1024])
    Wxq = AV(arena, 8192, [8, 1024])
    Wxo = AV(arena, 16384, [8, 1024])
    catT, h1T = xT[0], xT[1]
    catk, h1Tk = ("xT", 0), ("xT", 1)
    qxT = memT[:, :, 0:128]
    PX = memT[:, :, 128:256]
    mlao = vbf

    class _Tile:
        def __init__(self, v, ti, ntok):
            self.v, self.ti, self.n = v, ti, ntok

        def __getitem__(self, key):
            if isinstance(key, tuple):
                return self.v[:self.n, self.ti, key[1]]
            return self.v[:self.n, self.ti, :]

    class _Tile2:
        def __init__(self, v, ntok):
            self.v, self.n = v, ntok

        def __getitem__(self, key):
            if isinstance(key, tuple):
                return self.v[:self.n, key[1]]
            return self.v[:self.n, :]

    def layer_norm(pre, prek, gi, out, outk, ntok=128):
        n = ntok
        for hf in range(2):
            P.add("dve", lambda e, hf=hf: e.bn_stats(lnst[:n, hf, :], pre[:, hf * 512:(hf + 1) * 512]), r=[prek], w=["lnst"])
        P.add("dve", lambda e: e.bn_aggr(lnmv[:n, :], lnst[:n, :, :]), r=["lnst"], w=["lnmv"])
        B.rsqrt(lnrs[:n, 0:1], lnmv[:n, 1:2], LN_EPS, rsq[:n, 4:5], r=["lnmv"], w=["lnrs"], tk="rsq_ln")
        B.stt(lnrs[:n, 1:2], lnmv[:n, 0:1], -1.0, lnrs[:n, 0:1], ALU.mult, ALU.mult, r=["lnmv", "lnrs"], w=["lnrs"])
        B.act(big1[:n, :], pre[:], AF.Identity, r=[prek, "lnrs"], w=["t1", "SG"], scale=lnrs[:n, 0:1], bias=lnrs[:n, 1:2])
        B.tt(big1[:n, :], big1[:n, :], lnp[gi][:n, :], ALU.mult, r=["t1", "SG", ("lnp", gi)], w=["t1", "SG"])
        B.tt(out[:], big1[:n, :], lnp[gi + 1][:n, :], ALU.add, r=["t1", "SG", ("lnp", gi + 1)], w=[outk])

    def load_B_weights():
        load_w(Wout.ap, w_out, Wout.keys)
        load_w(Wxq.ap, w_xq, Wxq.keys)
        load_w(Wxo.ap, w_xo, Wxo.keys)

    catT_alt = gtmp[:, 0:2, :].rearrange("p a b -> p (a b)").bitcast(BF16).rearrange("p (c t) -> p c t", t=128)

    def phaseB(s):
        load_B_weights()
        B.bank_lim = 6
        B.psn = 0

        def front(t):
            tok0 = s * SEQ + t * 128
            i = cnt["x"] % 2
            cnt["x"] += 1
            xk = ("xin", i)
            catT, catk = (xT[0], [("xT", 0)]) if t % 2 == 0 else (catT_alt, ["e1", "l1"])
            B.dma(xin[i][:], xp[tok0:tok0 + 128, :], w=[xk])
            B.dma(qbf[:], sc_q[tok0:tok0 + 128, :], r=[("sc_q", s, t)], w=["qbf"])
            B.dma(gobf[:], sc_go[tok0:tok0 + 128, :], r=[("sc_go", s, t)], w=["gobf"])
            b = B.next_bank()
            pb = psum[b][:, :].bitcast(BF16)
            for h in range(8):
                B.tr(pb[0:96, h * 128:(h + 1) * 128], qbf[:, h * 96:(h + 1) * 96], identb[:], r=["qbf", "identb"], w=[("ps", b)])
            B.copy(qT, pb[0:96, :].rearrange("p (h t) -> p h t", t=128), r=[("ps", b)], w=["osb"])
            b = B.next_bank()
            pb = psum[b][:, :].bitcast(BF16)
            for c in range(4):
                B.tr(pb[:, c * 128:(c + 1) * 128], gobf[:, c * 128:(c + 1) * 128], identb[:], r=["gobf", "identb"], w=[("ps", b)])
            B.copy(catT[:, 0:4, :], pb[:, 0:512].rearrange("p (c t) -> p c t", t=128), r=[("ps", b)], w=catk)
            nk = t + 1

            def att_front(h, g0, g1):
                b = B.next_bank()
                for kt_ in range(g0, g1):
                    j = kt_ - g0
                    B.mm(psum[b][:, j * 128:(j + 1) * 128], kT_seq[0:96, h, kt_ * 128:(kt_ + 1) * 128], qT[:, h, :],
                         True, True, r=["osb"] + kT_keys, w=[("ps", b)])
                pi = cnt["st"] % 4
                cnt["st"] += 1
                n = g1 - g0
                B.act(PT[pi][:, 0:n, :], psum[b][:, 0:n * 128].rearrange("p (a b) -> p a b", b=128), AF.Exp,
                      r=[("ps", b)], w=[PTk[pi]])
                if g1 == nk:
                    B.tt(PT[pi][:, n - 1, :], PT[pi][:, n - 1, :], maskb[:], ALU.mult, r=[PTk[pi], "maskb"], w=[PTk[pi]])
                return pi

            def att_back(h, g0, g1, pi):
                ob = 6 + h // 4
                oc = (h % 4) * 65
                for kt_ in range(g0, g1):
                    j = kt_ - g0
                    B.mm(psum[ob][:, oc:oc + 65], PT[pi][:, j, :], V_seq[:, kt_, h, 0:65], kt_ == 0, kt_ == nk - 1,
                         r=[PTk[pi]] + V_keys, w=[("ps", ob)])

            pend = None
            for h in range(8):
                for g0 in range(0, nk, 4):
                    g1 = min(nk, g0 + 4)
                    pi = att_front(h, g0, g1)
                    if pend is not None:
                        att_back(*pend)
                    pend = (h, g0, g1, pi)
            att_back(*pend)
            for hg in range(2):
                ov = psum[6 + hg][:, 0:260].rearrange("p (h d) -> p h d", d=65)
                P.add("dve", lambda e, ov=ov, hg=hg: e.reciprocal(rec8[:, hg * 4:hg * 4 + 4], ov[:, :, 64]),
                      r=[("ps", 6 + hg)], w=[("rec8", hg)])
                B.tt(mlao[:, hg * 256:(hg + 1) * 256].rearrange("p (h d) -> p h d", d=64), ov[:, :, 0:64],
                     rec8[:, hg * 4:hg * 4 + 4].unsqueeze(2).to_broadcast([128, 4, 64]), ALU.mult,
                     r=[("ps", 6 + hg), ("rec8", hg)], w=["vbf"])
            b = B.next_bank()
            pb = psum[b][:, :].bitcast(BF16)
            for c in range(4):
                B.tr(pb[:, c * 128:(c + 1) * 128], mlao[:, c * 128:(c + 1) * 128], identb[:], r=["vbf", "identb"], w=[("ps", b)])
            B.copy(catT[:, 4:8, :], pb[:, 0:512].rearrange("p (c t) -> p c t", t=128), r=[("ps", b)], w=catk)
            return (t, tok0, i, xk, catT, catk)

        def tail(t, tok0, i, xk, catT, catk):
            pre, prek = stage[0], ("stage", 0)
            h1, h1k = stage[1], ("stage", 1)
            for hf in range(2):
                b = B.next_bank()
                for kc in range(8):
                    B.mm(psum[b][:, :], catT[:, kc, :], Wout.ap[:, kc, hf * 512:(hf + 1) * 512], kc == 0, kc == 7,
                         r=catk + Wout.keys, w=[("ps", b)])
                B.stt(pre[:, hf * 512:(hf + 1) * 512], xin[i][:, hf * 512:(hf + 1) * 512], float(ALPHA), psum[b][:, :],
                      ALU.mult, ALU.add, r=[xk, ("ps", b)], w=[prek])
            layer_norm(pre, prek, 0, h1, h1k)
            transpose_tile(h1, h1k, h1T, h1Tk, D)
            for jg in range(2):
                b = B.next_bank()
                for jj in range(4):
                    j = jg * 4 + jj
                    for kc in range(8):
                        B.mm(psum[b][:, jj * 128:(jj + 1) * 128], Wxq.ap[:, kc, j * 128:(j + 1) * 128], h1T[:, kc, :],
                             kc == 0, kc == 7, r=[h1Tk] + Wxq.keys, w=[("ps", b)])
                P.add("act", lambda e, b=b, jg=jg: e.mul(qxT[:, jg * 4:jg * 4 + 4, :],
                      psum[b][:, :].rearrange("p (a t) -> p a t", t=128), 1.0 / 16.0), r=[("ps", b)], w=["qxT"])
            for hg in range(2):
                b = B.next_bank()
                for hh in range(2):
                    h = hg * 2 + hh
                    for mt in range(2):
                        col = (hh * 2 + mt) * 128
                        for c in range(2):
                            B.mm(psum[b][:, col:col + 128], memkT[:, s, h * 2 + c, mt * 128:(mt + 1) * 128], qxT[:, h * 2 + c, :],
                                 c == 0, c == 1, r=["memkT", "qxT"], w=[("ps", b)])
                B.act(PX[:, hg * 4:hg * 4 + 4, :], psum[b][:, :].rearrange("p (a t) -> p a t", t=128), AF.Exp,
                      r=[("ps", b)], w=["PX"])
            for h in range(4):
                b = B.next_bank()
                for mt in range(2):
                    B.mm(psum[b][:, 0:257], PX[:, h * 2 + mt, :], memv[:, s, mt, h, 0:257], mt == 0, mt == 1,
                         r=["PX", "memv"], w=[("ps", b)])
                P.add("dve", lambda e, b=b, h=h: e.reciprocal(rec8[:, h:h + 1], psum[b][:, 256:257]), r=[("ps", b)], w=[("rec8", "x", h)])
                B.ts(oxb[:, h, :], psum[b][:, 0:256], rec8[:, h:h + 1], None, ALU.mult, r=[("ps", b), ("rec8", "x", h)], w=["zqk"])
            for cg in range(2):
                b = B.next_bank()
                pb = psum[b][:, :].bitcast(BF16)
                oxf = zqk[:, :].bitcast(BF16)
                for cc in range(4):
                    c = cg * 4 + cc
                    B.tr(pb[:, cc * 128:(cc + 1) * 128], oxf[:, c * 128:(c + 1) * 128], identb[:], r=["zqk", "identb"], w=[("ps", b)])
                B.copy(oxT[:, cg * 4:cg * 4 + 4, :], pb[:, 0:512].rearrange("p (c t) -> p c t", t=128), r=[("ps", b)], w=["sr"])
            for hf in range(2):
                b = B.next_bank()
                for kc in range(8):
                    B.mm(psum[b][:, :], oxT[:, kc, :], Wxo.ap[:, kc, hf * 512:(hf + 1) * 512], kc == 0, kc == 7,
                         r=["sr"] + Wxo.keys, w=[("ps", b)])
                B.stt(pre[:, hf * 512:(hf + 1) * 512], h1[:, hf * 512:(hf + 1) * 512], float(ALPHA), psum[b][:, :],
                      ALU.mult, ALU.add, r=[h1k, ("ps", b)], w=[prek])
            layer_norm(pre, prek, 2, xin[i], xk)
            B.dma(sc_h2[tok0:tok0 + 128, :], xin[i][:], r=[xk], w=[("sc_h2", tok0 // 128)], q="pool")

        pend = None
        for t in range(16):
            st = front(t)
            if pend is not None:
                tail(*pend)
            pend = st
        tail(*pend)
        B.bank_lim = 8


    SK = ["s_pg%d" % k_ for k_ in range(9)] + ["s_cT%d" % k_ for k_ in range(9)] + ["s_cb%d" % k_ for k_ in range(9)] + [
          "s_PS0", "s_PS1", "s_v32", "s_vm0", "s_vm1", "s_colT", "s_QM", "s_S0", "s_S1", "s_catS", "s_QLT", "s_qTs", "s_WukT",
          "s_ol", "s_olT", "s_om", "s_mk0", "s_mk1", "s_e16", "s_qx", "s_pxs", "s_om4", "s_den"]

    def sv(off, n, dt=F32, parts=128):
        a = arenaS[0:parts, off:off + n]
        return a.bitcast(dt) if dt != F32 else a
    NSL = 9
    s_pg = [sv(1040 + k_ * 352, 352) for k_ in range(8)] + [sv(0, 352)]
    s_cT = [sv(3856 + k_ * 192, 192, BF16).rearrange("p (c t) -> p c t", t=128) for k_ in range(8)] + [
        sv(352, 192, BF16).rearrange("p (c t) -> p c t", t=128)]
    s_cb = [sv(5392 + k_ * 130, 130, BF16) for k_ in range(8)] + [sv(544, 130, BF16)]
    s_PS = [sv(6432 + k_ * 16, 16, BF16) for k_ in range(2)]
    s_v32 = sv(6464, 512, parts=16)
    s_vm = [sv(6976 + k_ * 512, 512, parts=16) for k_ in range(2)]
    s_colT = sv(8000, 192, parts=64).rearrange("p (a b) -> p a b", b=16)
    s_QM = sv(8192, 1024, parts=64).rearrange("p (h b m) -> p h b m", b=16, m=16)
    s_S = [sv(9216 + k_ * 512, 512, parts=64).rearrange("p (h v) -> p h v", v=128) for k_ in range(2)]
    s_catS = sv(10240, 1024, parts=16)
    s_QLT = sv(11264, 128, BF16).rearrange("p (c h b) -> p c h b", h=8, b=16)
    s_qTs = sv(11392, 64, BF16, parts=96).rearrange("p (h b) -> p h b", b=16)
    s_WukT = sv(11456, 1024, BF16, parts=64).rearrange("p (h r) -> p h r", r=256)
    s_ol = sv(12480, 256, parts=8)
    s_olT = sv(12736, 8, BF16).rearrange("p (c h) -> p c h", h=8)
    s_om = sv(6976, 512, parts=8)
    s_mk = [sv(1040 + k_ * 1024, 1024) for k_ in range(2)]
    s_e16 = sv(3088, 128, parts=16)
    s_qx = sv(3216, 1024, parts=16)
    s_pxs = sv(4240, 4, BF16)
    s_om4 = sv(4244, 1024, parts=4)
    s_den = sv(5268, 8, parts=4)

    def rms_rows(src, n, sscol, eps_n, gain, out, srck, outk, ntok):
        B.act(junk[:ntok, 0:n], src, AF.Square, r=[srck], w=["junk", "sskv"], accum_out=sskv[:ntok, sscol:sscol + 1])
        B.rsqrt(rskv[:ntok, sscol:sscol + 1], sskv[:ntok, sscol:sscol + 1], eps_n, rsq[:ntok, 0:1], r=["sskv"], w=["rskv"])
        B.stt(out, src, rskv[:ntok, sscol:sscol + 1], gain, ALU.mult, ALU.mult, r=[srck, "rskv"], w=[outk])

    def sampleA():
        n = NS
        barrier(kT_keys + V_keys + SK)
        load_A_weights()
        B.bank_lim = 6
        B.psn = 0
        i = cnt["x"] % 2
        cnt["x"] += 1
        xk, xtk = ("xin", i), ("xT", i)
        B.dma(xin[i][:n, :], xs, w=[xk])
        transpose_tile(xin[i], xk, xT[i], xtk, D, ntok=n)
        for k_ in range(NSL):
            P.add("dve", lambda e, k_=k_: e.memset(s_pg[k_], 0.0), w=["s_pg%d" % k_])
            P.add("dve", lambda e, k_=k_: e.memset(s_cb[k_], 1.0), w=["s_cb%d" % k_])

        def zgroup(av, nn):
            b = B.next_bank()
            for kc in range(8):
                B.mm(psum[b][:n, 0:nn], xT[i][:, kc, 0:n], av.ap[:, kc, :], kc == 0, kc == 7, r=[xtk] + av.keys, w=[("ps", b)])
            return b
        b = zgroup(Wqk, 512)
        B.copy(zqk[:n, :], psum[b][:n, :], r=[("ps", b)], w=["zqk"])
        b = zgroup(Wv, 512)
        B.copy(s_v32, psum[b][:n, :], r=[("ps", b)], w=["s_v32"])
        b = zgroup(Wr, 512)
        B.act(sr[:n, :], psum[b][:n, :], AF.Silu, r=[("ps", b)], w=["sr"])
        b = zgroup(Wcq, 384)
        B.copy(cq[:n, :], psum[b][:n, 0:384], r=[("ps", b)], w=["cq"])
        b = zgroup(We, 288)
        B.copy(ze[:n, :], psum[b][:n, 0:288], r=[("ps", b)], w=["ze"])
        b = B.next_bank()
        for kc in range(8):
            B.mm(psum[b][0:16, 0:n], Wa.ap[:, kc, :], xT[i][:, kc, 0:n], kc == 0, kc == 7, r=[xtk] + Wa.keys, w=[("ps", b)])
        B.copy(aT[0:16, 0:n], psum[b][0:16, 0:n], r=[("ps", b)], w=["aT"])
        b = B.next_bank()
        B.mm(psum[b][:n, 0:256], aT[0:17, 0:n], wgate[0:17, :], True, True, r=["aT", "wgate"], w=[("ps", b)])
        B.act(e1[:n, :], psum[b][:n, 0:256], AF.Exp, r=[("ps", b)], w=["e1"], scale=-1.0)
        B.act(l1[:n, :], e1[:n, :], AF.Ln, r=["e1"], w=["l1"], bias=1.0)
        B.act(eb[:n, :], l1[:n, :], AF.Exp, r=["l1"], w=["eb"], scale=-1.0 / 16.0)
        b = B.next_bank()
        for xi, (src, c0, key) in enumerate(((eb, 0, "eb"), (zqk, 256, "zqk"), (zqk, 0, "zqk"))):
            for h in range(4):
                B.tr(psum[b][0:64, (xi * 4 + h) * 16:(xi * 4 + h + 1) * 16], src[:n, c0 + h * 64:c0 + (h + 1) * 64], ident[:n, :n],
                     r=[key, "ident"], w=[("ps", b)])
        B.copy(s_colT[:, 0:8, :], psum[b][0:64, 0:128].rearrange("p (a b) -> p a b", b=16), r=[("ps", b)], w=["s_colT"], eng="dve")
        P.add("act", lambda e, b=b: e.mul(s_colT[:, 8:12, :], psum[b][0:64, 128:192].rearrange("p (a b) -> p a b", b=16), 0.125),
              r=[("ps", b)], w=["s_colT"])
        B.tt(s_QM, s_colT[:, 8:12, :].unsqueeze(3).to_broadcast([64, 4, 16, 16]),
             i16[:, :, :].unsqueeze(1).to_broadcast([64, 4, 16, 16]), ALU.mult, r=["s_colT", "i16"], w=["s_QM"])
        B.bank_lim = 4
        B.psn = 0
        for bb in range(n):
            sl = bb % 2
            Sk = "s_S%d" % sl
            B.dma(s_S[sl], sgla_s[bb].rearrange("h d v -> d h v"), w=[Sk])
            B.ts(s_vm[sl], s_v32, ident[:n, bb:bb + 1], None, ALU.mult, r=["s_v32", "ident"], w=["s_vm%d" % sl])
            b = B.next_bank()
            for h in range(4):
                B.mm(psum[b][0:64, h * 128:(h + 1) * 128], zqk[:n, 256 + h * 64:256 + (h + 1) * 64], s_vm[sl][:, h * 128:(h + 1) * 128],
                     True, True, r=["zqk", "s_vm%d" % sl], w=[("ps", b)])
            for h in range(4):
                B.stt(s_S[sl][:, h, :], s_S[sl][:, h, :], s_colT[:, h, bb:bb + 1], psum[b][0:64, h * 128:(h + 1) * 128], ALU.mult, ALU.add,
                      r=[Sk, "s_colT", ("ps", b)], w=[Sk])
            B.dma(o_sgla_s[bb].rearrange("h d v -> d h v"), s_S[sl], r=[Sk], w=[("o_sgla_s", bb)])
            for h in range(4):
                B.mm(psum[4 + h][:n, 0:128], s_QM[:, h, bb, :], s_S[sl][:, h, :], bb == 0, bb == n - 1,
                     r=["s_QM", Sk], w=[("ps", 4 + h)])
        for h in range(4):
            B.copy(osb[:n, h, :], psum[4 + h][:n, 0:128], r=[("ps", 4 + h)], w=["osb"], eng="act")
        B.bank_lim = 6
        B.psn = 0
        of = osb[:n].rearrange("p a b -> p (a b)")
        B.tt(junk[:n, :], of, of, ALU.mult, r=["osb"], w=["junk"])
        B.red(ss4[:n, :], junk[:n, :].rearrange("p (a b) -> p a b", b=128), ALU.add, r=["junk"], w=["ss4"])
        B.rsqrt(rs4[:n, :], ss4[:n, :], 128.0 * RMS_EPS, rsq[:n, 0:4], r=["ss4"], w=["rs4"])
        B.tt(t1[:n], osb[:n], rs4[:n, :].unsqueeze(2).to_broadcast([n, 4, 128]), ALU.mult, r=["osb", "rs4"], w=["t1"])
        B.tt(SG[:n], sr[:n, :].rearrange("p (a b) -> p a b", b=128), bc_h(ggla[:n, :], 4), ALU.mult, r=["sr", "ggla"], w=["SG"])
        B.tt(s_catS[:, 0:512].rearrange("p (a b) -> p a b", b=128), t1[:n], SG[:n], ALU.mult, r=["t1", "SG"], w=["s_catS"])
        rms_rows(ze[:n, 0:256], 256, 0, 256.0 * RMS_EPS, gkv[:n, :], ckvn[:n, :], "ze", "ckvn", n)
        B.dma(o_lat_s, ckvn[:n, :], r=["ckvn"], w=["o_lat_s"])
        rope(ze[:n, 256:272].unsqueeze(1), ze[:n, 272:288].unsqueeze(1), kpe[:n, 64:80].unsqueeze(1), kpe[:n, 80:96].unsqueeze(1),
             cos_s[:, :], sin_s[:, :], 1, ["ze"], ["kpe"], ntok=n, ck="cos_s", sk_="sin_s")
        B.dma(o_kr_s, kpe[:n, 64:96], r=["kpe"], w=["o_kr_s"])
        rms_rows(cq[:n, :], 384, 1, 384.0 * RMS_EPS, gq[:n, :], cqn[:n, :], "cq", "cqn", n)
        transpose_tile(cqn, "cqn", cqnT, "cqnT", 384, ntok=n)
        for (c0, nn, h0, nh) in ((0, 480, 0, 5), (480, 288, 5, 3)):
            b = B.next_bank()
            for kc in range(3):
                B.mm(psum[b][:n, 0:nn], cqnT[:, kc, 0:n], Wuq.ap[:, kc, c0:c0 + nn], kc == 0, kc == 2, r=["cqnT"] + Wuq.keys, w=[("ps", b)])
            P.add("act", lambda e, b=b, nn=nn, h0=h0, nh=nh: e.mul(
                qtm[:n, h0:h0 + nh, :], psum[b][:n, 0:nn].rearrange("p (h d) -> p h d", d=96), float(MLA_SCALE)),
                r=[("ps", b)], w=["qtm"])
        rope(qtm[:n, :, 64:80], qtm[:n, :, 80:96], qtm[:n, :, 64:80], qtm[:n, :, 80:96], cos_s[:, :], sin_s[:, :], 8, ["qtm"], ["qtm"],
             ntok=n, ck="cos_s", sk_="sin_s")
        b = B.next_bank()
        for h in range(8):
            B.tr(psum[b][0:96, h * 16:(h + 1) * 16], qtm[:n, h, :], ident[:n, :n], r=["qtm", "ident"], w=[("ps", b)])
        B.copy(s_qTs, psum[b][0:96, 0:128].rearrange("p (h b) -> p h b", b=16), r=[("ps", b)], w=["s_qTs"])
        for kc in range(2):
            b = B.next_bank()
            pb = psum[b][:, :].bitcast(BF16)
            for h in range(8):
                B.tr(pb[0:64, h * 128:(h + 1) * 128], Wuk.ap[:, kc, h * 64:(h + 1) * 64], identb[:], r=Wuk.keys + ["identb"], w=[("ps", b)])
            B.copy(s_WukT[:, :, kc * 128:(kc + 1) * 128], pb[0:64, :].rearrange("p (h r) -> p h r", r=128), r=[("ps", b)], w=["s_WukT"])
        b = B.next_bank()
        for rc in range(2):
            for h in range(8):
                B.mm(psum[b][:, (rc * 8 + h) * 16:(rc * 8 + h + 1) * 16], s_WukT[:, h, rc * 128:(rc + 1) * 128], s_qTs[0:64, h, :],
                     True, True, r=["s_WukT", "s_qTs"], w=[("ps", b)])
        B.copy(s_QLT, psum[b][:, 0:256].rearrange("p (c h b) -> p c h b", h=8, b=16), r=[("ps", b)], w=["s_QLT"])
        B.bank_lim = 5
        B.psn = 0
        gcnt = [0]
        pgv = arenaS[:, 1040:1040 + 8 * 352].rearrange("p (s c) -> p s c", c=352)
        npg_tot = NPAGE + 1

        def pg_front(bb, grp):
            bs = B.next_bank()
            if grp[0][0] == "new":
                B.dma(s_pg[8][0:1, 0:256], o_lat_s[bb:bb + 1, :], r=["o_lat_s"], w=["s_pg8"])
                B.dma(s_pg[8][0:1, 320:352], o_kr_s[bb:bb + 1, :], r=["o_kr_s"], w=["s_pg8"])
            else:
                j0, s0 = grp[0][1], grp[0][2]
                row0 = (bb * NPAGE + j0) * 128
                wk4 = ["s_pg%d" % (s0 + k_) for k_ in range(4)]
                B.dma(pgv[:, s0:s0 + 4, 0:256], sc_lat[row0:row0 + 512, :].rearrange("(j p) r -> p j r", p=128),
                      r=[("sc_lat", bb)], w=wk4)
                B.dma(pgv[:, s0:s0 + 4, 320:352], sc_kr[row0:row0 + 512, :].rearrange("(j p) r -> p j r", p=128),
                      r=[("sc_kr", bb)], w=wk4)
            for jj, (kind, j, sl) in enumerate(grp):
                pgk, cTk, cbk = "s_pg%d" % sl, "s_cT%d" % sl, "s_cb%d" % sl
                bt = B.next_bank()
                B.tr(psum[bt][:, 0:128], s_pg[sl][:, 0:128], ident[:], r=[pgk, "ident"], w=[("ps", bt)])
                B.tr(psum[bt][:, 128:256], s_pg[sl][:, 128:256], ident[:], r=[pgk, "ident"], w=[("ps", bt)])
                B.tr(psum[bt][0:96, 256:384], s_pg[sl][:, 256:352], ident[:], r=[pgk, "ident"], w=[("ps", bt)])
                B.copy(s_cT[sl], psum[bt][:, 0:384].rearrange("p (c t) -> p c t", t=128), r=[("ps", bt)], w=[cTk])
                B.copy(s_cb[sl][:, 0:256], s_pg[sl][:, 0:256], r=[pgk], w=[cbk])
                B.mm(psum[bs][:, jj * 8:(jj + 1) * 8], s_cT[sl][:, 0, :], s_QLT[:, 0, :, bb], True, False, r=[cTk, "s_QLT"], w=[("ps", bs)])
                B.mm(psum[bs][:, jj * 8:(jj + 1) * 8], s_cT[sl][:, 1, :], s_QLT[:, 1, :, bb], False, False, r=[cTk, "s_QLT"], w=[("ps", bs)])
                B.mm(psum[bs][:, jj * 8:(jj + 1) * 8], s_cT[sl][64:96, 2, :], s_qTs[64:96, :, bb], False, True, r=[cTk, "s_qTs"], w=[("ps", bs)])
            ng = len(grp)
            pi = gcnt[0] % 2
            gcnt[0] += 1
            PSk = "s_PS%d" % pi
            B.act(s_PS[pi][:, 0:ng * 8], psum[bs][:, 0:ng * 8], AF.Exp, r=[("ps", bs)], w=[PSk])
            if grp[0][0] == "new":
                B.tt(s_PS[pi][:, 0:8], s_PS[pi][:, 0:8], maskb[:, 0:1].to_broadcast([128, 8]), ALU.mult, r=[PSk, "maskb"], w=[PSk])
            return pi

        def pg_back(bb, grp, pi, first, last):
            PSk = "s_PS%d" % pi
            for jj, (kind, j, sl) in enumerate(grp):
                B.mm(psum[6][0:8, 0:257], s_PS[pi][:, jj * 8:(jj + 1) * 8], s_cb[sl][:, 0:257],
                     first and jj == 0, last and jj == len(grp) - 1, r=[PSk, "s_cb%d" % sl], w=[("ps", 6)])
            if last:
                sample_post(bb)

        def sample_post(bb):
            P.add("dve", lambda e: e.reciprocal(rec8[0:8, 0:1], psum[6][0:8, 256:257]), r=[("ps", 6)], w=[("rec8", 0)])
            B.ts(s_ol, psum[6][0:8, 0:256], rec8[0:8, 0:1], None, ALU.mult, r=[("ps", 6), ("rec8", 0)], w=["s_ol"])
            b = B.next_bank()
            for c in range(2):
                B.tr(psum[b][:, c * 8:(c + 1) * 8], s_ol[:, c * 128:(c + 1) * 128], ident[:8, :8], r=["s_ol", "ident"], w=[("ps", b)])
            B.copy(s_olT, psum[b][:, 0:16].rearrange("p (c h) -> p c h", h=8), r=[("ps", b)], w=["s_olT"], eng="dve")
            b = B.next_bank()
            for kc in range(2):
                B.mm(psum[b][0:8, :], s_olT[:, kc, :], Wuv.ap[:, kc, :], kc == 0, kc == 1, r=["s_olT"] + Wuv.keys, w=[("ps", b)])
            B.tt(s_om, psum[b][0:8, :], bm8[:, :], ALU.mult, r=[("ps", b), "bm8"], w=["s_vm0"])
            B.mm(psum[7][:n, :], i16[0:8, bb, :], s_om, bb == 0, bb == n - 1, r=["i16", "s_vm0"], w=[("ps", 7)])

        all_groups = []
        for bb in range(n):
            all_groups.append((bb, [("new", 0, 8)], True, False))
            for gi in range(NPAGE // 4):
                s0 = (gi % 2) * 4
                all_groups.append((bb, [("page", gi * 4 + k_, s0 + k_) for k_ in range(4)], False, gi == NPAGE // 4 - 1))
        pend = None
        for (bb, grp, first, last) in all_groups:
            pi = pg_front(bb, grp)
            if pend is not None:
                pg_back(*pend)
            pend = (bb, grp, pi, first, last)
        pg_back(*pend)
        B.copy(s_catS[:, 512:1024], psum[7][:n, :], r=[("ps", 7)], w=["s_catS"], eng="dve")
        B.bank_lim = 8

    def sampleB():
        n = NS
        barrier(SK)
        load_B_weights()
        B.dma(bm4, bm4_d, w=["memkT"])
        B.bank_lim = 6
        B.psn = 0
        i = cnt["x"] % 2
        cnt["x"] += 1
        xk = ("xin", i)
        B.dma(xin[i][:n, :], xs, w=[xk])
        transpose_tile(s_catS, "s_catS", catT, catk, D, ntok=n)
        pre, prek = stage[0], ("stage", 0)
        h1, h1k = stage[1], ("stage", 1)
        for hf in range(2):
            b = B.next_bank()
            for kc in range(8):
                B.mm(psum[b][:n, :], catT[:, kc, 0:n], Wout.ap[:, kc, hf * 512:(hf + 1) * 512], kc == 0, kc == 7,
                     r=[catk] + Wout.keys, w=[("ps", b)])
            B.stt(pre[:n, hf * 512:(hf + 1) * 512], xin[i][:n, hf * 512:(hf + 1) * 512], float(ALPHA), psum[b][:n, :],
                  ALU.mult, ALU.add, r=[xk, ("ps", b)], w=[prek])
        layer_norm(_Tile2(pre, n), prek, 0, _Tile2(h1, n), h1k, ntok=n)
        transpose_tile(h1, h1k, h1T, h1Tk, D, ntok=n)
        for hf in range(2):
            b = B.next_bank()
            for kc in range(8):
                B.mm(psum[b][:n, :], h1T[:, kc, 0:n], Wxq.ap[:, kc, hf * 512:(hf + 1) * 512], kc == 0, kc == 7,
                     r=[h1Tk] + Wxq.keys, w=[("ps", b)])
            P.add("act", lambda e, b=b, hf=hf: e.mul(s_qx[:, hf * 512:(hf + 1) * 512], psum[b][:n, :], 1.0 / 16.0), r=[("ps", b)], w=["s_qx"])
        for bb in range(n):
            B.ts(s_e16, c_mask[0:n, :], 0.0, ident[:n, bb:bb + 1], ALU.mult, ALU.add, r=["c_mask", "ident"], w=["s_e16"])
            qb = []
            for hf in range(2):
                b = B.next_bank()
                B.mm(psum[b][:, :], s_e16, s_qx[:, hf * 512:(hf + 1) * 512], True, True, r=["s_e16", "s_qx"], w=[("ps", b)])
                qb.append(b)
            for mt in range(2):
                mk_, mkk = s_mk[0], "s_mk0"
                B.dma(mk_, cmk[bb, mt * 128:(mt + 1) * 128, :], w=[mkk])
                for hf in range(2):
                    B.tt(mk_[:, hf * 512:(hf + 1) * 512], mk_[:, hf * 512:(hf + 1) * 512], psum[qb[hf]][:, :], ALU.mult,
                         r=[mkk, ("ps", qb[hf])], w=[mkk])
                B.red(rsq[:, 0:4], mk_.rearrange("p (h d) -> p h d", d=256), ALU.add, r=[mkk], w=["rsq_tmp"])
                B.act(s_pxs[:, mt * 4:(mt + 1) * 4], rsq[:, 0:4], AF.Exp, r=["rsq_tmp"], w=["s_pxs"])
            bo = [B.next_bank(), B.next_bank()]
            bd = B.next_bank()
            for mt in range(2):
                mv_, mvk = s_mk[1], "s_mk1"
                B.dma(mv_, cmv[bb, mt * 128:(mt + 1) * 128, :], w=[mvk])
                mvb = oxT[:].rearrange("p c t -> p (c t)")
                B.copy(mvb, mv_, r=[mvk], w=["sr"])
                for hf in range(2):
                    B.mm(psum[bo[hf]][0:4, :], s_pxs[:, mt * 4:(mt + 1) * 4], mvb[:, hf * 512:(hf + 1) * 512], mt == 0, mt == 1,
                         r=["s_pxs", "sr"], w=[("ps", bo[hf])])
                B.mm(psum[bd][0:4, 0:1], s_pxs[:, mt * 4:(mt + 1) * 4], maskb[:, 127:128], mt == 0, mt == 1,
                     r=["s_pxs", "maskb"], w=[("ps", bd)])
            P.add("dve", lambda e, bd=bd: e.reciprocal(s_den[:, 0:1], psum[bd][0:4, 0:1]), r=[("ps", bd)], w=["s_den"])
            for hf in range(2):
                B.stt(s_om4[:, hf * 512:(hf + 1) * 512], psum[bo[hf]][0:4, :], s_den[:, 0:1], bm4[:, hf * 512:(hf + 1) * 512],
                      ALU.mult, ALU.mult, r=[("ps", bo[hf]), "s_den", "memkT"], w=["s_om4"])
                B.mm(psum[6 + hf][:n, :], i16[0:4, bb, :], s_om4[:, hf * 512:(hf + 1) * 512], bb == 0, bb == n - 1,
                     r=["i16", "s_om4"], w=[("ps", 6 + hf)])
        for hf in range(2):
            B.copy(s_catS[:, hf * 512:(hf + 1) * 512], psum[6 + hf][:n, :], r=[("ps", 6 + hf)], w=["s_catS"], eng="dve")
        transpose_tile(s_catS, "s_catS", catT, catk, D, ntok=n)
        for hf in range(2):
            b = B.next_bank()
            for kc in range(8):
                B.mm(psum[b][:n, :], catT[:, kc, 0:n], Wxo.ap[:, kc, hf * 512:(hf + 1) * 512], kc == 0, kc == 7,
                     r=[catk] + Wxo.keys, w=[("ps", b)])
            B.stt(pre[:n, hf * 512:(hf + 1) * 512], h1[:n, hf * 512:(hf + 1) * 512], float(ALPHA), psum[b][:n, :],
                  ALU.mult, ALU.add, r=[h1k, ("ps", b)], w=[prek])
        layer_norm(_Tile2(pre, n), prek, 2, _Tile2(xin[i], n), xk, ntok=n)
        B.dma(sc_h2[NT:NT + n, :], xin[i][:n, :], r=[xk], w=[("sc_h2", NT // 128)])
        B.bank_lim = 8

    if "A" in stages:
        for s in range(NSEQ):
            phaseA(s)
            if "B" in stages:
                phaseB(s)
    if "S" in stages:
        sampleA()
        sampleB()


    yacc_f, yacc_k = sview(0, 4096)
    yacc = yacc_f.rearrange("p (t d) -> p t d", d=D)
    h2Tf_f, h2Tf_k = sview(4096, 4096)
    h2Tf = h2Tf_f.rearrange("p (c t) -> p c t", t=512)
    h2Tb_f, h2Tb_k = sview(8192, 2048, BF16)
    h2Tb = h2Tb_f.rearrange("p (c t) -> p c t", t=512)
    hT_f, hT_k = sview(10240, 512, BF16)
    hT = hT_f.rearrange("p (f t) -> p f t", t=512)
    hT2_f, hT2_k = sview(11776, 512, BF16)
    hT2 = hT2_f.rearrange("p (f t) -> p f t", t=512)
    hTs = [(hT, ["hT_a"]), (hT2, ["hT_b"])]
    yout_f, yout_k = sview(10752, 1024)
    yout = [yout_f[:, 0:1024], yout_f[:, 0:1024]]
    EW = []
    for k_ in range(4):
        o = k_ * 6144
        EW.append((AV(arena, o, [8, 256]), AV(arena, o + 2048, [8, 256]), AV(arena, o + 4096, [2, 1024])))

    def load_expert(e):
        g_, u_, d_ = EW[e % 4]
        load_w(g_.ap, w_eg[e], g_.keys, step=8)
        load_w(u_.ap, w_eu[e], u_.keys, step=8)
        load_w(d_.ap, w_ed[e], d_.keys, kcn=2)

    def router(ti, ntok):
        R_ = lambda i, n=8: rt[:ntok, i, 0:n]
        rk = lambda i: ("rt", i)
        b = B.next_bank()
        for kc in range(8):
            B.mm(psum[b][:ntok, 0:36], h2Tf[:, kc, ti * 128:ti * 128 + ntok], wrt[:, kc, :], kc == 0, kc == 7,
                 r=h2Tf_k + ["wrt"], w=[("ps", b)])
        lg = rt[:ntok, 0, :]
        lgb = rt[:ntok, 1, :]
        B.copy(lg[:, 0:36] if False else rt[:ntok, 0:2, :].rearrange("p a b -> p (a b)")[:, 0:36], psum[b][:ntok, 0:36],
               r=[("ps", b)], w=[rk(0), rk(1)], eng="dve")
        lgf = rt[:ntok, 0:2, :].rearrange("p a b -> p (a b)")
        lbf = rt[:ntok, 2:4, :].rearrange("p a b -> p (a b)")
        B.tt(lbf[:, 0:36], lgf[:, 0:36], brt[:ntok, :], ALU.add, r=[rk(0), rk(1), "brt"], w=[rk(2), rk(3)])
        gl, glb = lgf[:, 0:4], lbf[:, 0:4]
        el = lgf[:, 4:36].rearrange("p (g e) -> p g e", e=8)
        elb = lbf[:, 4:36].rearrange("p (g e) -> p g e", e=8)
        sc = rt[:ntok, 4, :]
        sk = rk(4)
        B.red(sc[:, 0:1], gl, ALU.max, r=[rk(0)], w=[sk])
        B.ts(sc[:, 1:2], sc[:, 0:1], -1.0, None, ALU.mult, r=[sk], w=[sk])
        ex = rt[:ntok, 5, 0:4]
        B.act(ex, gl, AF.Exp, r=[rk(0), sk], w=[rk(5), sk], bias=sc[:, 1:2], accum_out=sc[:, 2:3])
        P.add("dve", lambda e: e.reciprocal(sc[:, 3:4], sc[:, 2:3]), r=[sk], w=[sk])
        B.red(sc[:, 4:5], glb, ALU.max, r=[rk(2)], w=[sk])
        ohg = rt[:ntok, 6, 0:4]
        B.ts(ohg, glb, sc[:, 4:5], None, ALU.is_equal, r=[rk(2), sk], w=[rk(6)])
        B.tt(ex, ex, ohg, ALU.mult, r=[rk(5), rk(6)], w=[rk(5)])
        B.red(sc[:, 5:6], ex, ALU.add, r=[rk(5)], w=[sk])
        B.ts(sc[:, 5:6], sc[:, 5:6], sc[:, 3:4], None, ALU.mult, r=[sk], w=[sk])
        ohg3 = ohg.unsqueeze(2).to_broadcast([ntok, 4, 8])
        tmp = rt[:ntok, 7, :].rearrange("p (g e) -> p g e", e=8)
        ing = rt[:ntok, 8, 0:8]
        B.tt(tmp, el, ohg3, ALU.mult, r=[rk(0), rk(1), rk(6)], w=[rk(7)])
        B.red(ing, tmp.rearrange("p g e -> p e g"), ALU.add, r=[rk(7)], w=[rk(8)])
        selb = rt[:ntok, 9, 0:8]
        B.tt(tmp, elb, ohg3, ALU.mult, r=[rk(2), rk(3), rk(6)], w=[rk(7)])
        B.red(selb, tmp.rearrange("p g e -> p e g"), ALU.add, r=[rk(7)], w=[rk(9)])
        oh1, oh2, sel2 = rt[:ntok, 10, 0:8], rt[:ntok, 11, 0:8], rt[:ntok, 12, 0:8]
        B.red(sc[:, 6:7], selb, ALU.max, r=[rk(9)], w=[sk])
        B.ts(oh1, selb, sc[:, 6:7], None, ALU.is_equal, r=[rk(9), sk], w=[rk(10)])
        B.stt(sel2, oh1, -1.0e30, selb, ALU.mult, ALU.add, r=[rk(10), rk(9)], w=[rk(12)])
        B.red(sc[:, 7:8], sel2, ALU.max, r=[rk(12)], w=[sk])
        B.ts(oh2, sel2, sc[:, 7:8], None, ALU.is_equal, r=[rk(12), sk], w=[rk(11)])
        t8 = rt[:ntok, 13, 0:8]
        B.tt(t8, oh1, ing, ALU.mult, r=[rk(10), rk(8)], w=[rk(13)])
        B.red(sc[:, 8:9], t8, ALU.add, r=[rk(13)], w=[sk])
        B.tt(t8, oh2, ing, ALU.mult, r=[rk(11), rk(8)], w=[rk(13)])
        B.red(sc[:, 9:10], t8, ALU.add, r=[rk(13)], w=[sk])
        B.tt(sc[:, 10:11], sc[:, 9:10], sc[:, 8:9], ALU.subtract, r=[sk], w=[sk])
        B.act(sc[:, 11:12], sc[:, 10:11], AF.Exp, r=[sk], w=[sk])
        B.ts(sc[:, 11:12], sc[:, 11:12], 1.0, None, ALU.add, r=[sk], w=[sk])
        P.add("dve", lambda e: e.reciprocal(sc[:, 11:12], sc[:, 11:12]), r=[sk], w=[sk])
        B.ts(sc[:, 12:13], sc[:, 11:12], -1.0, 1.0, ALU.mult, ALU.add, r=[sk], w=[sk])
        wsel = rt[:ntok, 14, 0:8]
        B.ts(wsel, oh1, sc[:, 11:12], None, ALU.mult, r=[rk(10), sk], w=[rk(14)])
        B.stt(wsel, oh2, sc[:, 12:13], wsel, ALU.mult, ALU.add, r=[rk(11), rk(14), sk], w=[rk(14)])
        B.ts(wsel, wsel, sc[:, 5:6], None, ALU.mult, r=[rk(14), sk], w=[rk(14)])
        B.tt(gates[:ntok, ti, :].rearrange("p (g e) -> p g e", e=8), ohg3, wsel.unsqueeze(1).to_broadcast([ntok, 4, 8]),
             ALU.mult, r=[rk(6), rk(14)], w=["gates"])

    def phaseC(row0, ntile, ntok, odram, orow0):
        N = (ntile - 1) * 128 + ntok
        B.bank_lim = 4
        B.psn = 0
        for e in range(3):
            load_expert(e)
        for ti in range(ntile):
            B.dma(yacc[:ntok, ti, :], sc_h2[row0 + ti * 128: row0 + ti * 128 + ntok, :],
                  r=[("sc_h2", (row0 + ti * 128) // 128)], w=yacc_k)
        for ti in range(ntile):
            transpose_tile(yacc[:, ti, :], yacc_k[0], h2Tf, h2Tf_k[0], D, tok0=ti * 128, ntok=ntok)
        B.copy(h2Tb[:, :, 0:N], h2Tf[:, :, 0:N], r=h2Tf_k, w=h2Tb_k, eng="dve")
        for ti in range(ntile):
            router(ti, ntok)
        P.add("act", lambda e: e.mul(yacc[:ntok, 0:ntile, :], yacc[:ntok, 0:ntile, :], float(ALPHA)), r=yacc_k + h2Tf_k, w=yacc_k)
        def gate_up(e):
            g_, u_, d_ = EW[e % 4]
            hTe, hTk = hTs[e % 2]
            for f in range(2):
                bg, bu = 2 * f, 2 * f + 1
                for kc in range(8):
                    B.mm(psum[bg][:, 0:N], g_.ap[:, kc, f * 128:(f + 1) * 128], h2Tb[:, kc, 0:N], kc == 0, kc == 7,
                         r=h2Tb_k + g_.keys, w=[("ps", bg)])
                for kc in range(8):
                    B.mm(psum[bu][:, 0:N], u_.ap[:, kc, f * 128:(f + 1) * 128], h2Tb[:, kc, 0:N], kc == 0, kc == 7,
                         r=h2Tb_k + u_.keys, w=[("ps", bu)])
                sgt, sgk = (zqk, "zqk") if f == 0 else (sr, "sr")
                B.act(sgt[:, 0:N], psum[bg][:, 0:N], AF.Silu, r=[("ps", bg)], w=[sgk])
                B.tt(hTe[:, f, 0:N], sgt[:, 0:N], psum[bu][:, 0:N], ALU.mult, r=[sgk, ("ps", bu)], w=hTk)

        def down(e):
            g_, u_, d_ = EW[e % 4]
            hTe, hTk = hTs[e % 2]
            for ti in range(ntile):
                for hf in range(2):
                    b = 4 + (B.next_bank())
                    for f in range(2):
                        B.mm(psum[b][:ntok, :], hTe[:, f, ti * 128:ti * 128 + ntok], d_.ap[:, f, hf * 512:(hf + 1) * 512],
                             f == 0, f == 1, r=hTk + d_.keys, w=[("ps", b)])
                    B.stt(yacc[:ntok, ti, hf * 512:(hf + 1) * 512], psum[b][:ntok, :], gates[:ntok, ti, e:e + 1],
                          yacc[:ntok, ti, hf * 512:(hf + 1) * 512], ALU.mult, ALU.add,
                          r=[("ps", b), "gates"] + yacc_k, w=yacc_k)

        gate_up(0)
        for e in range(32):
            if e + 3 < 32:
                load_expert(e + 3)
            if e + 1 < 32:
                gate_up(e + 1)
            down(e)
        for ti in range(ntile):
            yo = yout[ti % 2]
            pre_t = _Tile(yacc, ti, ntok)
            layer_norm(pre_t, yacc_k[0], 0, _Tile2(yo, ntok), yout_k[0], ntok=ntok)
            B.dma(odram[orow0 + ti * 128: orow0 + ti * 128 + ntok, :], yo[:ntok, :], r=yout_k, w=[("o_y", row0, ti)])
        B.bank_lim = 8

    if "C" in stages:
        barrier(SK + kT_keys + V_keys + yacc_k + h2Tf_k + h2Tb_k + hT_k + hT2_k + yout_k + ["hT_a", "hT_b"]
                + ["e1", "l1", "eb", "enb", "ekb", "wrt", "brt", "gates"] + [("rt", k_) for k_ in range(16)])
        B.dma(wrt[:], w_rt.rearrange("(kc p) n -> p kc n", p=128), w=["wrt"])
        B.dma(brt[:], _bc(b_rt, 36), w=["brt"])
        B.dma(lnp[0][:], _bc(ln_d[4], D), w=[("lnp", 0)])
        B.dma(lnp[1][:], _bc(ln_d[5], D), w=[("lnp", 1)])
        if "A" in stages:
            for blk in range(NT // 512):
                phaseC(blk * 512, 4, 128, o_y, blk * 512)
        if "S" in stages:
            phaseC(NT, 1, NS, o_ys, 0)

    if "dbg_h2" in stages:
        o_h2 = B.dout("o_h2", [NT, D])
        for tt_ in range(NT // 128):
            i = cnt["x"] % 2
            cnt["x"] += 1
            B.dma(xin[i][:], sc_h2[tt_ * 128:(tt_ + 1) * 128, :], r=[("sc_h2", tt_)], w=[("xin", i)])
            B.dma(o_h2[tt_ * 128:(tt_ + 1) * 128, :], xin[i][:], r=[("xin", i)], w=[("o_h2", tt_)])

    P.emit(nc, B.es)
    B.es.close()
    return nc


def make_consts():
    j = np.arange(128)[:, None]
    i = np.arange(128)[None, :]
    inv = (10000.0 ** (-np.arange(0, 32, 2, dtype=np.float32) / 32.0)).astype(np.float32)
    pos = (np.arange(16)[None, :] * 128 + np.arange(128)[:, None]).astype(np.float32)
    ang = pos[:, :, None] * inv[None, None, :]
    ang_s = (np.float32(8192.0) * inv)[None, :].repeat(NS, 0)
    return {
        "c_pcol": (np.arange(128) % 8).astype(np.float32).reshape(128, 1),
        "c_r8": (np.arange(128)[None, :] // 8 == np.arange(16)[:, None]).astype(np.float32),
        "c_i16": np.tile(np.eye(16, dtype=np.float32).reshape(1, 256), (64, 1)),
        "c_cos_s": np.cos(ang_s).astype(np.float32),
        "c_sin_s": np.sin(ang_s).astype(np.float32),
        "c_bm8": (np.arange(512)[None, :] // 64 == np.arange(8)[:, None]).astype(np.float32),
        "c_bm4": (np.arange(1024)[None, :] // 256 == np.arange(4)[:, None]).astype(np.float32),
        "ident": np.eye(128, dtype=np.float32),
        "c_us": np.where(j <= i, -1.0 / 16.0, 0.0).astype(np.float32),
        "c_lw": np.where(j > i, -1.0 / 16.0, 0.0).astype(np.float32),
        "c_mask": (j <= i).astype(np.float32),
        "c_cos": np.cos(ang).astype(np.float32),
        "c_sin": np.sin(ang).astype(np.float32),
    }


_NC_CACHE = {}


def _program():
    if "nc" not in _NC_CACHE:
        _NC_CACHE["nc"] = build(("memkv", "A", "B", "S", "C"))
    return _NC_CACHE["nc"]


def kernel(**inputs):
    f32 = lambda a: np.ascontiguousarray(np.asarray(a, dtype=np.float32))
    g = {k: np.asarray(v) for k, v in inputs.items()}
    consts = make_consts()
    shared = dict(consts)
    shared["w_mk"] = f32(g["w_mk"][0])
    shared["w_mv"] = f32(g["w_mv"][0])
    shared["w_in"] = f32(g["w_in"][0])
    shared["w_gate_aug"] = f32(np.concatenate([g["w_gla_gate"][0], g["b_gla_gate"]], axis=0))
    shared["gla_norm_g"] = f32(g["gla_norm_g"])
    shared["mla_q_norm_g"] = f32(g["mla_q_norm_g"])
    shared["mla_kv_norm_g"] = f32(g["mla_kv_norm_g"])
    shared["w_uq"] = f32(g["w_uq"][0].reshape(384, 768))
    shared["w_uk"] = f32(g["w_uk"][0].reshape(256, 512))
    shared["w_uv"] = f32(g["w_uv"][0].reshape(256, 512))
    shared["w_out"] = f32(g["w_out"][0])
    shared["w_xq"] = f32(g["w_xq"][0])
    shared["w_xo"] = f32(g["w_xo"][0])
    for i, k in enumerate(("ln1_g", "ln1_b", "ln2_g", "ln2_b", "ln3_g", "ln3_b")):
        shared["ln%d" % i] = f32(g[k])
    shared["w_rt"] = f32(np.concatenate([g["w_grp"][0], g["w_rtr"][0]], axis=1))
    shared["b_rt"] = f32(np.concatenate([g["b_grp"], g["b_rtr"]], axis=1))
    shared["w_e_gate"] = f32(g["w_e_gate"][0])
    shared["w_e_up"] = f32(g["w_e_up"][0])
    shared["w_e_down"] = f32(g["w_e_down"][0])
    shared["cache_lat"] = f32(g["cache_latent"][0].reshape(-1, 256))
    shared["cache_kr"] = f32(g["cache_krope"][0].reshape(-1, 32))
    in_maps = []
    for c in range(NCORES):
        m = dict(shared)
        m["xp"] = f32(g["x_prompt"][NSEQ * c:NSEQ * (c + 1)].reshape(NSEQ * SEQ, D))
        m["memp"] = f32(g["mem_prompt"][NSEQ * c:NSEQ * (c + 1)].reshape(NSEQ * 256, D))
        m["xs"] = f32(g["x_sample"][NS * c:NS * (c + 1), 0])
        m["sgla_s"] = f32(g["state_gla"][0, NS * c:NS * (c + 1)])
        m["cmk"] = f32(g["cache_mem_k"][0, NS * c:NS * (c + 1)].reshape(NS, 256, D))
        m["cmv"] = f32(g["cache_mem_v"][0, NS * c:NS * (c + 1)].reshape(NS, 256, D))
        m["pt"] = np.ascontiguousarray(g["page_table"][NS * c:NS * (c + 1)].reshape(1, NS * NPAGE).astype(np.int32))
        in_maps.append(m)
    nc = _program()
    res = run_bass_kernel_spmd(nc, in_maps, core_ids=list(range(NCORES)))
    R = res.results
    cat = lambda name: np.concatenate([np.asarray(R[c][name]) for c in range(NCORES)], axis=0)
    y_prompt = cat("o_y").reshape(16, SEQ, D)
    y_sample = cat("o_ys").reshape(128, 1, D)
    sgla_p = cat("o_sgla").reshape(1, 16, 4, 64, 128)
    lat_p = cat("o_lat").reshape(1, 16, SEQ, 256)
    kr_p = cat("o_kr").reshape(1, 16, SEQ, 32)
    mk_p = cat("o_memk").reshape(1, 16, 256, 4, 256)
    mv_p = cat("o_memv").reshape(1, 16, 256, 4, 256)
    sgla_s = cat("o_sgla_s").reshape(1, 128, 4, 64, 128)
    lat_s = cat("o_lat_s").reshape(1, 128, 1, 256)
    kr_s = cat("o_kr_s").reshape(1, 128, 1, 32)
    outs = (y_prompt, y_sample, sgla_p, lat_p, kr_p, mk_p, mv_p, sgla_s, lat_s, kr_s)
    return tuple(np.ascontiguousarray(o.astype(np.float32)) for o in outs)
```
